# Optimizing a Trainium2 kernel written in Bass

```python
import jax, jax.numpy as jnp
from jax import lax
import numpy as np

D_MODEL = 1024
BATCH = 8
SEQ = 4096
DEPTH = 2

GRID_W = 64
CTX_LEN = 256
MIX_W = D_MODEL
RG_W = MIX_W // 2
RG_BLOCKS = 8
RG_BLOCK_W = RG_W // RG_BLOCKS
RG_C = 8.0
CONV_W = 4
CONV_LEFT = 2
RET_W = MIX_W - RG_W
RET_HEADS = 4
RET_HEAD_DIM = RET_W // RET_HEADS
RET_CHUNK = 128
ROPE_BASE = 10000.0
IN_W = 2 * RG_W + 4 * RET_W
D_FF = 2816
N_EXPERTS = 8
TOP_K = 2
D_EXPERT = 3584
MOE_BLOCK = 512
N_DENSE = (DEPTH + 1) // 2
N_MOE = DEPTH // 2
EPS = 1e-6

kernel_name = 'hybrid_rglru_retention_moe_dit'


def rms_norm(x, g):
    xf = x.astype(jnp.float32)
    y = xf * lax.rsqrt(jnp.mean(xf * xf, axis=-1, keepdims=True) + EPS)
    return (y * g.astype(jnp.float32)).astype(x.dtype)


def modulate(h, shift, scale):
    return h * (1 + scale) + shift


def short_conv(u, w, b):
    n = u.shape[1]
    up = jnp.pad(u, ((0, 0), (CONV_LEFT, CONV_W - 1 - CONV_LEFT), (0, 0)))
    return sum(up[:, k:k + n] * w[k] for k in range(CONV_W)) + b


def _lin_combine(e1, e2):
    a1, b1 = e1
    a2, b2 = e2
    return a1 * a2, a2 * b1 + b2


def rglru(u, w_a, b_a, w_x, b_x, lam, h0, reverse):
    f32 = jnp.float32
    bsz, n, _ = u.shape
    ub = u.reshape(bsz, n, RG_BLOCKS, RG_BLOCK_W)
    r = jax.nn.sigmoid(jnp.einsum('bnkc,kcd->bnkd', ub, w_a.astype(f32)).reshape(bsz, n, RG_W) + b_a.astype(f32))
    i = jax.nn.sigmoid(jnp.einsum('bnkc,kcd->bnkd', ub, w_x.astype(f32)).reshape(bsz, n, RG_W) + b_x.astype(f32))
    log_a = -RG_C * r * jax.nn.softplus(-lam.astype(f32))
    a = jnp.exp(log_a)
    bt = jnp.sqrt(-jnp.expm1(2.0 * log_a)) * (i * u)
    a_cum, h = lax.associative_scan(_lin_combine, (a, bt), axis=1, reverse=reverse)
    h = h + a_cum * h0[:, None, :]
    return h, (h[:, 0] if reverse else h[:, -1])


def retention_dir(q, k, v, log_g, s0, strict):
    bsz, nh, n, d = q.shape
    L = RET_CHUNK
    nc = n // L
    q = q.reshape(bsz, nh, nc, L, d)
    k = k.reshape(bsz, nh, nc, L, d)
    v = v.reshape(bsz, nh, nc, L, d)
    j = jnp.arange(L, dtype=jnp.float32)
    diff = j[:, None] - j[None, :]
    mask = (diff > 0) if strict else (diff >= 0)
    dec = jnp.where(mask, jnp.exp(jnp.maximum(diff, 0.0) * log_g[:, None, None]), 0.0)
    scores = jnp.einsum('bhcid,bhcjd->bhcij', q, k) * dec[None, :, None]
    inner = jnp.einsum('bhcij,bhcjd->bhcid', scores, v)
    k_dec = k * jnp.exp((L - 1 - j)[None, :] * log_g[:, None])[None, :, None, :, None]
    kv = jnp.einsum('bhcjd,bhcje->cbhde', k_dec, v)
    chunk_decay = jnp.exp(L * log_g)[None, :, None, None]

    def step(s, kv_c):
        return chunk_decay * s + kv_c, s

    s_final, s_prev = lax.scan(step, s0, kv)
    q_dec = q * jnp.exp((j + 1)[None, :] * log_g[:, None])[None, :, None, :, None]
    cross = jnp.einsum('bhcid,cbhde->bhcie', q_dec, s_prev)
    return (inner + cross).reshape(bsz, nh, n, d), s_final


def retention_bidir(q, k, v, log_g_f, log_g_b, s0_f, s0_b):
    o_f, s_f = retention_dir(q, k, v, log_g_f, s0_f, False)
    flip = lambda t: jnp.flip(t, axis=2)
    o_b, s_b = retention_dir(flip(q), flip(k), flip(v), log_g_b, s0_b, True)
    return o_f + flip(o_b), s_f, s_b


def apply_rope(t, cos, sin):
    half = t.shape[-1] // 2
    t1, t2 = t[..., :half], t[..., half:]
    return jnp.concatenate([t1 * cos - t2 * sin, t1 * sin + t2 * cos], axis=-1)


def grid_rope(rows):
    row = jnp.repeat(jnp.arange(rows, dtype=jnp.float32), GRID_W)
    col = jnp.tile(jnp.arange(GRID_W, dtype=jnp.float32), rows)
    n_freq = RET_HEAD_DIM // 4
    inv = ROPE_BASE ** (-jnp.arange(n_freq, dtype=jnp.float32) / n_freq)
    ang = jnp.concatenate([row[:, None] * inv, col[:, None] * inv], axis=-1)
    return jnp.cos(ang), jnp.sin(ang)


def token_mixer(p, rope, states, conv_w, conv_b, rg_wa, rg_ba, rg_wx, rg_bx, rg_lam, ret_decay):
    f32 = jnp.float32
    bsz, n, _ = p.shape
    p = p.astype(f32)
    u, y_gate, q, k, v, g = jnp.split(
        p, [RG_W, 2 * RG_W, 2 * RG_W + RET_W, 2 * RG_W + 2 * RET_W, 2 * RG_W + 3 * RET_W], axis=-1)
    h_f0, h_b0, s_f0, s_b0 = states
    u = short_conv(u, conv_w.astype(f32), conv_b.astype(f32))
    h_f, hf_last = rglru(u, rg_wa[0], rg_ba[0], rg_wx[0], rg_bx[0], rg_lam[0], h_f0, False)
    h_b, hb_last = rglru(u, rg_wa[1], rg_ba[1], rg_wx[1], rg_bx[1], rg_lam[1], h_b0, True)
    rg_out = jax.nn.gelu(y_gate) * (h_f + h_b)
    heads = lambda t: t.reshape(bsz, n, RET_HEADS, RET_HEAD_DIM).transpose(0, 2, 1, 3)
    q, k, v = heads(q), heads(k) * RET_HEAD_DIM ** -0.5, heads(v)
    if rope is not None:
        cos, sin = rope
        q, k = apply_rope(q, cos, sin), apply_rope(k, cos, sin)
    log_g = jax.nn.log_sigmoid(ret_decay.astype(f32))
    o, s_f, s_b = retention_bidir(q, k, v, log_g[0], log_g[1], s_f0, s_b0)
    mu = jnp.mean(o, axis=-1, keepdims=True)
    var = jnp.mean(jnp.square(o - mu), axis=-1, keepdims=True)
    o = ((o - mu) * lax.rsqrt(var + EPS)).transpose(0, 2, 1, 3).reshape(bsz, n, RET_W)
    ret_out = jax.nn.silu(g) * o
    y = jnp.concatenate([rg_out, ret_out], axis=-1)
    return y, (hf_last, hb_last, s_f, s_b)


def swiglu(h, w1, w3, w2):
    return (jax.nn.silu(h @ w1) * (h @ w3)) @ w2


def moe_swiglu(h, w_router, b_router, w1, w3, w2):
    bsz, n, d = h.shape
    t = h.reshape(-1, d)
    n_tok = t.shape[0]
    n_asg = n_tok * TOP_K
    logits = (t @ w_router).astype(jnp.float32) + b_router.astype(jnp.float32)
    top_v, top_i = lax.top_k(logits, TOP_K)
    top_w = jax.nn.softmax(top_v, axis=-1)
    flat_e = top_i.reshape(-1)
    flat_w = top_w.reshape(-1)
    flat_tok = jnp.arange(n_asg, dtype=jnp.int32) // TOP_K
    order = jnp.argsort(flat_e)
    sorted_e = flat_e[order]
    counts = jnp.zeros((N_EXPERTS,), jnp.int32).at[flat_e].add(1)
    padded = (counts + MOE_BLOCK - 1) // MOE_BLOCK * MOE_BLOCK
    pad_end = jnp.cumsum(padded)
    pad_start = pad_end - padded
    start = jnp.cumsum(counts) - counts
    rank = jnp.arange(n_asg, dtype=jnp.int32) - start[sorted_e]
    dest = pad_start[sorted_e] + rank
    n_rows = (-(-n_asg // MOE_BLOCK)) * MOE_BLOCK + N_EXPERTS * MOE_BLOCK
    n_blocks = n_rows // MOE_BLOCK
    buf_tok = jnp.zeros((n_rows,), jnp.int32).at[dest].set(flat_tok[order])
    buf_w = jnp.zeros((n_rows,), jnp.float32).at[dest].set(flat_w[order])
    block_e = jnp.minimum(
        jnp.searchsorted(pad_end, jnp.arange(n_blocks, dtype=jnp.int32) * MOE_BLOCK, side='right'),
        N_EXPERTS - 1)

    def block_ffn(args):
        tok, e = args
        return swiglu(t[tok], w1[e], w3[e], w2[e])

    y = lax.map(block_ffn, (buf_tok.reshape(n_blocks, MOE_BLOCK), block_e))
    out = jnp.zeros((n_tok, d), jnp.float32).at[buf_tok].add(
        buf_w[:, None] * y.reshape(n_rows, d).astype(jnp.float32))
    return out.reshape(bsz, n, d).astype(h.dtype)


def channel_mixer(l, h, ffn_w1, ffn_w3, ffn_w2, moe_router, moe_router_b, moe_w1, moe_w3, moe_w2):
    i = l // 2
    if l % 2 == 0:
        return swiglu(h, ffn_w1[i], ffn_w3[i], ffn_w2[i])
    return moe_swiglu(h, moe_router[i], moe_router_b[i], moe_w1[i], moe_w3[i], moe_w2[i])


def setup_inputs(seed: int = 0) -> dict:
    key = jax.random.key(seed)
    ks = jax.random.split(key, 32)
    f32 = jnp.float32
    nrm = lambda k, shape, s: jax.random.normal(k, shape, f32) * s
    D = D_MODEL
    u = jax.random.uniform(ks[14], (DEPTH, 2, RG_W), f32, 0.9, 0.999)
    a = u ** (1.0 / RG_C)
    rg_lam = jnp.log(a) - jnp.log1p(-a)
    gamma = 1.0 - 2.0 ** (-5.0 - jnp.arange(RET_HEADS, dtype=f32))
    ret_decay = (jnp.log(gamma) - jnp.log1p(-gamma))[None, None, :] + nrm(ks[15], (DEPTH, 2, RET_HEADS), 0.1)
    return {
        'x': nrm(ks[0], (BATCH, SEQ, D), 1.0),
        'c': nrm(ks[1], (BATCH, D), 1.0),
        'ctx': nrm(ks[2], (BATCH, CTX_LEN, D), 1.0),
        'c_ctx': nrm(ks[3], (D,), 1.0),
        'w_mod': nrm(ks[4], (DEPTH, D, 6 * D), D ** -0.5),
        'b_mod': nrm(ks[5], (DEPTH, 6 * D), 0.02),
        'g_mix': 1.0 + nrm(ks[6], (DEPTH, D), 0.02),
        'g_ffn': 1.0 + nrm(ks[7], (DEPTH, D), 0.02),
        'g_final': 1.0 + nrm(ks[8], (D,), 0.02),
        'w_in': nrm(ks[9], (DEPTH, D, IN_W), D ** -0.5),
        'w_out': nrm(ks[10], (DEPTH, MIX_W, D), MIX_W ** -0.5),
        'conv_w': nrm(ks[11], (DEPTH, CONV_W, RG_W), CONV_W ** -0.5),
        'conv_b': nrm(ks[12], (DEPTH, RG_W), 0.02),
        'rg_wa': nrm(ks[13], (DEPTH, 2, RG_BLOCKS, RG_BLOCK_W, RG_BLOCK_W), RG_BLOCK_W ** -0.5),
        'rg_ba': nrm(ks[16], (DEPTH, 2, RG_W), 0.02),
        'rg_wx': nrm(ks[17], (DEPTH, 2, RG_BLOCKS, RG_BLOCK_W, RG_BLOCK_W), RG_BLOCK_W ** -0.5),
        'rg_bx': nrm(ks[18], (DEPTH, 2, RG_W), 0.02),
        'rg_lam': rg_lam,
        'ret_decay': ret_decay,
        'ffn_w1': nrm(ks[19], (N_DENSE, D, D_FF), D ** -0.5),
        'ffn_w3': nrm(ks[20], (N_DENSE, D, D_FF), D ** -0.5),
        'ffn_w2': nrm(ks[21], (N_DENSE, D_FF, D), D_FF ** -0.5),
        'moe_router': nrm(ks[22], (N_MOE, D, N_EXPERTS), D ** -0.5),
        'moe_router_b': nrm(ks[23], (N_MOE, N_EXPERTS), 0.01),
        'moe_w1': nrm(ks[24], (N_MOE, N_EXPERTS, D, D_EXPERT), D ** -0.5),
        'moe_w3': nrm(ks[25], (N_MOE, N_EXPERTS, D, D_EXPERT), D ** -0.5),
        'moe_w2': nrm(ks[26], (N_MOE, N_EXPERTS, D_EXPERT, D), D_EXPERT ** -0.5),
    }


def reference(x, c, ctx, c_ctx, w_mod, b_mod, g_mix, g_ffn, g_final, w_in, w_out, conv_w, conv_b,
              rg_wa, rg_ba, rg_wx, rg_bx, rg_lam, ret_decay, ffn_w1, ffn_w3, ffn_w2,
              moe_router, moe_router_b, moe_w1, moe_w3, moe_w2):
    f32 = jnp.float32
    bsz, n_lat, _ = x.shape
    rows = n_lat // GRID_W
    rope = grid_rope(rows)
    sc_lat = jax.nn.silu(c)
    sc_ctx = jax.nn.silu(c_ctx)
    ffn_args = (ffn_w1, ffn_w3, ffn_w2, moe_router, moe_router_b, moe_w1, moe_w3, moe_w2)
    for l in range(DEPTH):
        last = l == DEPTH - 1
        mod_lat = jnp.split((sc_lat @ w_mod[l] + b_mod[l])[:, None, :], 6, axis=-1)
        mod_ctx = jnp.split((sc_ctx @ w_mod[l] + b_mod[l])[None, None, :], 6, axis=-1)
        mix_p = (conv_w[l], conv_b[l], rg_wa[l], rg_ba[l], rg_wx[l], rg_bx[l], rg_lam[l], ret_decay[l])
        zero_states = (jnp.zeros((bsz, RG_W), f32), jnp.zeros((bsz, RG_W), f32),
                       jnp.zeros((bsz, RET_HEADS, RET_HEAD_DIM, RET_HEAD_DIM), f32),
                       jnp.zeros((bsz, RET_HEADS, RET_HEAD_DIM, RET_HEAD_DIM), f32))
        pc = modulate(rms_norm(ctx, g_mix[l]), mod_ctx[0], mod_ctx[1]) @ w_in[l]
        y_ctx, ctx_states = token_mixer(pc, None, zero_states, *mix_p)
        px = modulate(rms_norm(x, g_mix[l]), mod_lat[0], mod_lat[1]) @ w_in[l]
        y_lat, _ = token_mixer(px, rope, ctx_states, *mix_p)
        x = x + mod_lat[2] * (y_lat.astype(x.dtype) @ w_out[l])
        hx = modulate(rms_norm(x, g_ffn[l]), mod_lat[3], mod_lat[4])
        x = x + mod_lat[5] * channel_mixer(l, hx, *ffn_args)
        if not last:
            ctx = ctx + mod_ctx[2] * (y_ctx.astype(ctx.dtype) @ w_out[l])
            hc = modulate(rms_norm(ctx, g_ffn[l]), mod_ctx[3], mod_ctx[4])
            ctx = ctx + mod_ctx[5] * channel_mixer(l, hc, *ffn_args)
    return rms_norm(x, g_final)
```

```python
import contextlib
import numpy as np
import ml_dtypes
import concourse.bass as bass
import concourse.mybir as mybir
from concourse.bass_utils import run_bass_kernel_spmd

F32 = mybir.dt.float32
BF16 = mybir.dt.bfloat16
I32 = mybir.dt.int32
AF = mybir.ActivationFunctionType
ALU = mybir.AluOpType
AX = mybir.AxisListType

D = 1024
NLAT = 4096
NCTX = 256
NT = NLAT + NCTX
NCH = NT // 128
UW = NT + 8
DFF = 2816
DEXP = 3584
NEXP = 8
EPS = 1e-6
MACROS = [(0, 256)] + [(256 + 512 * i, 512) for i in range(8)]

ENGS = ("pe", "act", "dve", "pool", "sp")
DMA_RING = 24
SAME_ENGINE_ALL = True


def ucol(g):
    return g + 2 if g < NCTX else g + 5


class Op:
    __slots__ = ("eng", "fn", "deps", "dma", "signals", "sigval", "dsem", "dval", "dprev", "ring")

    def __init__(self, eng, fn, dma):
        self.eng = eng
        self.fn = fn
        self.dma = dma
        self.deps = []
        self.signals = False
        self.sigval = 0
        self.dsem = None
        self.dval = 0
        self.dprev = 0
        self.ring = "n"


class Prog:
    def __init__(self, nc):
        self.nc = nc
        self.ops = {e: [] for e in ENGS}
        self.lastw = {}
        self.readers = {}
        self.ndma = {e: 0 for e in ENGS}
        self.ndma_bg = {e: 0 for e in ENGS}
        self.dmas_bg = {e: [] for e in ENGS}
        self.lastc = {e: None for e in ENGS}
        self.dmas = {e: [] for e in ENGS}

    def _add(self, eng, fn, reads, writes, dma, extra=(), bg=False):
        op = Op(eng, fn, dma)
        deps = list(extra)
        for k in reads:
            w = self.lastw.get(k)
            if w is not None:
                deps.append(w)
            if k.startswith("ps"):
                deps.extend(r for r in self.readers.get(k, ()) if r.eng != eng)
        nraw = len(deps)
        for k in writes:
            w = self.lastw.get(k)
            if w is not None:
                deps.append(w)
            deps.extend(self.readers.get(k, ()))
        raw_ids = set(id(d) for d in deps[:nraw])
        for k in reads:
            self.readers.setdefault(k, []).append(op)
        for k in writes:
            self.lastw[k] = op
            self.readers[k] = []
        seen = set()
        for d in deps:
            if d is op or id(d) in seen:
                continue
            seen.add(id(d))
            if (not d.dma) and (not dma) and d.eng == eng and fn is not None:
                if eng == "pe" or (id(d) not in raw_ids and not SAME_ENGINE_ALL):
                    continue
            op.deps.append(d)
            if not d.dma:
                d.signals = True
        if dma and bg:
            i = self.ndma_bg[eng]
            self.ndma_bg[eng] = i + 1
            op.ring = "bg"
            op.dsem = i % DMA_RING
            op.dval = 16 * (i // DMA_RING + 1)
            op.dprev = 16 * (i // DMA_RING)
            self.dmas_bg[eng].append(op)
        elif dma:
            i = self.ndma[eng]
            self.ndma[eng] = i + 1
            op.dsem = i % DMA_RING
            op.dval = 16 * (i // DMA_RING + 1)
            op.dprev = 16 * (i // DMA_RING)
            self.dmas[eng].append(op)
        elif fn is not None:
            self.lastc[eng] = op
        self.ops[eng].append(op)
        return op

    def op(self, eng, fn, reads=(), writes=()):
        return self._add(eng, fn, reads, writes, False)

    def dma(self, eng, fn, reads=(), writes=(), bg=False):
        return self._add(eng, fn, reads, writes, True, bg=bg)

    def barrier(self, include_bg=False):
        pend = [o for o in self.lastc.values() if o is not None]
        for e in ENGS:
            pend.extend(self.dmas[e][-DMA_RING:])
            if include_bg:
                pend.extend(self.dmas_bg[e][-DMA_RING:])
        for e in ENGS:
            self._add(e, None, (), (), False, extra=pend)
        self.lastw = {}
        self.readers = {}

    def emit(self):
        nc = self.nc
        for e in ENGS:
            cnt = 0
            for op in self.ops[e]:
                if (not op.dma) and op.fn is not None and op.signals:
                    cnt += 1
                    op.sigval = cnt
        with contextlib.ExitStack() as st:
            csem = {e: st.enter_context(nc.semaphore("c_" + e)) for e in ENGS}
            dsem = {(e, "n"): [st.enter_context(nc.semaphore("d_%s%d" % (e, i))) for i in range(DMA_RING)]
                    for e in ENGS if self.ndma[e] > 0}
            dsem.update({(e, "bg"): [st.enter_context(nc.semaphore("b_%s%d" % (e, i))) for i in range(DMA_RING)]
                         for e in ENGS if self.ndma_bg[e] > 0})
            block = st.enter_context(nc.Block())
            handles = {"pe": nc.tensor, "act": nc.scalar, "dve": nc.vector, "pool": nc.gpsimd, "sp": nc.sync}

            def build(e):
                eng = handles[e]
                seen = {}

                def wait(key, sem, val):
                    if seen.get(key, 0) >= val:
                        return
                    seen[key] = val
                    eng.wait_ge(sem, val)

                for op in self.ops[e]:
                    for d in op.deps:
                        if d.dma:
                            wait(("d", d.eng, d.ring, d.dsem), dsem[(d.eng, d.ring)][d.dsem], d.dval)
                        else:
                            wait(("c", d.eng), csem[d.eng], d.sigval)
                    if op.fn is None:
                        continue
                    if op.dma:
                        if op.dprev > 0:
                            wait(("d", e, op.ring, op.dsem), dsem[(e, op.ring)][op.dsem], op.dprev)
                        op.fn(eng).then_inc(dsem[(e, op.ring)][op.dsem], 16)
                    else:
                        ins = op.fn(eng)
                        if op.signals:
                            ins.then_inc(csem[e], 1)

            @block.tensor
            def _(t):
                build("pe")

            @block.scalar
            def _(t):
                build("act")

            @block.vector
            def _(t):
                build("dve")

            @block.gpsimd
            def _(t):
                build("pool")

            @block.sync
            def _(t):
                build("sp")


class Arena:
    def __init__(self, nc):
        self.nc = nc
        self.base = (nc.sbuf_base + 63) // 64 * 64
        self.top = nc.sbuf_top
        self.off = self.base
        self.n = 0

    def alloc(self, name, shape, dt):
        nbytes = int(np.prod(shape[1:])) * (2 if dt == BF16 else 4)
        nbytes = (nbytes + 63) // 64 * 64
        assert self.off + nbytes <= self.top, ("SBUF overflow", name, self.off, nbytes, self.top)
        self.n += 1
        t = self.nc.alloc_sbuf_tensor_at("%s_%d" % (name, self.n), list(shape), dt, offset=self.off)
        self.off += nbytes
        return t

    def mark(self):
        return self.off

    def reset(self, m):
        self.off = m


class _Stop(Exception):
    pass


def build_program(n_layers=2, debug=False, stop=None, n_exp=NEXP):
    nc = bass.Bass("TRN2", target_bir_lowering=False)
    P = Prog(nc)
    A = Arena(nc)
    ikind = "ExternalOutput" if debug else "Internal"

    def din(name, shape, dt=F32):
        return nc.dram_tensor(name, list(shape), dt, kind="ExternalInput").ap()

    def dscr(name, shape, dt):
        return nc.dram_tensor(name, list(shape), dt, kind=ikind).ap()

    xin = din("xin", [NT, D])
    c_b = din("c_b", [D])
    c_ctx = din("c_ctx", [D])
    w_mod = din("w_mod", [2, D, 6 * D])
    b_mod = din("b_mod", [2, 6 * D])
    g_mix = din("g_mix", [2, D])
    g_ffn = din("g_ffn", [2, D])
    g_final = din("g_final", [D])
    w_in = din("w_in", [2, D, 4096])
    w_out = din("w_out", [2, D, D])
    conv_w = din("conv_w", [2, 4, 512])
    conv_b = din("conv_b", [2, 512])
    rg_wa = din("rg_wa", [2, 2, 8, 64, 64])
    rg_ba = din("rg_ba", [2, 2, 512])
    rg_wx = din("rg_wx", [2, 2, 8, 64, 64])
    rg_bx = din("rg_bx", [2, 2, 512])
    rg_lam = din("rg_lam", [2, 2, 512])
    ret_decay = din("ret_decay", [2, 8])
    ffn_w1 = din("ffn_w1", [1, D, DFF])
    ffn_w3 = din("ffn_w3", [1, D, DFF])
    ffn_w2 = din("ffn_w2", [1, DFF, D])
    moe_router = din("moe_router", [D, NEXP])
    moe_router_b = din("moe_router_b", [NEXP])
    moe_w1 = din("moe_w1", [NEXP, D, DEXP])
    moe_w3 = din("moe_w3", [NEXP, D, DEXP])
    moe_w2 = din("moe_w2", [NEXP, DEXP, D])
    k_identb = din("k_identb", [128, 128], BF16)
    k_identf = din("k_identf", [128, 128])
    k_cos = din("k_cos", [128, NT])
    k_sin = din("k_sin", [128, NT])
    k_tab = din("k_tab", [128, 8, 128])
    out = nc.dram_tensor("out", [NLAT, D], F32, kind="ExternalOutput").ap()

    XS = dscr("s_x", [NT, D], F32)
    X1 = dscr("s_x1", [NT, D], F32)
    MODB = dscr("s_modb", [4, 128, 6 * D], F32)
    U = dscr("s_u", [512, UW], BF16)
    GY = dscr("s_gy", [512, NT], BF16)
    Q = dscr("s_q", [512, NT], BF16)
    K = dscr("s_k", [512, NT], BF16)
    KT = dscr("s_kt", [NT, 512], BF16)
    VT = dscr("s_vt", [NT, 512], BF16)
    SGT = dscr("s_sgt", [NT, 512], BF16)
    SF = dscr("s_sf", [NCH, 128, 512], BF16)
    SB = dscr("s_sb", [NCH, 128, 512], BF16)
    YRG = dscr("s_yrg", [512, NT], BF16)
    H2 = dscr("s_h2", [D, NT], BF16)

    def dscr_i(name, shape, dt):
        return nc.dram_tensor(name, list(shape), dt, kind="Internal").ap()

    FW1b = dscr_i("s_fw1", [128, 8, DFF], BF16)
    FW3b = dscr_i("s_fw3", [128, 8, DFF], BF16)
    FW2b = dscr_i("s_fw2", [128, DFF // 128, D], BF16)
    MW1q = dscr_i("s_mw1", [NEXP * 4 * 128, 8 * 896], BF16)
    MW3q = dscr_i("s_mw3", [NEXP * 4 * 128, 8 * 896], BF16)
    MW2q = dscr_i("s_mw2", [NEXP * 4 * 128, 7 * D], BF16)
    UNIT = 512
    NUN = 8192 // UNIT + NEXP
    NROWS = NUN * UNIT
    H2T = dscr("s_h2t", [NLAT, D], BF16)
    HS = dscr("s_hs", [NROWS, D], BF16)
    YS = dscr("s_ys", [NROWS, D], F32)
    k_zero = din("k_zero", [128, 4096], BF16)
    k_tab2 = din("k_tab2", [128, 2, 128])

    uid = [0]

    def key(s):
        uid[0] += 1
        return "%s#%d" % (s, uid[0])

    class T:
        def __init__(self, t, k):
            self.t = t
            self.k = k

        def __getitem__(self, idx):
            return self.t[idx]

    def sb(name, shape, dt):
        return T(A.alloc(name, shape, dt), key(name))

    psum_ctx = contextlib.ExitStack()
    PS = [T(psum_ctx.enter_context(nc.psum_tensor("ps%d" % i, [128, 512], F32)), "ps%d" % i) for i in range(8)]

    def ps_bf(i):
        return PS[i].t.bitcast(BF16) if hasattr(PS[i].t, "bitcast") else None

    def I(eng, name, *args, reads=(), writes=(), **kw):
        return P.op(eng, lambda e: getattr(e, name)(*args, **kw), reads=reads, writes=writes)

    def dma(q, out_ap, in_ap, reads, writes):
        return P.dma(q, lambda e: e.dma_start(out=out_ap, in_=in_ap), reads=reads, writes=writes)

    def cdma(out_ap, in_ap, reads, writes):
        return P.dma("pool", lambda e: e.dma_start(out=out_ap, in_=in_ap), reads=reads, writes=writes)

    pre_list = []

    def add_pre(w1, w3, w2, d1, d3, d2, F):
        hw = F // 2
        for kc in range(8):
            for hh in range(2):
                pre_list.append((d1[:, kc, hh * hw:(hh + 1) * hw], w1[kc * 128:(kc + 1) * 128, hh * hw:(hh + 1) * hw]))
                pre_list.append((d3[:, kc, hh * hw:(hh + 1) * hw], w3[kc * 128:(kc + 1) * 128, hh * hw:(hh + 1) * hw]))
        for fc in range(F // 128):
            pre_list.append((d2[:, fc, :], w2[fc * 128:(fc + 1) * 128, :]))

    add_pre(ffn_w1[0], ffn_w3[0], ffn_w2[0], FW1b, FW3b, FW2b, DFF)
    n_ffn_pre = len(pre_list)
    for e_ in range(n_exp):
        for q in range(4):
            r0_ = (e_ * 4 + q) * 128
            for kc in range(8):
                pre_list.append((MW1q[r0_:r0_ + 128, kc * 896:(kc + 1) * 896], moe_w1[e_][kc * 128:(kc + 1) * 128, q * 896:(q + 1) * 896]))
                pre_list.append((MW3q[r0_:r0_ + 128, kc * 896:(kc + 1) * 896], moe_w3[e_][kc * 128:(kc + 1) * 128, q * 896:(q + 1) * 896]))
            for j in range(7):
                pre_list.append((MW2q[r0_:r0_ + 128, j * D:(j + 1) * D], moe_w2[e_][(q * 7 + j) * 128:(q * 7 + j + 1) * 128, :]))
    pre_pos = [0]

    def precast_some(n):
        while n > 0 and pre_pos[0] < len(pre_list):
            d_, s_ = pre_list[pre_pos[0]]
            pre_pos[0] += 1
            n -= 1
            P.dma("pool", lambda e, d_=d_, s_=s_: e.dma_start(out=d_, in_=s_), reads=[], writes=["Wb"], bg=(pre_pos[0] > n_ffn_pre))

    g_base = A.mark()
    identb = sb("identb", [128, 128], BF16)
    identf = sb("identf", [128, 128], F32)
    ones = sb("ones", [128, 128], F32)
    zeros_bf = sb("zeros", [128, 8], BF16)
    RW = sb("RW", [128, 32, NEXP], F32)
    SEL = sb("SEL", [128, 32, NEXP], F32)
    OH1 = sb("OH1", [128, 32, NEXP], F32)
    dma("sp", identb[:], k_identb[:], [], [identb.k])
    dma("sp", identf[:], k_identf[:], [], [identf.k])
    I("dve", "memset", ones[:], 1.0, writes=[ones.k])
    I("dve", "memset", zeros_bf[:], 0.0, writes=[zeros_bf.k])
    phase_base = A.mark()

    def sqrt_recip(dst, src, scale, nparts=128):
        I("act", "activation", out=dst[:], in_=src[:], func=AF.Sqrt, bias=epsc[:], scale=scale,
             reads=[src.k, epsc.k], writes=[dst.k])
        I("dve", "reciprocal", out=dst[:], in_=dst[:], reads=[dst.k], writes=[dst.k])

    epsc = sb("epsc", [128, 1], F32)
    I("dve", "memset", epsc[:], EPS, writes=[epsc.k])
    phase_base = A.mark()

    def chk(l_, ph):
        if stop is not None and stop == (l_, ph):
            raise _Stop()

    def moe_sorted(l):
        A.reset(phase_base)
        precast_some(10 ** 6)
        P.barrier(include_bg=True)
        tab2 = sb("tab2", [128, 2, 128], F32)
        dma("sp", tab2[:], k_tab2[:], [], [tab2.k])
        ACCa = sb("ACCa", [128, 33, NEXP], F32)
        I("dve", "memset", ACCa[:, 0, :], 0.0, writes=[ACCa.k])
        for t in range(32):
            I("dve", "tensor_tensor", out=ACCa[:, t + 1, :], in0=ACCa[:, t, :], in1=SEL[:, t, :], op=ALU.add, reads=[ACCa.k, SEL.k], writes=[ACCa.k])
        pc_, pr_ = PS[0], PS[1]
        I("pe", "matmul", pc_[:, 0:NEXP], lhsT=ones[:], rhs=ACCa[:, 32, :], start=True, stop=True, reads=[ones.k, ACCa.k], writes=[pc_.k])
        sm = sb("sm", [128, 8, NEXP], F32)
        I("dve", "tensor_copy", out=sm[:, 0, :], in_=pc_[:, 0:NEXP], reads=[pc_.k], writes=[sm.k])
        I("dve", "tensor_scalar", out=sm[:, 1, :], in0=sm[:, 0, :], scalar1=0.5, scalar2=None, op0=ALU.is_gt, reads=[sm.k], writes=[sm.k])
        for j in range(1, 8):
            I("dve", "scalar_tensor_tensor", out=sm[:, 1, :], in0=sm[:, 0, :], scalar=UNIT * j + 0.5, in1=sm[:, 1, :], op0=ALU.is_gt, op1=ALU.add,
              reads=[sm.k], writes=[sm.k])
        I("dve", "tensor_copy", out=sm[:, 2, 0:1], in_=sm[:, 1, 0:1], reads=[sm.k], writes=[sm.k])
        for e_ in range(1, NEXP):
            I("dve", "tensor_tensor", out=sm[:, 2, e_:e_ + 1], in0=sm[:, 2, e_ - 1:e_], in1=sm[:, 1, e_:e_ + 1], op=ALU.add, reads=[sm.k], writes=[sm.k])
        I("dve", "tensor_tensor", out=sm[:, 3, :], in0=sm[:, 2, :], in1=sm[:, 1, :], op=ALU.subtract, reads=[sm.k], writes=[sm.k])
        I("dve", "tensor_scalar", out=sm[:, 4, :], in0=sm[:, 3, :], scalar1=UNIT / 128.0, scalar2=None, op0=ALU.mult, reads=[sm.k], writes=[sm.k])
        I("dve", "tensor_scalar", out=sm[:, 2, :], in0=sm[:, 2, :], scalar1=float(UNIT), scalar2=None, op0=ALU.mult, reads=[sm.k], writes=[sm.k])
        for t in range(32):
            cs = slice(t * NEXP, (t + 1) * NEXP)
            I("pe", "matmul", pr_[:, cs], lhsT=ones[:], rhs=ACCa[:, t, :], start=True, stop=False, reads=[ones.k, ACCa.k], writes=[pr_.k])
            I("pe", "matmul", pr_[:, cs], lhsT=ones[:], rhs=sm[:, 4, :], start=False, stop=False, reads=[ones.k, sm.k], writes=[pr_.k])
            I("pe", "matmul", pr_[:, cs], lhsT=tab2[:, 0, :], rhs=SEL[:, t, :], start=False, stop=True, reads=[tab2.k, SEL.k], writes=[pr_.k])
        DST = sb("DST", [128, 32, NEXP], F32)
        I("dve", "tensor_tensor", out=DST[:].rearrange("p t e -> p (t e)"), in0=pr_[:, 0:256], in1=SEL[:].rearrange("p t e -> p (t e)"), op=ALU.subtract,
          reads=[pr_.k, SEL.k], writes=[DST.k])
        OH2 = sb("OH2", [128, 32, NEXP], F32)
        I("dve", "tensor_tensor", out=OH2[:], in0=SEL[:], in1=OH1[:], op=ALU.subtract, reads=[SEL.k, OH1.k], writes=[OH2.k])
        tmpr = sb("tmpr", [128, 32, NEXP], F32)
        IDXf = sb("IDXf", [128, 2, 32], F32)
        WGT = sb("WGT", [128, 2, 32], F32)
        IDXi = sb("IDXi", [128, 2, 32], I32)
        for k_, oh in enumerate((OH1, OH2)):
            I("dve", "tensor_tensor", out=tmpr[:], in0=oh[:], in1=DST[:], op=ALU.mult, reads=[oh.k, DST.k], writes=[tmpr.k])
            I("dve", "tensor_reduce", out=IDXf[:, k_, :], in_=tmpr[:], axis=AX.X, op=ALU.add, reads=[tmpr.k], writes=[IDXf.k])
            I("dve", "tensor_tensor", out=tmpr[:], in0=oh[:], in1=RW[:], op=ALU.mult, reads=[oh.k, RW.k], writes=[tmpr.k])
            I("dve", "tensor_reduce", out=WGT[:, k_, :], in_=tmpr[:], axis=AX.X, op=ALU.add, reads=[tmpr.k], writes=[WGT.k])
        I("dve", "tensor_copy", out=IDXi[:], in_=IDXf[:], reads=[IDXf.k], writes=[IDXi.k])
        eu = sb("eu", [128, NUN], F32)
        pq = sb("pq", [128, 4], F32)
        for q in range(4):
            I("dve", "tensor_scalar", out=pq[:, q:q + 1], in0=tab2[:, 1, 0:1], scalar1=128.0 * q, scalar2=None, op0=ALU.add, reads=[tab2.k], writes=[pq.k])
        for u in range(NUN):
            I("dve", "tensor_scalar", out=sm[:, 5, :], in0=sm[:, 2, :], scalar1=float(UNIT) * u + 0.5, scalar2=None, op0=ALU.is_lt, reads=[sm.k], writes=[sm.k])
            I("dve", "tensor_reduce", out=eu[:, u:u + 1], in_=sm[:, 5, :], axis=AX.X, op=ALU.add, reads=[sm.k], writes=[eu.k])
        I("dve", "tensor_scalar", out=eu[:], in0=eu[:], scalar1=7.0, scalar2=None, op0=ALU.min, reads=[eu.k], writes=[eu.k])
        WIf = sb("WIf", [128, NUN, 4], F32)
        WIi = sb("WIi", [128, NUN, 4], I32)
        for q in range(4):
            I("dve", "tensor_scalar", out=WIf[:, :, q], in0=eu[:], scalar1=512.0, scalar2=pq[:, q:q + 1], op0=ALU.mult, op1=ALU.add,
              reads=[eu.k, pq.k], writes=[WIf.k])
        I("dve", "tensor_copy", out=WIi[:], in_=WIf[:], reads=[WIf.k], writes=[WIi.k])
        if debug:
            dbg_idx = nc.dram_tensor("dbg_idx", [128, 64], F32, kind="ExternalOutput").ap()
            dbg_w = nc.dram_tensor("dbg_w", [128, 64], F32, kind="ExternalOutput").ap()
            dbg_e = nc.dram_tensor("dbg_e", [128, NUN * 5], F32, kind="ExternalOutput").ap()
            dma("sp", dbg_idx[:], IDXf[:].rearrange("p k t -> p (k t)"), [IDXf.k], ["dbg1"])
            dma("sp", dbg_w[:], WGT[:].rearrange("p k t -> p (k t)"), [WGT.k], ["dbg2"])
            dma("sp", dbg_e[:, 0:NUN], eu[:], [eu.k], ["dbg3"])
            dma("sp", dbg_e[:, NUN:NUN * 5], WIf[:].rearrange("p u q -> p (u q)"), [WIf.k], ["dbg3"])
        gfl = sb("gfl", [128, D], F32)
        dma("sp", gfl[:], MODB[2 * l + 0, :, 5 * D:6 * D], ["MODB"], [gfl.k])
        persist = A.mark()
        import concourse.bass as _b
        H2bs = [sb("H2b%d" % i, [128, 8, UNIT], BF16) for i in range(2)]
        NWS = 3
        Wset = [(sb("W1q%d" % i, [128, 8, 896], BF16), sb("W3q%d" % i, [128, 8, 896], BF16), sb("W2q%d" % i, [128, 7, D], BF16)) for i in range(NWS)]
        actq = [sb("actq%d" % i, [128, 7, UNIT], BF16) for i in range(2)]
        yacc = sb("yacc", [128, UNIT // 128, D], F32)
        slt = [sb("slt%d" % i, [128, 512], F32) for i in range(4)]
        hst = [sb("hst%d" % i, [128, D], BF16) for i in range(2)]

        def L13(s_):
            w1q, w3q, _ = Wset[s_ % NWS]
            u, q = s_ // 4, s_ % 4
            for wt, src in ((w1q, MW1q), (w3q, MW3q)):
                P.dma("pool", lambda e, wt=wt, src=src, u=u, q=q: e.indirect_dma_start(
                    out=wt[:].rearrange("p k n -> p (k n)"), out_offset=None, in_=src[:, :],
                    in_offset=_b.IndirectOffsetOnAxis(ap=WIi[:, u, q:q + 1], axis=0)), reads=["Wb", WIi.k], writes=[wt.k])

        def L2(s_):
            w2q = Wset[s_ % NWS][2]
            u, q = s_ // 4, s_ % 4
            P.dma("pool", lambda e, w2q=w2q, u=u, q=q: e.indirect_dma_start(
                out=w2q[:].rearrange("p k n -> p (k n)"), out_offset=None, in_=MW2q[:, :],
                in_offset=_b.IndirectOffsetOnAxis(ap=WIi[:, u, q:q + 1], axis=0)), reads=["Wb", WIi.k], writes=[w2q.k])

        for s_ in range(NWS):
            L13(s_)
            L2(s_)
        dbufs = A.mark()
        htl = [sb("htl%d" % i, [128, D], BF16) for i in range(3)]
        for t in range(32):
            ht_ = htl[t % 3]
            dma("sp", ht_[:], H2T[t * 128:(t + 1) * 128, :], ["H2T"], [ht_.k])
            for k_ in range(2):
                P.dma("pool", lambda e, ht_=ht_, k_=k_, t=t: e.indirect_dma_start(
                    out=HS[:, :], out_offset=_b.IndirectOffsetOnAxis(ap=IDXi[:, k_, t:t + 1], axis=0), in_=ht_[:, :], in_offset=None),
                    reads=[ht_.k, IDXi.k], writes=["HS"])
        A.reset(dbufs)
        P.barrier()
        wkeep = [t.k for ws in Wset for t in ws]
        NU = NUN * 4
        cntd = {"ps": 0, "py": 0, "h": 0}

        def prep_unit(u):
            H2b = H2bs[u % 2]
            for tt in range(UNIT // 128):
                cntd["h"] += 1
                h_ = hst[cntd["h"] % 2]
                r0 = u * UNIT + tt * 128
                dma("sp", h_[:], HS[r0:r0 + 128, :], ["HS"], [h_.k])
                pst = PS[6 + cntd["h"] % 2]
                for kc in range(8):
                    I("pe", "transpose", out=pst[:].bitcast(BF16)[:, kc * 128:(kc + 1) * 128], in_=h_[:, kc * 128:(kc + 1) * 128], identity=identb[:],
                      reads=[h_.k, identb.k], writes=[pst.k])
                I("act", "activation", out=H2b[:, :, tt * 128:(tt + 1) * 128], in_=pst[:].bitcast(BF16)[:, 0:1024].rearrange("p (k t) -> p k t", k=8),
                  func=AF.Copy, reads=[pst.k], writes=[H2b.k])

        def S1(s_):
            w1q, w3q, _ = Wset[s_ % NWS]
            aq = actq[s_ % 2]
            u, q = s_ // 4, s_ % 4
            H2b = H2bs[u % 2]
            if s_ == 0:
                prep_unit(0)
            if q == 3 and u + 1 < NUN:
                prep_unit(u + 1)
            for j in range(7):
                for p0 in range(0, UNIT, 512):
                    cntd["ps"] += 1
                    n_ = cntd["ps"]
                    pa, pb = PS[(n_ % 2) * 2], PS[(n_ % 2) * 2 + 1]
                    for kc in range(8):
                        I("pe", "matmul", pa[:], lhsT=w1q[:, kc, j * 128:(j + 1) * 128], rhs=H2b[:, kc, p0:p0 + 512], start=(kc == 0), stop=(kc == 7),
                          reads=[w1q.k, H2b.k], writes=[pa.k])
                    for kc in range(8):
                        I("pe", "matmul", pb[:], lhsT=w3q[:, kc, j * 128:(j + 1) * 128], rhs=H2b[:, kc, p0:p0 + 512], start=(kc == 0), stop=(kc == 7),
                          reads=[w3q.k, H2b.k], writes=[pb.k])
                    sl_ = slt[n_ % len(slt)]
                    I("act", "activation", out=sl_[:], in_=pa[:], func=AF.Silu, reads=[pa.k], writes=[sl_.k])
                    I("dve", "tensor_tensor", out=aq[:, j, p0:p0 + 512], in0=sl_[:], in1=pb[:], op=ALU.mult, reads=[sl_.k, pb.k], writes=[aq.k + "_%d" % j])

        def S2(s_):
            w2q = Wset[s_ % NWS][2]
            aq = actq[s_ % 2]
            u, q = s_ // 4, s_ % 4
            for tt in range(UNIT // 128):
                ts_ = slice(tt * 128, (tt + 1) * 128)
                yk0 = yacc.k + "_%d" % tt
                for half in range(2):
                    yk = yk0 + "h%d" % half
                    cs = slice(half * 512, (half + 1) * 512)
                    cntd["py"] += 1
                    py = PS[4 + cntd["py"] % 2]
                    for j in range(7):
                        I("pe", "matmul", py[:], lhsT=aq[:, j, ts_], rhs=w2q[:, j, cs], start=(j == 0), stop=(j == 6), reads=[aq.k + "_%d" % j, w2q.k], writes=[py.k])
                    if q == 0:
                        I("act", "activation", out=yacc[:, tt, cs], in_=py[:], func=AF.Copy, reads=[py.k], writes=[yk])
                    else:
                        I("dve", "tensor_tensor", out=yacc[:, tt, cs], in0=py[:], in1=yacc[:, tt, cs], op=ALU.add, reads=[py.k, yk], writes=[yk])
                if q == 3:
                    r0 = u * UNIT + tt * 128
                    I("dve", "tensor_tensor", out=yacc[:, tt, :], in0=yacc[:, tt, :], in1=gfl[:], op=ALU.mult, reads=[yk0 + "h0", yk0 + "h1", gfl.k],
                      writes=[yk0 + "h0", yk0 + "h1"])
                    dma("sp", YS[r0:r0 + 128, :], yacc[:, tt, :], [yk0 + "h0", yk0 + "h1"], ["YS"])

        for s_ in range(NU):
            if s_ >= 1 and s_ + NWS - 1 < NU:
                L13(s_ + NWS - 1)
            S1(s_)
            if s_ >= 1:
                S2(s_ - 1)
                if s_ + NWS - 1 < NU:
                    L2(s_ + NWS - 1)
        S2(NU - 1)
        A.reset(persist)
        P.barrier()
        gfin = sb("gfin", [128, D], F32)
        dma("sp", gfin[:], g_final.rearrange("(o n) -> o n", o=1).broadcast_to([128, D]), [], [gfin.k])
        g1 = [sb("g1_%d" % i, [128, D], F32) for i in range(3)]
        g2 = [sb("g2_%d" % i, [128, D], F32) for i in range(3)]
        x1s = [sb("x1_%d" % i, [128, D], F32) for i in range(3)]
        junk = sb("junk", [128, D], F32)
        ssqs = [sb("ssq%d" % i, [128, 1], F32) for i in range(2)]
        rstds = [sb("rstd%d" % i, [128, 1], F32) for i in range(2)]
        for t in range(32):
            i2 = t % 2
            r0 = NCTX + t * 128
            x1, ssq, rstd, ga, gb_ = x1s[t % 3], ssqs[i2], rstds[i2], g1[t % 3], g2[t % 3]
            for k_, gt in enumerate((ga, gb_)):
                P.dma("pool", lambda e, gt=gt, k_=k_, t=t: e.indirect_dma_start(
                    out=gt[:, :], out_offset=None, in_=YS[:, :], in_offset=_b.IndirectOffsetOnAxis(ap=IDXi[:, k_, t:t + 1], axis=0)),
                    reads=["YS", IDXi.k], writes=[gt.k])
            dma("sp", x1[:], X1[r0:r0 + 128, :], ["X1"], [x1.k])
            I("act", "activation", out=ga[:], in_=ga[:], func=AF.Identity, scale=WGT[:, 0, t:t + 1], reads=[ga.k, WGT.k], writes=[ga.k])
            I("dve", "scalar_tensor_tensor", out=ga[:], in0=gb_[:], scalar=WGT[:, 1, t:t + 1], in1=ga[:], op0=ALU.mult, op1=ALU.add,
              reads=[gb_.k, WGT.k, ga.k], writes=[ga.k])
            I("dve", "tensor_tensor", out=x1[:], in0=x1[:], in1=ga[:], op=ALU.add, reads=[x1.k, ga.k], writes=[x1.k])
            I("act", "activation", out=junk[:], in_=x1[:], func=AF.Square, accum_out=ssq[:], reads=[x1.k], writes=[junk.k, ssq.k])
            sqrt_recip(rstd, ssq, 1.0 / D)
            I("dve", "scalar_tensor_tensor", out=x1[:], in0=x1[:], scalar=rstd[:], in1=gfin[:], op0=ALU.mult, op1=ALU.mult,
              reads=[x1.k, rstd.k, gfin.k], writes=[x1.k])
            dma("sp", out[r0 - NCTX:r0 - NCTX + 128, :], x1[:], [x1.k], ["OUT"])

    try:
      for l in range(n_layers):
        last = l == n_layers - 1 and n_layers == 2
        xsrc = xin if l == 0 else XS
        A.reset(phase_base)
        P.barrier()
        Win = sb("Win", [128, 8, 4096], BF16)
        for kc in range(8):
            for hh in range(2):
                cdma(Win[:, kc, hh * 2048:(hh + 1) * 2048], w_in[l, kc * 128:(kc + 1) * 128, hh * 2048:(hh + 1) * 2048], [], [Win.k])
        if l == 0:
            precast_some(n_ffn_pre)
        win_base = A.mark()
        cc_t = sb("cc", [128, 2, 8], F32)
        dma("sp", cc_t[:, 0, :], c_b.rearrange("(k p) -> p k", p=128), [], [cc_t.k])
        dma("sp", cc_t[:, 1, :], c_ctx.rearrange("(k p) -> p k", p=128), [], [cc_t.k])
        sc_t = sb("sc", [128, 2, 8], F32)
        I("act", "activation", out=sc_t[:], in_=cc_t[:], func=AF.Silu, reads=[cc_t.k], writes=[sc_t.k])
        rep = sb("rep", [128, 2, 8, 128], F32)
        for r in range(2):
            for kc in range(8):
                I("dve", "tensor_scalar", out=rep[:, r, kc, :], in0=ones[:], scalar1=sc_t[:, r, kc:kc + 1],
                                                                scalar2=None, op0=ALU.mult,
                     reads=[ones.k, sc_t.k], writes=[rep.k])
        bm = sb("bm", [128, 6 * D], F32)
        dma("sp", bm[:], b_mod[l:l + 1, :].broadcast_to([128, 6 * D]), [], [bm.k])
        gmx = sb("gmx", [128, D], F32)
        gff = sb("gff", [128, D], F32)
        dma("sp", gmx[:], g_mix[l:l + 1, :].broadcast_to([128, D]), [], [gmx.k])
        dma("sp", gff[:], g_ffn[l:l + 1, :].broadcast_to([128, D]), [], [gff.k])
        modt = [sb("modt%d" % r, [128, 6 * D], F32) for r in range(2)]
        wms = [sb("wm%d" % i, [128, 8, 512], F32) for i in range(2)]
        for cc in range(12):
            wm = wms[cc % 2]
            dma("sp", wm[:], w_mod[l, :, cc * 512:(cc + 1) * 512].rearrange("(k p) n -> p k n", p=128), [], [wm.k])
            for r in range(2):
                ps = PS[(cc * 2 + r) % 4]
                for kc in range(8):
                    I("pe", "matmul", ps[:], lhsT=rep[:, r, kc, :], rhs=wm[:, kc, :],
                                                                           start=(kc == 0), stop=(kc == 7),
                         reads=[rep.k, wm.k], writes=[ps.k])
                I("dve", "tensor_tensor", out=modt[r][:, cc * 512:(cc + 1) * 512], in0=ps[:],
                                                                       in1=bm[:, cc * 512:(cc + 1) * 512], op=ALU.add,
                     reads=[ps.k, bm.k], writes=[modt[r].k])
        for r in range(2):
            I("dve", "scalar_tensor_tensor", out=modt[r][:, D:2 * D], in0=modt[r][:, D:2 * D], scalar=1.0, in1=gmx[:],
                                                             op0=ALU.add, op1=ALU.mult,
                 reads=[modt[r].k, gmx.k], writes=[modt[r].k])
            I("dve", "scalar_tensor_tensor", out=modt[r][:, 4 * D:5 * D], in0=modt[r][:, 4 * D:5 * D], scalar=1.0,
                                                             in1=gff[:], op0=ALU.add, op1=ALU.mult,
                 reads=[modt[r].k, gff.k], writes=[modt[r].k])
            dma("sp", MODB[2 * l + r], modt[r][:], [modt[r].k], ["MODB"])

        def load_mod(slot, name):
            ts = []
            for r in range(2):
                t = sb("%s%d" % (name, r), [128, D], F32)
                dma("sp", t[:], MODB[2 * l + r, :, slot * D:(slot + 1) * D], ["MODB"], [t.k])
                ts.append(t)
            return ts

        def modsel(ts, g0):
            return ts[1] if g0 < NCTX else ts[0]

        chk(l, "M")
        A.reset(win_base)
        P.barrier()
        P.lastw[Win.k] = None
        if l == 1:
            HSv = HS.rearrange("(p r) n -> p (r n)", p=128)
            for zi in range(NROWS // 128 * D // 4096):
                dma("sp", HSv[:, zi * 4096:(zi + 1) * 4096], k_zero[:, :], [], ["HS"])
        G1 = load_mod(1, "G1")
        SH1 = load_mod(0, "SH1")
        for (c0, w) in ((0, 2), (258, 3), (UW - 3, 3)):
            for ch in range(4):
                dma("sp", U[ch * 128:(ch + 1) * 128, c0:c0 + w], zeros_bf[:, 0:w], [zeros_bf.k], ["U"])
        xts = [sb("xt%d" % i, [128, D], F32) for i in range(8)]
        junk = sb("junk", [128, D], F32)
        t1s = [sb("t1_%d" % i, [128, D], F32) for i in range(2)]
        hts = [sb("ht%d" % i, [128, D], BF16) for i in range(2)]
        ssqs = [sb("ssq%d" % i, [128, 1], F32) for i in range(2)]
        rstds = [sb("rstd%d" % i, [128, 1], F32) for i in range(2)]
        hfms = [sb("hfm%d" % i, [128, 8, 512], BF16) for i in range(2)]
        cos_t = [sb("cos%d" % i, [128, 512], F32) for i in range(2)]
        sin_t = [sb("sin%d" % i, [128, 512], F32) for i in range(2)]
        ust = [sb("ust%d" % i, [128, 512], BF16) for i in range(6)]
        qks = [sb("qks%d" % i, [128, 4, 512], BF16) for i in range(2)]
        ra = [sb("ra%d" % i, [128, 512], F32) for i in range(2)]
        rb = [sb("rb%d" % i, [128, 512], F32) for i in range(2)]
        tok_st = [sb("tokst%d" % i, [128, 512], BF16) for i in range(6)]
        cnt = {"x": 0, "u": 0, "r": 0, "ps": 0, "tk": 0}

        def a_loads(mi):
            g0, T_ = MACROS[mi]
            ct, stb = cos_t[mi % 2], sin_t[mi % 2]
            for tt in range(T_ // 128):
                xt = xts[(mi % 2) * 4 + tt]
                r0 = g0 + tt * 128
                precast_some(4)
                dma("sp", xt[:], xsrc[r0:r0 + 128, :], ["XS"], [xt.k])
            dma("sp", ct[:, 0:T_], k_cos[:, g0:g0 + T_], [], [ct.k])
            dma("sp", stb[:, 0:T_], k_sin[:, g0:g0 + T_], [], [stb.k])

        def a_chain(mi, tt):
            g0, T_ = MACROS[mi]
            g1, sh1 = modsel(G1, g0), modsel(SH1, g0)
            i = (mi * 4 + tt) % 2
            xt = xts[(mi % 2) * 4 + tt]
            t1, ht, ssq, rstd = t1s[i], hts[i], ssqs[i], rstds[i]
            I("act", "activation", out=junk[:], in_=xt[:], func=AF.Square, accum_out=ssq[:], reads=[xt.k], writes=[junk.k, ssq.k])
            sqrt_recip(rstd, ssq, 1.0 / D)
            I("dve", "scalar_tensor_tensor", out=t1[:], in0=xt[:], scalar=rstd[:], in1=g1[:], op0=ALU.mult, op1=ALU.mult,
              reads=[xt.k, rstd.k, g1.k], writes=[t1.k])
            I("dve", "tensor_tensor", out=ht[:], in0=t1[:], in1=sh1[:], op=ALU.add, reads=[t1.k, sh1.k], writes=[ht.k])

        def a_xpose(mi, tt):
            i = (mi * 4 + tt) % 2
            ht = hts[i]
            hfm = hfms[mi % 2]
            pst = PS[6 + i]
            for kc in range(8):
                I("pe", "transpose", out=pst[:].bitcast(BF16)[:, kc * 128:(kc + 1) * 128], in_=ht[:, kc * 128:(kc + 1) * 128], identity=identb[:],
                  reads=[ht.k, identb.k], writes=[pst.k])
            I("act", "activation", out=hfm[:, :, tt * 128:(tt + 1) * 128], in_=pst[:].bitcast(BF16)[:, 0:1024].rearrange("p (k t) -> p k t", k=8),
              func=AF.Copy, reads=[pst.k], writes=[hfm.k])

        def nps():
            cnt["ps"] += 1
            return PS[cnt["ps"] % 4]

        def a_section(mi, sec):
            g0, T_ = MACROS[mi]
            hfm = hfms[mi % 2]
            ct, stb = cos_t[mi % 2], sin_t[mi % 2]

            def proj_fm(oc, ps):
                for kc in range(8):
                    I("pe", "matmul", ps[:, 0:T_], lhsT=Win[:, kc, oc * 128:(oc + 1) * 128], rhs=hfm[:, kc, 0:T_], start=(kc == 0), stop=(kc == 7),
                      reads=[Win.k, hfm.k], writes=[ps.k])

            if sec == 0:
                for oc in range(8):
                    ps = nps()
                    proj_fm(oc, ps)
                    u_ = ust[cnt["u"] % 6]
                    cnt["u"] += 1
                    fn = AF.Copy if oc < 4 else AF.Gelu
                    I("act", "activation", out=u_[:, 0:T_], in_=ps[:, 0:T_], func=fn, reads=[ps.k], writes=[u_.k])
                    if oc < 4:
                        dma("sp", U[oc * 128:(oc + 1) * 128, ucol(g0):ucol(g0) + T_], u_[:, 0:T_], [u_.k], ["U"])
                    else:
                        dma("sp", GY[(oc - 4) * 128:(oc - 3) * 128, g0:g0 + T_], u_[:, 0:T_], [u_.k], ["GY"])
            elif sec in (1, 2):
                qk = sec - 1
                st = qks[qk]
                for h in range(4):
                    psq = nps()
                    proj_fm(8 + qk * 4 + h, psq)
                    pss = nps()
                    proj_fm(24 + qk * 4 + h, pss)
                    a_, b_ = ra[cnt["r"] % 2], rb[cnt["r"] % 2]
                    cnt["r"] += 1
                    sc = 1.0 if qk == 0 else 128.0 ** -0.5
                    I("dve", "scalar_tensor_tensor", out=a_[:, 0:T_], in0=psq[:, 0:T_], scalar=sc, in1=ct[:, 0:T_], op0=ALU.mult, op1=ALU.mult,
                      reads=[psq.k, ct.k], writes=[a_.k])
                    I("dve", "scalar_tensor_tensor", out=b_[:, 0:T_], in0=pss[:, 0:T_], scalar=sc, in1=stb[:, 0:T_], op0=ALU.mult, op1=ALU.mult,
                      reads=[pss.k, stb.k], writes=[b_.k])
                    I("dve", "tensor_tensor", out=st[:, h, 0:T_], in0=a_[:, 0:T_], in1=b_[:, 0:T_], op=ALU.add, reads=[a_.k, b_.k], writes=[st.k + "_%d" % h])
                dst = Q if qk == 0 else K
                dma("sp", dst[:, g0:g0 + T_].rearrange("(h d) t -> d h t", d=128), st[:, :, 0:T_], [st.k + "_%d" % h_ for h_ in range(4)], ["Q" if qk == 0 else "K"])
            else:
                kst = qks[1]
                for tt in range(T_ // 128):
                    r0 = g0 + tt * 128
                    pst = PS[4]
                    for h in range(4):
                        I("pe", "transpose", out=pst[:].bitcast(BF16)[:, h * 128:(h + 1) * 128], in_=kst[:, h, tt * 128:(tt + 1) * 128], identity=identb[:],
                          reads=[kst.k + "_%d" % h, identb.k], writes=[pst.k])
                    tk = tok_st[cnt["tk"] % 6]
                    cnt["tk"] += 1
                    I("dve", "tensor_copy", out=tk[:], in_=pst[:].bitcast(BF16)[:, 0:512], reads=[pst.k], writes=[tk.k])
                    dma("sp", KT[r0:r0 + 128, :], tk[:], [tk.k], ["KT"])
                    for which in range(2):
                        ps = PS[5] if which == 0 else nps()
                        c0 = 2048 + which * 512
                        for kc in range(8):
                            I("pe", "matmul", ps[:], lhsT=hfm[:, kc, tt * 128:(tt + 1) * 128], rhs=Win[:, kc, c0:c0 + 512], start=(kc == 0), stop=(kc == 7),
                              reads=[hfm.k, Win.k], writes=[ps.k])
                        tk = tok_st[cnt["tk"] % 6]
                        cnt["tk"] += 1
                        fn = AF.Copy if which == 0 else AF.Silu
                        I("act", "activation", out=tk[:], in_=ps[:], func=fn, reads=[ps.k], writes=[tk.k])
                        dma("sp", (VT if which == 0 else SGT)[r0:r0 + 128, :], tk[:], [tk.k], ["VT" if which == 0 else "SGT"])

        NM = len(MACROS)
        a_loads(0)
        a_loads(1)
        for tt in range(MACROS[0][1] // 128):
            a_chain(0, tt)
            a_xpose(0, tt)
        for mi in range(NM):
            nxt = mi + 1 if mi + 1 < NM else None
            ntn = (MACROS[nxt][1] // 128) if nxt is not None else 0
            for sec in range(4):
                if nxt is not None and sec < ntn:
                    a_chain(nxt, sec)
                a_section(mi, sec)
                if nxt is not None and sec < ntn:
                    a_xpose(nxt, sec)
            if mi + 2 < NM:
                a_loads(mi + 2)

        A.reset(phase_base)
        P.barrier()
        tab = sb("tab", [128, 8, 128], F32)
        dma("sp", tab[:], k_tab[:], [], [tab.k])
        rd = sb("rd", [128, 8], F32)
        dma("sp", rd[:], ret_decay[l:l + 1, :].broadcast_to([128, 8]), [], [rd.k])
        lg = sb("lg", [128, 8], F32)
        I("act", "activation", out=lg[:], in_=rd[:], func=AF.Exp, scale=-1.0, reads=[rd.k], writes=[lg.k])
        I("act", "activation", out=lg[:], in_=lg[:], func=AF.Ln, bias=ones[:, 0:1], reads=[lg.k, ones.k], writes=[lg.k])
        I("dve", "tensor_scalar", out=lg[:], in0=lg[:], scalar1=-1.0, scalar2=None, op0=ALU.mult, reads=[lg.k], writes=[lg.k])
        kd = sb("kd", [128, 8], F32)
        cd = sb("cd", [128, 8], F32)
        for dr in range(2):
            for h in range(4):
                j = dr * 4 + h
                I("act", "activation", out=kd[:, j:j + 1], in_=tab[:, 6 + dr, 0:1], func=AF.Exp, scale=lg[:, j:j + 1],
                     reads=[tab.k, lg.k], writes=[kd.k])
        I("act", "activation", out=cd[:], in_=lg[:], func=AF.Exp, scale=128.0, reads=[lg.k], writes=[cd.k])
        KDT = [sb("KDT%d" % i, [128, 512], F32) for i in range(2)]
        for dr in range(2):
            for h in range(4):
                I("dve", "tensor_scalar", out=KDT[dr][:, h * 128:(h + 1) * 128], in0=ones[:], scalar1=kd[:, dr * 4 + h:dr * 4 + h + 1], scalar2=None,
                  op0=ALU.mult, reads=[ones.k, kd.k], writes=[KDT[dr].k])
        Sd = [sb("S%d" % i, [128, 512], F32) for i in range(2)]
        Sb16 = [sb("Sb16_%d" % i, [128, 512], BF16) for i in range(4)]
        ktl = [sb("ktl%d" % i, [128, 512], BF16) for i in range(8)]
        vtl = [sb("vtl%d" % i, [128, 512], BF16) for i in range(8)]
        kts = [sb("kts%d" % i, [128, 512], BF16) for i in range(4)]
        orders = [list(range(NCH)), [1, 0] + list(range(NCH - 1, 1, -1))]
        for dr in range(2):
            I("dve", "memset", Sd[dr][:], 0.0, writes=[Sd[dr].k + "_%d" % h_ for h_ in range(4)])

        def a2_loads(step):
            for dr in range(2):
                c = orders[dr][step]
                i8 = (step % 4) * 2 + dr
                dma("sp", ktl[i8][:], KT[c * 128:(c + 1) * 128, :], ["KT"], [ktl[i8].k])
                dma("sp", vtl[i8][:], VT[c * 128:(c + 1) * 128, :], ["VT"], [vtl[i8].k])

        for step in range(3):
            a2_loads(step)
        a2n = [0, 0]

        def a2_step(step):
            if step + 3 < NCH:
                a2_loads(step + 3)
            for dr in range(2):
                n = a2n[0] = a2n[0] + 1
                c = orders[dr][step]
                S = Sd[dr]
                dst = SF if dr == 0 else SB
                i = (step % 2) * 2 + dr
                i8 = (step % 4) * 2 + dr
                s16, kt_, vt_, ks_ = Sb16[i], ktl[i8], vtl[i8], kts[i]
                I("act", "activation", out=s16[:], in_=S[:], func=AF.Copy, reads=[S.k + "_%d" % h_ for h_ in range(4)], writes=[s16.k])
                dma("sp", dst[c], s16[:], [s16.k], ["SF" if dr == 0 else "SB"])
                ps = PS[6 + n % 2]
                I("dve", "tensor_tensor", out=ks_[:], in0=kt_[:], in1=KDT[dr][:], op=ALU.mult, reads=[kt_.k, KDT[dr].k], writes=[ks_.k])
                for h in range(4):
                    I("pe", "matmul", ps[:, h * 128:(h + 1) * 128], lhsT=ks_[:, h * 128:(h + 1) * 128], rhs=vt_[:, h * 128:(h + 1) * 128],
                      start=True, stop=True, reads=[ks_.k, vt_.k], writes=[ps.k])
                for h in range(4):
                    j = dr * 4 + h
                    I("dve", "scalar_tensor_tensor", out=S[:, h * 128:(h + 1) * 128], in0=S[:, h * 128:(h + 1) * 128], scalar=cd[:, j:j + 1],
                      in1=ps[:, h * 128:(h + 1) * 128], op0=ALU.mult, op1=ALU.add, reads=[S.k + "_%d" % h, cd.k, ps.k], writes=[S.k + "_%d" % h])

        def a2_advance(k):
            while k > 0 and a2n[1] < NCH:
                a2_step(a2n[1])
                a2n[1] += 1
                k -= 1

        cw = sb("cw", [128, 4, 4], F32)
        dma("sp", cw[:], conv_w[l].rearrange("k (c p) -> p k c", p=128), [], [cw.k])
        cb = sb("cb", [128, 4], F32)
        dma("sp", cb[:], conv_b[l].rearrange("(c p) -> p c", p=128), [], [cb.k])
        gb = sb("gb", [128, 3, 2, 4], F32)
        for wi, src in enumerate((rg_ba, rg_bx, rg_lam)):
            dma("sp", gb[:, wi], src[l].rearrange("d (c p) -> p d c", p=128), [], [gb.k])
        cv = sb("cv", [128, 2, 4], F32)
        I("act", "activation", out=cv[:], in_=gb[:, 2], func=AF.Exp, scale=-1.0, reads=[gb.k], writes=[cv.k])
        I("act", "activation", out=cv[:], in_=cv[:], func=AF.Ln, bias=ones[:, 0:1], reads=[cv.k, ones.k], writes=[cv.k])
        I("dve", "tensor_scalar", out=cv[:], in0=cv[:], scalar1=-8.0, scalar2=None, op0=ALU.mult, reads=[cv.k], writes=[cv.k])
        Up = sb("Up", [128, UW], BF16)
        gyt = sb("gyt", [128, NT], BF16)
        uc32 = sb("uc32", [128, NT], F32)
        ucb = sb("ucb", [128, NT], BF16)
        AB = [[sb("A%d" % d_, [128, NT], F32), sb("B%d" % d_, [128, NT], F32)] for d_ in range(2)]
        Hd = [sb("H%d" % d_, [128, NT], F32) for d_ in range(2)]
        dg = [sb("dg%d" % k_, [128, 128], BF16) for k_ in range(4)]
        bd = [[sb("bd%d%d" % (d_, w_), [128, 128], BF16) for w_ in range(2)] for d_ in range(2)]
        rgs = [sb("rgs%d" % i, [128, 512], BF16) for i in range(2)]
        n = 0
        for c in range(4):
            dma("sp", Up[:], U[c * 128:(c + 1) * 128, :], ["U"], [Up.k])
            dma("sp", gyt[:], GY[c * 128:(c + 1) * 128, :], ["GY"], [gyt.k])
            for k_ in range(4):
                I("dve", "tensor_scalar", out=dg[k_][:], in0=identf[:], scalar1=cw[:, k_, c:c + 1], scalar2=None, op0=ALU.mult,
                     reads=[identf.k, cw.k], writes=[dg[k_].k])
            for d_ in range(2):
                for w_, src in enumerate((rg_wa, rg_wx)):
                    t_ = bd[d_][w_]
                    I("pool", "memset", t_[:], 0.0, writes=[t_.k])
                    for hb in range(2):
                        cdma(t_[hb * 64:(hb + 1) * 64, hb * 64:(hb + 1) * 64], src[l, d_, 2 * c + hb], [], [t_.k])
            for (g0, T_) in MACROS:
                n += 1
                if n % 2 == 0:
                    a2_advance(1)
                ps = PS[n % 2]
                for k_ in range(4):
                    c0 = ucol(g0) + k_ - 2
                    I("pe", "matmul", ps[:, 0:T_], lhsT=dg[k_][:], rhs=Up[:, c0:c0 + T_], start=(k_ == 0), stop=(k_ == 3),
                      reads=[dg[k_].k, Up.k], writes=[ps.k])
                I("act", "activation", out=uc32[:, g0:g0 + T_], in_=ps[:, 0:T_], func=AF.Identity, bias=cb[:, c:c + 1],
                  reads=[ps.k, cb.k], writes=[uc32.k + "_%d" % g0])
                I("dve", "tensor_copy", out=ucb[:, g0:g0 + T_], in_=uc32[:, g0:g0 + T_], reads=[uc32.k + "_%d" % g0], writes=[ucb.k + "_%d" % g0])
            AK = [[AB[d_][0].k + "_%d" % g0 for (g0, T_) in MACROS] for d_ in range(2)]
            BK = [[AB[d_][1].k + "_%d" % g0 for (g0, T_) in MACROS] for d_ in range(2)]
            for mi_, (g0, T_) in enumerate(MACROS):
                a2_advance(1)
                for d_ in range(2):
                    n += 1
                    pr, pi = PS[2 + (n % 2) * 2], PS[3 + (n % 2) * 2]
                    Aa, Bb = AB[d_]
                    I("pe", "matmul", pr[:, 0:T_], lhsT=bd[d_][0][:], rhs=ucb[:, g0:g0 + T_], start=True, stop=True,
                      reads=[bd[d_][0].k, ucb.k + "_%d" % g0], writes=[pr.k])
                    I("pe", "matmul", pi[:, 0:T_], lhsT=bd[d_][1][:], rhs=ucb[:, g0:g0 + T_], start=True, stop=True,
                      reads=[bd[d_][1].k, ucb.k + "_%d" % g0], writes=[pi.k])
                    I("act", "activation", out=Aa[:, g0:g0 + T_], in_=pr[:, 0:T_], func=AF.Sigmoid, bias=gb[:, 0, d_, c:c + 1],
                      reads=[pr.k, gb.k], writes=[AK[d_][mi_]])
                    I("act", "activation", out=Bb[:, g0:g0 + T_], in_=pi[:, 0:T_], func=AF.Sigmoid, bias=gb[:, 1, d_, c:c + 1],
                      reads=[pi.k, gb.k], writes=[BK[d_][mi_]])
            uall = [uc32.k + "_%d" % g0 for (g0, T_) in MACROS]
            for d_ in range(2):
                Aa, Bb = AB[d_]
                I("act", "activation", out=Aa[:], in_=Aa[:], func=AF.Exp, scale=cv[:, d_, c:c + 1], reads=AK[d_] + [cv.k], writes=AK[d_])
            for d_ in range(2):
                Aa, Bb = AB[d_]
                I("act", "activation", out=Hd[d_][:], in_=Aa[:], func=AF.Square, reads=AK[d_], writes=[Hd[d_].k])
                I("dve", "tensor_tensor", out=Bb[:], in0=Bb[:], in1=uc32[:], op=ALU.mult, reads=BK[d_] + uall, writes=BK[d_])
            for d_ in range(2):
                I("act", "activation", out=Hd[d_][:], in_=Hd[d_][:], func=AF.Sqrt, scale=-1.0, bias=ones[:, 0:1],
                  reads=[Hd[d_].k, ones.k], writes=[Hd[d_].k])
            for d_ in range(2):
                Aa, Bb = AB[d_]
                I("dve", "tensor_tensor", out=Bb[:], in0=Bb[:], in1=Hd[d_][:], op=ALU.mult, reads=BK[d_] + [Hd[d_].k], writes=BK[d_])
            Aa, Bb = AB[0]
            I("dve", "tensor_tensor_scan", out=Hd[0][:, 0:NCTX], data0=Aa[:, 0:NCTX], data1=Bb[:, 0:NCTX], initial=0.0,
                                                                  op0=ALU.mult, op1=ALU.add, reads=AK[0] + BK[0], writes=[Hd[0].k])
            I("dve", "tensor_tensor_scan", out=Hd[0][:, NCTX:NT], data0=Aa[:, NCTX:NT], data1=Bb[:, NCTX:NT],
                                                                  initial=Hd[0][:, NCTX - 1:NCTX], op0=ALU.mult, op1=ALU.add,
                 reads=AK[0] + BK[0] + [Hd[0].k], writes=[Hd[0].k])
            Aa, Bb = AB[1]
            I("dve", "tensor_tensor_scan", out=Hd[1][:, NCTX - 1::-1], data0=Aa[:, NCTX - 1::-1], data1=Bb[:, NCTX - 1::-1],
                                                                  initial=0.0, op0=ALU.mult, op1=ALU.add, reads=AK[1] + BK[1], writes=[Hd[1].k])
            I("dve", "tensor_tensor_scan", out=Hd[1][:, NT - 1:NCTX - 1:-1], data0=Aa[:, NT - 1:NCTX - 1:-1],
                                                                  data1=Bb[:, NT - 1:NCTX - 1:-1], initial=Hd[1][:, 0:1], op0=ALU.mult, op1=ALU.add,
                 reads=AK[1] + BK[1] + [Hd[1].k], writes=[Hd[1].k])
            I("dve", "tensor_tensor", out=Hd[0][:], in0=Hd[0][:], in1=Hd[1][:], op=ALU.add, reads=[Hd[0].k, Hd[1].k], writes=[Hd[0].k])
            for (g0, T_) in MACROS:
                n += 1
                rg_ = rgs[n % 2]
                I("dve", "tensor_tensor", out=rg_[:, 0:T_], in0=Hd[0][:, g0:g0 + T_], in1=gyt[:, g0:g0 + T_], op=ALU.mult,
                  reads=[Hd[0].k, gyt.k], writes=[rg_.k])
                dma("sp", YRG[c * 128:(c + 1) * 128, g0:g0 + T_], rg_[:, 0:T_], [rg_.k], ["YRG"])

        a2_advance(NCH)
        A.reset(phase_base)
        P.barrier()
        Wout = sb("Wout", [128, 8, D], BF16)
        for kc in range(8):
            cdma(Wout[:, kc, :], w_out[l, kc * 128:(kc + 1) * 128, :], [], [Wout.k])
        tab = sb("tab", [128, 8, 128], F32)
        dma("sp", tab[:], k_tab[:], [], [tab.k])
        rd = sb("rd", [128, 8], F32)
        dma("sp", rd[:], ret_decay[l:l + 1, :].broadcast_to([128, 8]), [], [rd.k])
        lg = sb("lg", [128, 8], F32)
        I("act", "activation", out=lg[:], in_=rd[:], func=AF.Exp, scale=-1.0, reads=[rd.k], writes=[lg.k])
        I("act", "activation", out=lg[:], in_=lg[:], func=AF.Ln, bias=ones[:, 0:1], reads=[lg.k, ones.k], writes=[lg.k])
        I("dve", "tensor_scalar", out=lg[:], in0=lg[:], scalar1=-1.0, scalar2=None, op0=ALU.mult, reads=[lg.k], writes=[lg.k])
        maskT = sb("maskT", [128, 4, 128], F32)
        mtmp = sb("mtmp", [128, 128], F32)
        QFt = sb("QFt", [128, 4, 128], F32)
        QBt = sb("QBt", [128, 4, 128], F32)
        for h in range(4):
            I("act", "activation", out=maskT[:, h, :], in_=tab[:, 0, :], func=AF.Exp, scale=lg[:, h:h + 1],
                 reads=[tab.k, lg.k], writes=[maskT.k])
            I("dve", "tensor_tensor", out=maskT[:, h, :], in0=maskT[:, h, :], in1=tab[:, 1, :], op=ALU.mult,
                 reads=[maskT.k, tab.k], writes=[maskT.k])
            I("act", "activation", out=mtmp[:], in_=tab[:, 2, :], func=AF.Exp, scale=lg[:, 4 + h:5 + h],
                 reads=[tab.k, lg.k], writes=[mtmp.k])
            I("dve", "tensor_tensor", out=mtmp[:], in0=mtmp[:], in1=tab[:, 3, :], op=ALU.mult, reads=[mtmp.k, tab.k], writes=[mtmp.k])
            I("dve", "tensor_tensor", out=maskT[:, h, :], in0=maskT[:, h, :], in1=mtmp[:], op=ALU.add,
                 reads=[maskT.k, mtmp.k], writes=[maskT.k])
            I("act", "activation", out=QFt[:, h, :], in_=tab[:, 4, :], func=AF.Exp, scale=lg[:, h:h + 1],
                 reads=[tab.k, lg.k], writes=[QFt.k])
            I("act", "activation", out=QBt[:, h, :], in_=tab[:, 5, :], func=AF.Exp, scale=lg[:, 4 + h:5 + h],
                 reads=[tab.k, lg.k], writes=[QBt.k])
        GM = load_mod(2, "GM")
        G2 = load_mod(4, "G2")
        SH2 = load_mod(3, "SH2")
        moe_layer = (l == 1)
        if moe_layer:
            wr = sb("wr", [128, 8, 128], F32)
            I("dve", "memset", wr[:], 0.0, writes=[wr.k])
            dma("sp", wr[:, :, 0:NEXP], moe_router.rearrange("(k p) n -> p k n", p=128), [], [wr.k])
            brt = sb("brt", [128, NEXP], F32)
            dma("sp", brt[:], moe_router_b.rearrange("(o n) -> o n", o=1).broadcast_to([128, NEXP]), [], [brt.k])
        Qm = [sb("Qm%d" % i, [128, 4, 512], BF16) for i in range(2)]
        Km = [sb("Km%d" % i, [128, 4, 512], BF16) for i in range(2)]
        Qf = [sb("Qf%d" % i, [128, 4, 512], BF16) for i in range(2)]
        Qb = [sb("Qb%d" % i, [128, 4, 512], BF16) for i in range(2)]
        Yrg = [sb("Yrg%d" % i, [128, 4, 512], BF16) for i in range(2)]
        Yret = [sb("Yret%d" % i, [128, 4, 512], BF16) for i in range(2)]
        H2st = [sb("H2st%d" % i, [128, 8, 512], BF16) for i in range(2)] if not moe_layer else [None, None]
        stm = [sb("stm%d" % i, [128, 512], BF16) for i in range(2)]
        xc = [sb("xc%d" % i, [128, 512], F32) for i in range(2)]
        rett = [sb("rett%d" % i, [128, 512], BF16) for i in range(2)]
        small = [sb("small%d" % i, [128, 16], F32) for i in range(2)]
        junk2 = sb("junk2", [128, 128], F32)
        t2s = [sb("t2_%d" % i, [128, D], F32) for i in range(2)]
        junk = sb("junk", [128, D], F32)
        ssqs = [sb("ssq%d" % i, [128, 1], F32) for i in range(2)]
        rstds = [sb("rstd%d" % i, [128, 1], F32) for i in range(2)]
        h2f = [sb("h2f%d" % i, [128, 8, 128], F32) for i in range(2)] if moe_layer else None
        rt = [sb("rt%d" % i, [128, 6, NEXP], F32) for i in range(2)]
        rsm = [sb("rsm%d" % i, [128, 8], F32) for i in range(2)]
        h2tb = [sb("h2tb%d" % i, [128, D], BF16) for i in range(2)]
        cl3 = [[sb("cl3_%d_%d" % (i, j), [128, 512], BF16) for j in range(4)] for i in range(3)]
        xts4 = [sb("xt4_%d" % i, [128, D], F32) for i in range(8)]
        x1s4 = [sb("x1q_%d" % i, [128, D], F32) for i in range(4)]
        chunks = [(mi, ci) for mi, (g0, T_) in enumerate(MACROS) for ci in range(T_ // 128)]

        def c_macro_loads(mi):
            g0, T_ = MACROS[mi]
            dma("sp", Qm[mi % 2][:, :, 0:T_], Q[:, g0:g0 + T_].rearrange("(h d) t -> d h t", d=128), ["Q"], [Qm[mi % 2].k])
            dma("sp", Km[mi % 2][:, :, 0:T_], K[:, g0:g0 + T_].rearrange("(h d) t -> d h t", d=128), ["K"], [Km[mi % 2].k])
            dma("sp", Yrg[mi % 2][:, :, 0:T_], YRG[:, g0:g0 + T_].rearrange("(c p) t -> p c t", p=128), ["YRG"], [Yrg[mi % 2].k])

        def c_chunk_loads(n_):
            mi, ci = chunks[n_]
            gc = MACROS[mi][0] // 128 + ci
            vt_, sg_, sf_, sb_ = cl3[n_ % 3]
            dma("sp", vt_[:], VT[gc * 128:(gc + 1) * 128, :], ["VT"], [vt_.k])
            dma("sp", sg_[:], SGT[gc * 128:(gc + 1) * 128, :], ["SGT"], [sg_.k])
            dma("sp", sf_[:], SF[gc], ["SF"], [sf_.k])
            dma("sp", sb_[:], SB[gc], ["SB"], [sb_.k])
            if not (MACROS[mi][0] < NCTX and last):
                r0 = gc * 128
                dma("sp", xts4[n_ % 8][:], xsrc[r0:r0 + 128, :], ["XS"], [xts4[n_ % 8].k])

        c_macro_loads(0)
        c_macro_loads(1)
        c_chunk_loads(0)
        c_chunk_loads(1)
        cnt_c = [0]
        ntl = [0]
        for mi, (g0, T_) in enumerate(MACROS):
            is_ctx = g0 < NCTX
            qm, km, qf, qb, yrg, yret, h2st = Qm[mi % 2], Km[mi % 2], Qf[mi % 2], Qb[mi % 2], Yrg[mi % 2], Yret[mi % 2], H2st[mi % 2]
            for ci in range(T_ // 128):
                sl = slice(ci * 128, (ci + 1) * 128)
                I("dve", "tensor_tensor", out=qf[:, :, sl], in0=qm[:, :, sl], in1=QFt[:], op=ALU.mult, reads=[qm.k, QFt.k], writes=[qf.k + "_%d" % ci])
                I("dve", "tensor_tensor", out=qb[:, :, sl], in0=qm[:, :, sl], in1=QBt[:], op=ALU.mult, reads=[qm.k, QBt.k], writes=[qb.k + "_%d" % ci])
            cstate = {}

            def c_front(ci):
                nonlocal_n = cnt_c
                sl = slice(ci * 128, (ci + 1) * 128)
                if nonlocal_n[0] + 2 < len(chunks):
                    c_chunk_loads(nonlocal_n[0] + 2)
                vt_, sg_, sf_, sb_ = cl3[nonlocal_n[0] % 3]
                nonlocal_n[0] += 1
                i2 = nonlocal_n[0] % 2
                pst, pso = PS[i2], PS[2 + i2]
                for h in range(4):
                    I("pe", "matmul", pst[:, h * 128:(h + 1) * 128], lhsT=km[:, h, sl], rhs=qm[:, h, sl], start=True, stop=True,
                      reads=[km.k, qm.k], writes=[pst.k])
                sm_ = stm[i2]
                I("dve", "tensor_tensor", out=sm_[:], in0=pst[:], in1=maskT[:].rearrange("p h i -> p (h i)"), op=ALU.mult,
                  reads=[pst.k, maskT.k], writes=[sm_.k])
                for h in range(4):
                    hs = slice(h * 128, (h + 1) * 128)
                    I("pe", "matmul", pso[:, hs], lhsT=sm_[:, hs], rhs=vt_[:, hs], start=True, stop=False, reads=[sm_.k, vt_.k], writes=[pso.k])
                    I("pe", "matmul", pso[:, hs], lhsT=qf[:, h, sl], rhs=sf_[:, hs], start=False, stop=False, reads=[qf.k + "_%d" % ci, sf_.k], writes=[pso.k])
                    I("pe", "matmul", pso[:, hs], lhsT=qb[:, h, sl], rhs=sb_[:, hs], start=False, stop=True, reads=[qb.k + "_%d" % ci, sb_.k], writes=[pso.k])
                smal = small[i2]
                xc_ = xc[i2]
                for h in range(4):
                    hs = slice(h * 128, (h + 1) * 128)
                    I("act", "activation", out=junk2[:], in_=pso[:, hs], func=AF.Copy, accum_out=smal[:, h:h + 1], reads=[pso.k], writes=[junk2.k, smal.k])
                    I("act", "activation", out=junk2[:], in_=pso[:, hs], func=AF.Square, accum_out=smal[:, 4 + h:5 + h], reads=[pso.k], writes=[junk2.k, smal.k])
                I("dve", "tensor_scalar", out=smal[:, 0:4], in0=smal[:, 0:4], scalar1=1.0 / 128, scalar2=None, op0=ALU.mult, reads=[smal.k], writes=[smal.k])
                I("dve", "tensor_tensor", out=smal[:, 12:16], in0=smal[:, 0:4], in1=smal[:, 0:4], op=ALU.mult, reads=[smal.k], writes=[smal.k])
                I("dve", "scalar_tensor_tensor", out=smal[:, 8:12], in0=smal[:, 4:8], scalar=1.0 / 128, in1=smal[:, 12:16], op0=ALU.mult, op1=ALU.subtract,
                  reads=[smal.k], writes=[smal.k])
                I("act", "activation", out=smal[:, 8:12], in_=smal[:, 8:12], func=AF.Sqrt, bias=epsc[:], reads=[smal.k, epsc.k], writes=[smal.k])
                I("dve", "reciprocal", out=smal[:, 8:12], in_=smal[:, 8:12], reads=[smal.k], writes=[smal.k])
                I("dve", "scalar_tensor_tensor", out=smal[:, 12:16], in0=smal[:, 0:4], scalar=-1.0, in1=smal[:, 8:12], op0=ALU.mult, op1=ALU.mult,
                  reads=[smal.k], writes=[smal.k])
                for h in range(4):
                    hs = slice(h * 128, (h + 1) * 128)
                    I("act", "activation", out=xc_[:, hs], in_=pso[:, hs], func=AF.Identity, scale=smal[:, 8 + h:9 + h], bias=smal[:, 12 + h:13 + h],
                      reads=[pso.k, smal.k], writes=[xc_.k])
                rt_ = rett[i2]
                I("dve", "tensor_tensor", out=rt_[:], in0=xc_[:], in1=sg_[:], op=ALU.mult, reads=[xc_.k, sg_.k], writes=[rt_.k])
                cstate[ci] = (i2, rt_, pst)

            def c_back(ci):
                sl = slice(ci * 128, (ci + 1) * 128)
                i2, rt_, ptr = cstate[ci]
                for h in range(4):
                    hs = slice(h * 128, (h + 1) * 128)
                    I("pe", "transpose", out=ptr[:].bitcast(BF16)[:, hs], in_=rt_[:, hs], identity=identb[:], reads=[rt_.k, identb.k], writes=[ptr.k])
                I("act", "activation", out=yret[:, :, sl], in_=ptr[:].bitcast(BF16)[:, 0:512].rearrange("p (h t) -> p h t", h=4), func=AF.Copy,
                  reads=[ptr.k], writes=[yret.k + "_%d" % ci])

            nci = T_ // 128
            c_front(0)
            for ci in range(nci):
                if ci + 1 < nci:
                    c_front(ci + 1)
                c_back(ci)
            need_ffn = not (is_ctx and last)
            tstate = {}

            def t_front(tt):
                ntl[0] += 1
                ntile = ntl[0]
                i2 = ntile % 2
                r0 = g0 + tt * 128
                ts_ = slice(tt * 128, (tt + 1) * 128)
                x1, t2, ssq, rstd = x1s4[ntile % 4], t2s[i2], ssqs[i2], rstds[i2]
                xt = xts4[(ntile - 1) % 8]
                precast_some(4)
                if is_ctx and last:
                    tstate[tt] = None
                    return
                gm = modsel(GM, g0)
                for half in range(2):
                    cs = slice(half * 512, (half + 1) * 512)
                    py = PS[4 + half]
                    for kc in range(8):
                        src = yrg if kc < 4 else yret
                        I("pe", "matmul", py[:], lhsT=src[:, kc % 4, ts_], rhs=Wout[:, kc, cs], start=(kc == 0), stop=(kc == 7),
                          reads=[src.k if kc < 4 else yret.k + "_%d" % tt, Wout.k], writes=[py.k])
                    I("dve", "tensor_tensor", out=x1[:, cs], in0=py[:], in1=gm[:, cs], op=ALU.mult, reads=[py.k, gm.k], writes=[x1.k])
                I("dve", "tensor_tensor", out=x1[:], in0=x1[:], in1=xt[:], op=ALU.add, reads=[x1.k, xt.k], writes=[x1.k])
                dma("sp", X1[r0:r0 + 128, :], x1[:], [x1.k], ["X1"])
                I("act", "activation", out=junk[:], in_=x1[:], func=AF.Square, accum_out=ssq[:], reads=[x1.k], writes=[junk.k, ssq.k])
                sqrt_recip(rstd, ssq, 1.0 / D)
                g2, sh2 = modsel(G2, g0), modsel(SH2, g0)
                I("dve", "scalar_tensor_tensor", out=t2[:], in0=x1[:], scalar=rstd[:], in1=g2[:], op0=ALU.mult, op1=ALU.mult,
                  reads=[x1.k, rstd.k, g2.k], writes=[t2.k])
                I("dve", "tensor_tensor", out=t2[:], in0=t2[:], in1=sh2[:], op=ALU.add, reads=[t2.k, sh2.k], writes=[t2.k])
                tstate[tt] = (i2, r0, ts_, t2)

            def t_back(tt):
                if tstate[tt] is None:
                    return
                i2, r0, ts_, t2 = tstate[tt]
                if not moe_layer:
                    hb_ = h2tb[i2]
                    I("act", "activation", out=hb_[:], in_=t2[:], func=AF.Copy, reads=[t2.k], writes=[hb_.k])
                    pa = PS[6 + i2]
                    for kc in range(8):
                        I("pe", "transpose", out=pa[:].bitcast(BF16)[:, kc * 128:(kc + 1) * 128], in_=hb_[:, kc * 128:(kc + 1) * 128], identity=identb[:],
                          reads=[hb_.k, identb.k], writes=[pa.k])
                    I("act", "activation", out=h2st[:, :, ts_], in_=pa[:].bitcast(BF16)[:, 0:1024].rearrange("p (k t) -> p k t", k=8), func=AF.Copy,
                      reads=[pa.k], writes=[h2st.k])
                elif not is_ctx:
                    pa, pb = PS[6], PS[7]
                    for kc in range(8):
                        pp = pa if kc < 4 else pb
                        I("pe", "transpose", out=pp[:, (kc % 4) * 128:(kc % 4 + 1) * 128], in_=t2[:, kc * 128:(kc + 1) * 128], identity=identf[:],
                          reads=[t2.k, identf.k], writes=[pp.k])
                    hf = h2f[i2]
                    for hh, pp in enumerate((pa, pb)):
                        I("dve", "tensor_copy", out=hf[:, hh * 4:(hh + 1) * 4, :], in_=pp[:].rearrange("p (k t) -> p k t", k=4), reads=[pp.k], writes=[hf.k])
                    pl = PS[4]
                    for kc in range(8):
                        I("pe", "matmul", pl[:, 0:128], lhsT=hf[:, kc, :], rhs=wr[:, kc, :], start=(kc == 0), stop=(kc == 7),
                          reads=[hf.k, wr.k], writes=[pl.k])
                    r_ = rt[i2]
                    s_ = rsm[i2]
                    ti = (r0 - NCTX) // 128
                    I("dve", "tensor_tensor", out=r_[:, 0, :], in0=pl[:, 0:NEXP], in1=brt[:], op=ALU.add, reads=[pl.k, brt.k], writes=[r_.k])
                    I("dve", "reduce_max", out=s_[:, 0:1], in_=r_[:, 0, :], axis=AX.X, reads=[r_.k], writes=[s_.k])
                    I("dve", "tensor_scalar", out=r_[:, 1, :], in0=r_[:, 0, :], scalar1=s_[:, 0:1], scalar2=None, op0=ALU.is_ge, reads=[r_.k, s_.k], writes=[r_.k])
                    I("dve", "scalar_tensor_tensor", out=r_[:, 2, :], in0=r_[:, 1, :], scalar=-1e30, in1=r_[:, 0, :], op0=ALU.mult, op1=ALU.add, reads=[r_.k], writes=[r_.k])
                    I("dve", "reduce_max", out=s_[:, 1:2], in_=r_[:, 2, :], axis=AX.X, reads=[r_.k], writes=[s_.k])
                    I("dve", "tensor_scalar", out=r_[:, 3, :], in0=r_[:, 0, :], scalar1=s_[:, 1:2], scalar2=None, op0=ALU.is_ge, reads=[r_.k, s_.k], writes=[r_.k])
                    I("dve", "tensor_scalar", out=s_[:, 2:3], in0=s_[:, 0:1], scalar1=-1.0, scalar2=None, op0=ALU.mult, reads=[s_.k], writes=[s_.k])
                    I("act", "activation", out=r_[:, 4, :], in_=r_[:, 0, :], func=AF.Exp, bias=s_[:, 2:3], reads=[r_.k, s_.k], writes=[r_.k])
                    I("dve", "tensor_tensor", out=r_[:, 5, :], in0=r_[:, 4, :], in1=r_[:, 3, :], op=ALU.mult, reads=[r_.k], writes=[r_.k])
                    I("dve", "reduce_sum", out=s_[:, 3:4], in_=r_[:, 5, :], axis=AX.X, reads=[r_.k], writes=[s_.k])
                    I("dve", "reciprocal", out=s_[:, 4:5], in_=s_[:, 3:4], reads=[s_.k], writes=[s_.k])
                    I("dve", "tensor_scalar", out=RW[:, ti, :], in0=r_[:, 5, :], scalar1=s_[:, 4:5], scalar2=None, op0=ALU.mult, reads=[r_.k, s_.k], writes=[RW.k])
                    I("dve", "tensor_copy", out=SEL[:, ti, :], in_=r_[:, 3, :], reads=[r_.k], writes=[SEL.k])
                    I("dve", "tensor_copy", out=OH1[:, ti, :], in_=r_[:, 1, :], reads=[r_.k], writes=[OH1.k])
                    hb_ = h2tb[i2]
                    I("act", "activation", out=hb_[:], in_=t2[:], func=AF.Copy, reads=[t2.k], writes=[hb_.k])
                    dma("sp", H2T[r0 - NCTX:r0 - NCTX + 128, :], hb_[:], [hb_.k], ["H2T"])
            ntt = T_ // 128
            t_front(0)
            for tt in range(ntt):
                if tt + 1 < ntt:
                    t_front(tt + 1)
                t_back(tt)
            if need_ffn and not moe_layer:
                dma("sp", H2[:, g0:g0 + T_].rearrange("(k p) t -> p k t", p=128), h2st[:, :, 0:T_], [h2st.k], ["H2"])
            if mi + 2 < len(MACROS):
                c_macro_loads(mi + 2)

        if moe_layer:
            chk(l, "C")
            moe_sorted(l)
            continue
        A.reset(phase_base)
        P.barrier()
        GF = load_mod(5, "GF")
        if last:
            gfin = sb("gfin", [128, D], F32)
            dma("sp", gfin[:], g_final.rearrange("(o n) -> o n", o=1).broadcast_to([128, D]), [], [gfin.k])
        if moe_layer:
            blocks = [(NCTX + 1024 * i, 1024) for i in range(4)]
            experts = [(MW1b[e_], MW3b[e_], MW2b[e_], e_) for e_ in range(n_exp)]
            FC = DEXP // 128
            FS = 7
        else:
            blocks = [(0, 256)] + [(NCTX + 1024 * i, 1024) for i in range(4)]
            experts = [(FW1b, FW3b, FW2b, None)]
            FC = DFF // 128
            FS = 6
        H2bs = [sb("H2b%d" % i, [128, 8, 1024], BF16) for i in range(2)]
        Wset = [(sb("W1q%d" % i, [128, 8, 7 * 128], BF16), sb("W3q%d" % i, [128, 8, 7 * 128], BF16), sb("W2q%d" % i, [128, 7, D], BF16))
                for i in range(2)]
        actq = [sb("actq%d" % i, [128, 7, 1024], BF16) for i in range(2)]
        yacc = sb("yacc", [128, 8, D], F32)
        slt = [sb("slt%d" % i, [128, 512], F32) for i in range(3)]
        x1s = [sb("x1_%d" % i, [128, D], F32) for i in range(2)]
        junk = sb("junk", [128, D], F32)
        ssqs = [sb("ssq%d" % i, [128, 1], F32) for i in range(2)]
        rstds = [sb("rstd%d" % i, [128, 1], F32) for i in range(2)]
        units = []
        for bi, (g0, TD) in enumerate(blocks):
            per = [(w, f0, min(FS, FC - f0)) for w in experts for f0 in range(0, FC, FS)]
            for ui, (w, f0, fs) in enumerate(per):
                units.append((bi, g0, TD, w, f0, fs, ui == 0, ui == len(per) - 1))
        cntd = {"ps": 0, "fin": 0, "py": 0}

        def L13(s_):
            bi, g0, TD, w, f0, fs, fst, lst = units[s_]
            w1q, w3q, _ = Wset[s_ % 2]
            dma("sp", w1q[:, :, 0:fs * 128], w[0][:, :, f0 * 128:(f0 + fs) * 128], ["Wb"], [w1q.k])
            dma("sp", w3q[:, :, 0:fs * 128], w[1][:, :, f0 * 128:(f0 + fs) * 128], ["Wb"], [w3q.k])

        def L2(s_):
            bi, g0, TD, w, f0, fs, fst, lst = units[s_]
            w2q = Wset[s_ % 2][2]
            dma("sp", w2q[:, 0:fs, :], w[2][:, f0:f0 + fs, :], ["Wb"], [w2q.k])

        def S1(s_):
            bi, g0, TD, w, f0, fs, fst, lst = units[s_]
            w1q, w3q, _ = Wset[s_ % 2]
            aq = actq[s_ % 2]
            H2b = H2bs[bi % 2]
            if fst and bi + 1 < len(blocks):
                g0n, TDn = blocks[bi + 1]
                dma("sp", H2bs[(bi + 1) % 2][:, :, 0:TDn], H2[:, g0n:g0n + TDn].rearrange("(k p) t -> p k t", p=128), ["H2"], [H2bs[(bi + 1) % 2].k])
            pieces = [(p0, min(512, TD - p0)) for p0 in range(0, TD, 512)]
            for j in range(fs):
                for (p0, pw) in pieces:
                    cntd["ps"] += 1
                    n_ = cntd["ps"]
                    pa, pb = PS[(n_ % 2) * 2], PS[(n_ % 2) * 2 + 1]
                    for kc in range(8):
                        I("pe", "matmul", pa[:, 0:pw], lhsT=w1q[:, kc, j * 128:(j + 1) * 128], rhs=H2b[:, kc, p0:p0 + pw], start=(kc == 0), stop=(kc == 7),
                          reads=[w1q.k, H2b.k], writes=[pa.k])
                    for kc in range(8):
                        I("pe", "matmul", pb[:, 0:pw], lhsT=w3q[:, kc, j * 128:(j + 1) * 128], rhs=H2b[:, kc, p0:p0 + pw], start=(kc == 0), stop=(kc == 7),
                          reads=[w3q.k, H2b.k], writes=[pb.k])
                    sl_ = slt[n_ % len(slt)]
                    I("act", "activation", out=sl_[:, 0:pw], in_=pa[:, 0:pw], func=AF.Silu, reads=[pa.k], writes=[sl_.k])
                    I("dve", "tensor_tensor", out=aq[:, j, p0:p0 + pw], in0=sl_[:, 0:pw], in1=pb[:, 0:pw], op=ALU.mult,
                      reads=[sl_.k, pb.k], writes=[aq.k + "_%d_%d" % (j, p0)])

        def S2(s_):
            bi, g0, TD, w, f0, fs, fst, lst = units[s_]
            w2q = Wset[s_ % 2][2]
            aq = actq[s_ % 2]
            eidx = w[3]
            for tt in range(TD // 128):
                ts_ = slice(tt * 128, (tt + 1) * 128)
                for half in range(2):
                    cs = slice(half * 512, (half + 1) * 512)
                    cntd["py"] += 1
                    py = PS[4 + cntd["py"] % 4]
                    for j in range(fs):
                        I("pe", "matmul", py[:], lhsT=aq[:, j, ts_], rhs=w2q[:, j, cs], start=(j == 0), stop=(j == fs - 1),
                          reads=[aq.k + "_%d_0" % j, aq.k + "_%d_512" % j, w2q.k], writes=[py.k])
                    yk = yacc.k + "_%dh%d" % (tt, half)
                    if eidx is None:
                        if fst:
                            I("act", "activation", out=yacc[:, tt, cs], in_=py[:], func=AF.Copy, reads=[py.k], writes=[yk])
                        else:
                            I("dve", "tensor_tensor", out=yacc[:, tt, cs], in0=py[:], in1=yacc[:, tt, cs], op=ALU.add, reads=[py.k, yk], writes=[yk])
                    else:
                        ti = (g0 - NCTX) // 128 + tt
                        if fst:
                            I("dve", "tensor_scalar", out=yacc[:, tt, cs], in0=py[:], scalar1=RW[:, ti, eidx:eidx + 1], scalar2=None, op0=ALU.mult,
                              reads=[py.k, RW.k], writes=[yk])
                        else:
                            I("dve", "scalar_tensor_tensor", out=yacc[:, tt, cs], in0=py[:], scalar=RW[:, ti, eidx:eidx + 1], in1=yacc[:, tt, cs],
                              op0=ALU.mult, op1=ALU.add, reads=[py.k, RW.k, yk], writes=[yk])
            if lst:
                gf = modsel(GF, g0)
                for tt in range(TD // 128):
                    cntd["fin"] += 1
                    i2 = cntd["fin"] % 2
                    r0 = g0 + tt * 128
                    x1, ssq, rstd = x1s[i2], ssqs[i2], rstds[i2]
                    yk = yacc.k + "_%dh0" % tt
                    yk1 = yacc.k + "_%dh1" % tt
                    dma("sp", x1[:], X1[r0:r0 + 128, :], ["X1"], [x1.k])
                    I("dve", "tensor_tensor", out=yacc[:, tt, :], in0=yacc[:, tt, :], in1=gf[:], op=ALU.mult, reads=[yk, yk1, gf.k], writes=[yk, yk1])
                    I("dve", "tensor_tensor", out=x1[:], in0=x1[:], in1=yacc[:, tt, :], op=ALU.add, reads=[x1.k, yk, yk1], writes=[x1.k])
                    if not last:
                        dma("sp", XS[r0:r0 + 128, :], x1[:], [x1.k], ["XS"])
                    else:
                        I("act", "activation", out=junk[:], in_=x1[:], func=AF.Square, accum_out=ssq[:],
                          reads=[x1.k], writes=[junk.k, ssq.k])
                        sqrt_recip(rstd, ssq, 1.0 / D)
                        I("dve", "scalar_tensor_tensor", out=x1[:], in0=x1[:], scalar=rstd[:], in1=gfin[:], op0=ALU.mult, op1=ALU.mult,
                          reads=[x1.k, rstd.k, gfin.k], writes=[x1.k])
                        dma("sp", out[r0 - NCTX:r0 - NCTX + 128, :], x1[:], [x1.k], ["OUT"])

        NU = len(units)
        dma("sp", H2bs[0][:, :, 0:blocks[0][1]], H2[:, blocks[0][0]:blocks[0][0] + blocks[0][1]].rearrange("(k p) t -> p k t", p=128), ["H2"], [H2bs[0].k])
        for s_ in range(min(2, NU)):
            L13(s_)
            L2(s_)
        for s_ in range(NU):
            if s_ >= 1 and s_ + 1 < NU:
                L13(s_ + 1)
            S1(s_)
            if s_ >= 1:
                S2(s_ - 1)
                if s_ + 1 < NU:
                    L2(s_ + 1)
            if not moe_layer:
                precast_some(10)
        S2(NU - 1)

    except _Stop:
        pass
    P.barrier()
    with nc.allow_non_contiguous_dma(reason="small parameter / head-split loads"):
        P.emit()
    psum_ctx.close()
    return nc


def host_constants():
    n_freq = 32
    pos = np.arange(NLAT)
    row = (pos // 64).astype(np.float32)
    col = (pos % 64).astype(np.float32)
    inv = (10000.0 ** (-np.arange(n_freq, dtype=np.float32) / n_freq)).astype(np.float32)
    ang = np.concatenate([row[:, None] * inv, col[:, None] * inv], axis=-1).astype(np.float32)
    cos = np.cos(ang).astype(np.float32).T
    sin = np.sin(ang).astype(np.float32).T
    k_cos = np.ones((128, NT), np.float32)
    k_sin = np.zeros((128, NT), np.float32)
    k_cos[0:64, NCTX:] = cos
    k_cos[64:128, NCTX:] = cos
    k_sin[0:64, NCTX:] = -sin
    k_sin[64:128, NCTX:] = sin
    j = np.arange(128, dtype=np.float32)[:, None]
    i = np.arange(128, dtype=np.float32)[None, :]
    tab = np.zeros((128, 8, 128), np.float32)
    tab[:, 0] = np.maximum(i - j, 0)
    tab[:, 1] = (i >= j)
    tab[:, 2] = np.maximum(j - i, 0)
    tab[:, 3] = (j > i)
    tab[:, 4] = np.broadcast_to(i + 1, (128, 128))
    tab[:, 5] = np.broadcast_to(128 - i, (128, 128))
    tab[:, 6] = np.broadcast_to(127 - j, (128, 128))
    tab[:, 7] = np.broadcast_to(j, (128, 128))
    tab2 = np.zeros((128, 2, 128), np.float32)
    tab2[:, 0] = (j <= i)
    tab2[:, 1] = np.broadcast_to(j, (128, 128))
    return {
        "k_zero": np.zeros((128, 4096), np.float32).astype(ml_dtypes.bfloat16),
        "k_tab2": tab2,
        "k_identb": np.eye(128, dtype=np.float32).astype(ml_dtypes.bfloat16),
        "k_identf": np.eye(128, dtype=np.float32),
        "k_cos": k_cos, "k_sin": k_sin, "k_tab": tab,
    }


def make_in_maps(inputs, cores):
    f = lambda a: np.ascontiguousarray(np.asarray(a, dtype=np.float32))
    w_in = f(inputs["w_in"])
    idx = []
    for blk in range(8):
        b0 = 1024 + blk * 128
        idx.extend(list(range(b0 + 64, b0 + 128)) + list(range(b0, b0 + 64)))
    w_in_ext = np.ascontiguousarray(np.concatenate([w_in, w_in[:, :, idx]], axis=-1))
    shared = {
        "c_ctx": f(inputs["c_ctx"]), "w_mod": f(inputs["w_mod"]), "b_mod": f(inputs["b_mod"]),
        "g_mix": f(inputs["g_mix"]), "g_ffn": f(inputs["g_ffn"]), "g_final": f(inputs["g_final"]),
        "w_in": w_in_ext, "w_out": f(inputs["w_out"]), "conv_w": f(inputs["conv_w"]), "conv_b": f(inputs["conv_b"]),
        "rg_wa": f(inputs["rg_wa"]), "rg_ba": f(inputs["rg_ba"]), "rg_wx": f(inputs["rg_wx"]), "rg_bx": f(inputs["rg_bx"]),
        "rg_lam": f(inputs["rg_lam"]), "ret_decay": f(inputs["ret_decay"]).reshape(2, 8),
        "ffn_w1": f(inputs["ffn_w1"]), "ffn_w3": f(inputs["ffn_w3"]), "ffn_w2": f(inputs["ffn_w2"]),
        "moe_router": f(inputs["moe_router"])[0], "moe_router_b": f(inputs["moe_router_b"])[0],
        "moe_w1": f(inputs["moe_w1"])[0], "moe_w3": f(inputs["moe_w3"])[0], "moe_w2": f(inputs["moe_w2"])[0],
    }
    shared.update(host_constants())
    x = f(inputs["x"])
    ctx = f(inputs["ctx"])
    c = f(inputs["c"])
    maps = []
    for b in cores:
        m = dict(shared)
        m["xin"] = np.ascontiguousarray(np.concatenate([ctx[b], x[b]], axis=0))
        m["c_b"] = np.ascontiguousarray(c[b])
        maps.append(m)
    return maps


_NC_CACHE = {}


def kernel(**inputs):
    if "nc" not in _NC_CACHE:
        _NC_CACHE["nc"] = build_program()
    nc = _NC_CACHE["nc"]
    in_maps = make_in_maps(inputs, list(range(8)))
    res = run_bass_kernel_spmd(nc, in_maps, core_ids=list(range(8)))
    return np.stack([np.asarray(r["out"], dtype=np.float32) for r in res.results], axis=0)
```

```python
import contextlib
import numpy as np
import ml_dtypes
import concourse.bass as bass
import concourse.mybir as mybir
from concourse.bass_utils import run_bass_kernel_spmd

F32 = mybir.dt.float32
BF16 = mybir.dt.bfloat16
I32 = mybir.dt.int32
AF = mybir.ActivationFunctionType
ALU = mybir.AluOpType
AX = mybir.AxisListType

D = 1024
NLAT = 4096
NCTX = 256
NT = NLAT + NCTX
NCH = NT // 128
UW = NT + 8
DFF = 2816
DEXP = 3584
NEXP = 8
EPS = 1e-6
MACROS = [(0, 256)] + [(256 + 512 * i, 512) for i in range(8)]

ENGS = ("pe", "act", "dve", "pool", "sp")
DMA_RING = 24
SAME_ENGINE_ALL = True


def ucol(g):
    return g + 2 if g < NCTX else g + 5


class Op:
    __slots__ = ("eng", "fn", "deps", "dma", "signals", "sigval", "dsem", "dval", "dprev", "ring")

    def __init__(self, eng, fn, dma):
        self.eng = eng
        self.fn = fn
        self.dma = dma
        self.deps = []
        self.signals = False
        self.sigval = 0
        self.dsem = None
        self.dval = 0
        self.dprev = 0
        self.ring = "n"


class Prog:
    def __init__(self, nc):
        self.nc = nc
        self.ops = {e: [] for e in ENGS}
        self.lastw = {}
        self.readers = {}
        self.ndma = {e: 0 for e in ENGS}
        self.ndma_bg = {e: 0 for e in ENGS}
        self.dmas_bg = {e: [] for e in ENGS}
        self.lastc = {e: None for e in ENGS}
        self.dmas = {e: [] for e in ENGS}

    def _add(self, eng, fn, reads, writes, dma, extra=(), bg=False):
        op = Op(eng, fn, dma)
        deps = list(extra)
        for k in reads:
            w = self.lastw.get(k)
            if w is not None:
                deps.append(w)
            if k.startswith("ps"):
                deps.extend(r for r in self.readers.get(k, ()) if r.eng != eng)
        nraw = len(deps)
        for k in writes:
            w = self.lastw.get(k)
            if w is not None:
                deps.append(w)
            deps.extend(self.readers.get(k, ()))
        raw_ids = set(id(d) for d in deps[:nraw])
        for k in reads:
            self.readers.setdefault(k, []).append(op)
        for k in writes:
            self.lastw[k] = op
            self.readers[k] = []
        seen = set()
        for d in deps:
            if d is op or id(d) in seen:
                continue
            seen.add(id(d))
            if (not d.dma) and (not dma) and d.eng == eng and fn is not None:
                if eng == "pe" or (id(d) not in raw_ids and not SAME_ENGINE_ALL):
                    continue
            op.deps.append(d)
            if not d.dma:
                d.signals = True
        if dma and bg:
            i = self.ndma_bg[eng]
            self.ndma_bg[eng] = i + 1
            op.ring = "bg"
            op.dsem = i % DMA_RING
            op.dval = 16 * (i // DMA_RING + 1)
            op.dprev = 16 * (i // DMA_RING)
            self.dmas_bg[eng].append(op)
        elif dma:
            i = self.ndma[eng]
            self.ndma[eng] = i + 1
            op.dsem = i % DMA_RING
            op.dval = 16 * (i // DMA_RING + 1)
            op.dprev = 16 * (i // DMA_RING)
            self.dmas[eng].append(op)
        elif fn is not None:
            self.lastc[eng] = op
        self.ops[eng].append(op)
        return op

    def op(self, eng, fn, reads=(), writes=()):
        return self._add(eng, fn, reads, writes, False)

    def dma(self, eng, fn, reads=(), writes=(), bg=False):
        return self._add(eng, fn, reads, writes, True, bg=bg)

    def barrier(self, include_bg=False):
        pend = [o for o in self.lastc.values() if o is not None]
        for e in ENGS:
            pend.extend(self.dmas[e][-DMA_RING:])
            if include_bg:
                pend.extend(self.dmas_bg[e][-DMA_RING:])
        for e in ENGS:
            self._add(e, None, (), (), False, extra=pend)
        self.lastw = {}
        self.readers = {}

    def emit(self):
        nc = self.nc
        for e in ENGS:
            cnt = 0
            for op in self.ops[e]:
                if (not op.dma) and op.fn is not None and op.signals:
                    cnt += 1
                    op.sigval = cnt
        with contextlib.ExitStack() as st:
            csem = {e: st.enter_context(nc.semaphore("c_" + e)) for e in ENGS}
            dsem = {(e, "n"): [st.enter_context(nc.semaphore("d_%s%d" % (e, i))) for i in range(DMA_RING)]
                    for e in ENGS if self.ndma[e] > 0}
            dsem.update({(e, "bg"): [st.enter_context(nc.semaphore("b_%s%d" % (e, i))) for i in range(DMA_RING)]
                         for e in ENGS if self.ndma_bg[e] > 0})
            block = st.enter_context(nc.Block())
            handles = {"pe": nc.tensor, "act": nc.scalar, "dve": nc.vector, "pool": nc.gpsimd, "sp": nc.sync}

            def build(e):
                eng = handles[e]
                seen = {}

                def wait(key, sem, val):
                    if seen.get(key, 0) >= val:
                        return
                    seen[key] = val
                    eng.wait_ge(sem, val)

                for op in self.ops[e]:
                    for d in op.deps:
                        if d.dma:
                            wait(("d", d.eng, d.ring, d.dsem), dsem[(d.eng, d.ring)][d.dsem], d.dval)
                        else:
                            wait(("c", d.eng), csem[d.eng], d.sigval)
                    if op.fn is None:
                        continue
                    if op.dma:
                        if op.dprev > 0:
                            wait(("d", e, op.ring, op.dsem), dsem[(e, op.ring)][op.dsem], op.dprev)
                        op.fn(eng).then_inc(dsem[(e, op.ring)][op.dsem], 16)
                    else:
                        ins = op.fn(eng)
                        if op.signals:
                            ins.then_inc(csem[e], 1)

            @block.tensor
            def _(t):
                build("pe")

            @block.scalar
            def _(t):
                build("act")

            @block.vector
            def _(t):
                build("dve")

            @block.gpsimd
            def _(t):
                build("pool")

            @block.sync
            def _(t):
                build("sp")


class Arena:
    def __init__(self, nc):
        self.nc = nc
        self.base = (nc.sbuf_base + 63) // 64 * 64
        self.top = nc.sbuf_top
        self.off = self.base
        self.n = 0

    def alloc(self, name, shape, dt):
        nbytes = int(np.prod(shape[1:])) * (2 if dt == BF16 else 4)
        nbytes = (nbytes + 63) // 64 * 64
        assert self.off + nbytes <= self.top, ("SBUF overflow", name, self.off, nbytes, self.top)
        self.n += 1
        t = self.nc.alloc_sbuf_tensor_at("%s_%d" % (name, self.n), list(shape), dt, offset=self.off)
        self.off += nbytes
        return t

    def mark(self):
        return self.off

    def reset(self, m):
        self.off = m


class _Stop(Exception):
    pass


def build_program(n_layers=2, debug=False, stop=None, n_exp=NEXP):
    nc = bass.Bass("TRN2", target_bir_lowering=False)
    P = Prog(nc)
    A = Arena(nc)
    ikind = "ExternalOutput" if debug else "Internal"

    def din(name, shape, dt=F32):
        return nc.dram_tensor(name, list(shape), dt, kind="ExternalInput").ap()

    def dscr(name, shape, dt):
        return nc.dram_tensor(name, list(shape), dt, kind=ikind).ap()

    xin = din("xin", [NT, D])
    c_b = din("c_b", [D])
    c_ctx = din("c_ctx", [D])
    w_mod = din("w_mod", [2, D, 6 * D])
    b_mod = din("b_mod", [2, 6 * D])
    g_mix = din("g_mix", [2, D])
    g_ffn = din("g_ffn", [2, D])
    g_final = din("g_final", [D])
    w_in = din("w_in", [2, D, 4096])
    w_out = din("w_out", [2, D, D])
    conv_w = din("conv_w", [2, 4, 512])
    conv_b = din("conv_b", [2, 512])
    rg_wa = din("rg_wa", [2, 2, 8, 64, 64])
    rg_ba = din("rg_ba", [2, 2, 512])
    rg_wx = din("rg_wx", [2, 2, 8, 64, 64])
    rg_bx = din("rg_bx", [2, 2, 512])
    rg_lam = din("rg_lam", [2, 2, 512])
    ret_decay = din("ret_decay", [2, 8])
    ffn_w1 = din("ffn_w1", [1, D, DFF])
    ffn_w3 = din("ffn_w3", [1, D, DFF])
    ffn_w2 = din("ffn_w2", [1, DFF, D])
    moe_router = din("moe_router", [D, NEXP])
    moe_router_b = din("moe_router_b", [NEXP])
    moe_w1 = din("moe_w1", [NEXP, D, DEXP])
    moe_w3 = din("moe_w3", [NEXP, D, DEXP])
    moe_w2 = din("moe_w2", [NEXP, DEXP, D])
    k_identb = din("k_identb", [128, 128], BF16)
    k_identf = din("k_identf", [128, 128])
    k_cos = din("k_cos", [128, NT])
    k_sin = din("k_sin", [128, NT])
    k_tab = din("k_tab", [128, 8, 128])
    out = nc.dram_tensor("out", [NLAT, D], F32, kind="ExternalOutput").ap()

    XS = dscr("s_x", [NT, D], F32)
    X1 = dscr("s_x1", [NT, D], F32)
    MODB = dscr("s_modb", [4, 128, 6 * D], F32)
    U = dscr("s_u", [512, UW], BF16)
    GY = dscr("s_gy", [512, NT], BF16)
    Q = dscr("s_q", [512, NT], BF16)
    K = dscr("s_k", [512, NT], BF16)
    KT = dscr("s_kt", [NT, 512], BF16)
    VT = dscr("s_vt", [NT, 512], BF16)
    SGT = dscr("s_sgt", [NT, 512], BF16)
    SF = dscr("s_sf", [NCH, 128, 512], BF16)
    SB = dscr("s_sb", [NCH, 128, 512], BF16)
    YRG = dscr("s_yrg", [512, NT], BF16)
    H2 = dscr("s_h2", [D, NT], BF16)

    def dscr_i(name, shape, dt):
        return nc.dram_tensor(name, list(shape), dt, kind="Internal").ap()

    FW1b = dscr_i("s_fw1", [128, 8, DFF], BF16)
    FW3b = dscr_i("s_fw3", [128, 8, DFF], BF16)
    FW2b = dscr_i("s_fw2", [128, DFF // 128, D], BF16)
    MW1q = dscr_i("s_mw1", [NEXP * 4 * 128, 8 * 896], BF16)
    MW3q = dscr_i("s_mw3", [NEXP * 4 * 128, 8 * 896], BF16)
    MW2q = dscr_i("s_mw2", [NEXP * 4 * 128, 7 * D], BF16)
    UNIT = 512
    NUN = 8192 // UNIT + NEXP
    NROWS = NUN * UNIT
    H2T = dscr("s_h2t", [NLAT, D], BF16)
    HS = dscr("s_hs", [NROWS, D], BF16)
    YS = dscr("s_ys", [NROWS, D], F32)
    k_zero = din("k_zero", [128, 4096], BF16)
    k_tab2 = din("k_tab2", [128, 2, 128])

    uid = [0]

    def key(s):
        uid[0] += 1
        return "%s#%d" % (s, uid[0])

    class T:
        def __init__(self, t, k):
            self.t = t
            self.k = k

        def __getitem__(self, idx):
            return self.t[idx]

    def sb(name, shape, dt):
        return T(A.alloc(name, shape, dt), key(name))

    psum_ctx = contextlib.ExitStack()
    PS = [T(psum_ctx.enter_context(nc.psum_tensor("ps%d" % i, [128, 512], F32)), "ps%d" % i) for i in range(8)]

    def ps_bf(i):
        return PS[i].t.bitcast(BF16) if hasattr(PS[i].t, "bitcast") else None

    def I(eng, name, *args, reads=(), writes=(), **kw):
        return P.op(eng, lambda e: getattr(e, name)(*args, **kw), reads=reads, writes=writes)

    def dma(q, out_ap, in_ap, reads, writes):
        return P.dma(q, lambda e: e.dma_start(out=out_ap, in_=in_ap), reads=reads, writes=writes)

    def cdma(out_ap, in_ap, reads, writes):
        return P.dma("pool", lambda e: e.dma_start(out=out_ap, in_=in_ap), reads=reads, writes=writes)

    pre_list = []

    def add_pre(w1, w3, w2, d1, d3, d2, F):
        hw = F // 2
        for kc in range(8):
            for hh in range(2):
                pre_list.append((d1[:, kc, hh * hw:(hh + 1) * hw], w1[kc * 128:(kc + 1) * 128, hh * hw:(hh + 1) * hw]))
                pre_list.append((d3[:, kc, hh * hw:(hh + 1) * hw], w3[kc * 128:(kc + 1) * 128, hh * hw:(hh + 1) * hw]))
        for fc in range(F // 128):
            pre_list.append((d2[:, fc, :], w2[fc * 128:(fc + 1) * 128, :]))

    add_pre(ffn_w1[0], ffn_w3[0], ffn_w2[0], FW1b, FW3b, FW2b, DFF)
    n_ffn_pre = len(pre_list)
    for e_ in range(n_exp):
        for q in range(4):
            r0_ = (e_ * 4 + q) * 128
            for kc in range(8):
                pre_list.append((MW1q[r0_:r0_ + 128, kc * 896:(kc + 1) * 896], moe_w1[e_][kc * 128:(kc + 1) * 128, q * 896:(q + 1) * 896]))
                pre_list.append((MW3q[r0_:r0_ + 128, kc * 896:(kc + 1) * 896], moe_w3[e_][kc * 128:(kc + 1) * 128, q * 896:(q + 1) * 896]))
            for j in range(7):
                pre_list.append((MW2q[r0_:r0_ + 128, j * D:(j + 1) * D], moe_w2[e_][(q * 7 + j) * 128:(q * 7 + j + 1) * 128, :]))
    pre_pos = [0]

    def precast_some(n):
        while n > 0 and pre_pos[0] < len(pre_list):
            d_, s_ = pre_list[pre_pos[0]]
            pre_pos[0] += 1
            n -= 1
            P.dma("pool", lambda e, d_=d_, s_=s_: e.dma_start(out=d_, in_=s_), reads=[], writes=["Wb"], bg=(pre_pos[0] > n_ffn_pre))

    g_base = A.mark()
    identb = sb("identb", [128, 128], BF16)
    identf = sb("identf", [128, 128], F32)
    ones = sb("ones", [128, 128], F32)
    zeros_bf = sb("zeros", [128, 8], BF16)
    RW = sb("RW", [128, 32, NEXP], F32)
    SEL = sb("SEL", [128, 32, NEXP], F32)
    OH1 = sb("OH1", [128, 32, NEXP], F32)
    dma("sp", identb[:], k_identb[:], [], [identb.k])
    dma("sp", identf[:], k_identf[:], [], [identf.k])
    I("dve", "memset", ones[:], 1.0, writes=[ones.k])
    I("dve", "memset", zeros_bf[:], 0.0, writes=[zeros_bf.k])
    phase_base = A.mark()

    def sqrt_recip(dst, src, scale, nparts=128):
        I("act", "activation", out=dst[:], in_=src[:], func=AF.Sqrt, bias=epsc[:], scale=scale,
             reads=[src.k, epsc.k], writes=[dst.k])
        I("dve", "reciprocal", out=dst[:], in_=dst[:], reads=[dst.k], writes=[dst.k])

    epsc = sb("epsc", [128, 1], F32)
    I("dve", "memset", epsc[:], EPS, writes=[epsc.k])
    phase_base = A.mark()

    def chk(l_, ph):
        if stop is not None and stop == (l_, ph):
            raise _Stop()

    def moe_sorted(l):
        A.reset(phase_base)
        precast_some(10 ** 6)
        P.barrier(include_bg=True)
        tab2 = sb("tab2", [128, 2, 128], F32)
        dma("sp", tab2[:], k_tab2[:], [], [tab2.k])
        ACCa = sb("ACCa", [128, 33, NEXP], F32)
        I("dve", "memset", ACCa[:, 0, :], 0.0, writes=[ACCa.k])
        for t in range(32):
            I("dve", "tensor_tensor", out=ACCa[:, t + 1, :], in0=ACCa[:, t, :], in1=SEL[:, t, :], op=ALU.add, reads=[ACCa.k, SEL.k], writes=[ACCa.k])
        pc_, pr_ = PS[0], PS[1]
        I("pe", "matmul", pc_[:, 0:NEXP], lhsT=ones[:], rhs=ACCa[:, 32, :], start=True, stop=True, reads=[ones.k, ACCa.k], writes=[pc_.k])
        sm = sb("sm", [128, 8, NEXP], F32)
        I("dve", "tensor_copy", out=sm[:, 0, :], in_=pc_[:, 0:NEXP], reads=[pc_.k], writes=[sm.k])
        I("dve", "tensor_scalar", out=sm[:, 1, :], in0=sm[:, 0, :], scalar1=0.5, scalar2=None, op0=ALU.is_gt, reads=[sm.k], writes=[sm.k])
        for j in range(1, 8):
            I("dve", "scalar_tensor_tensor", out=sm[:, 1, :], in0=sm[:, 0, :], scalar=UNIT * j + 0.5, in1=sm[:, 1, :], op0=ALU.is_gt, op1=ALU.add,
              reads=[sm.k], writes=[sm.k])
        I("dve", "tensor_copy", out=sm[:, 2, 0:1], in_=sm[:, 1, 0:1], reads=[sm.k], writes=[sm.k])
        for e_ in range(1, NEXP):
            I("dve", "tensor_tensor", out=sm[:, 2, e_:e_ + 1], in0=sm[:, 2, e_ - 1:e_], in1=sm[:, 1, e_:e_ + 1], op=ALU.add, reads=[sm.k], writes=[sm.k])
        I("dve", "tensor_tensor", out=sm[:, 3, :], in0=sm[:, 2, :], in1=sm[:, 1, :], op=ALU.subtract, reads=[sm.k], writes=[sm.k])
        I("dve", "tensor_scalar", out=sm[:, 4, :], in0=sm[:, 3, :], scalar1=UNIT / 128.0, scalar2=None, op0=ALU.mult, reads=[sm.k], writes=[sm.k])
        I("dve", "tensor_scalar", out=sm[:, 2, :], in0=sm[:, 2, :], scalar1=float(UNIT), scalar2=None, op0=ALU.mult, reads=[sm.k], writes=[sm.k])
        for t in range(32):
            cs = slice(t * NEXP, (t + 1) * NEXP)
            I("pe", "matmul", pr_[:, cs], lhsT=ones[:], rhs=ACCa[:, t, :], start=True, stop=False, reads=[ones.k, ACCa.k], writes=[pr_.k])
            I("pe", "matmul", pr_[:, cs], lhsT=ones[:], rhs=sm[:, 4, :], start=False, stop=False, reads=[ones.k, sm.k], writes=[pr_.k])
            I("pe", "matmul", pr_[:, cs], lhsT=tab2[:, 0, :], rhs=SEL[:, t, :], start=False, stop=True, reads=[tab2.k, SEL.k], writes=[pr_.k])
        DST = sb("DST", [128, 32, NEXP], F32)
        I("dve", "tensor_tensor", out=DST[:].rearrange("p t e -> p (t e)"), in0=pr_[:, 0:256], in1=SEL[:].rearrange("p t e -> p (t e)"), op=ALU.subtract,
          reads=[pr_.k, SEL.k], writes=[DST.k])
        OH2 = sb("OH2", [128, 32, NEXP], F32)
        I("dve", "tensor_tensor", out=OH2[:], in0=SEL[:], in1=OH1[:], op=ALU.subtract, reads=[SEL.k, OH1.k], writes=[OH2.k])
        tmpr = sb("tmpr", [128, 32, NEXP], F32)
        IDXf = sb("IDXf", [128, 2, 32], F32)
        WGT = sb("WGT", [128, 2, 32], F32)
        IDXi = sb("IDXi", [128, 2, 32], I32)
        for k_, oh in enumerate((OH1, OH2)):
            I("dve", "tensor_tensor", out=tmpr[:], in0=oh[:], in1=DST[:], op=ALU.mult, reads=[oh.k, DST.k], writes=[tmpr.k])
            I("dve", "tensor_reduce", out=IDXf[:, k_, :], in_=tmpr[:], axis=AX.X, op=ALU.add, reads=[tmpr.k], writes=[IDXf.k])
            I("dve", "tensor_tensor", out=tmpr[:], in0=oh[:], in1=RW[:], op=ALU.mult, reads=[oh.k, RW.k], writes=[tmpr.k])
            I("dve", "tensor_reduce", out=WGT[:, k_, :], in_=tmpr[:], axis=AX.X, op=ALU.add, reads=[tmpr.k], writes=[WGT.k])
        I("dve", "tensor_copy", out=IDXi[:], in_=IDXf[:], reads=[IDXf.k], writes=[IDXi.k])
        eu = sb("eu", [128, NUN], F32)
        pq = sb("pq", [128, 4], F32)
        for q in range(4):
            I("dve", "tensor_scalar", out=pq[:, q:q + 1], in0=tab2[:, 1, 0:1], scalar1=128.0 * q, scalar2=None, op0=ALU.add, reads=[tab2.k], writes=[pq.k])
        for u in range(NUN):
            I("dve", "tensor_scalar", out=sm[:, 5, :], in0=sm[:, 2, :], scalar1=float(UNIT) * u + 0.5, scalar2=None, op0=ALU.is_lt, reads=[sm.k], writes=[sm.k])
            I("dve", "tensor_reduce", out=eu[:, u:u + 1], in_=sm[:, 5, :], axis=AX.X, op=ALU.add, reads=[sm.k], writes=[eu.k])
        I("dve", "tensor_scalar", out=eu[:], in0=eu[:], scalar1=7.0, scalar2=None, op0=ALU.min, reads=[eu.k], writes=[eu.k])
        WIf = sb("WIf", [128, NUN, 4], F32)
        WIi = sb("WIi", [128, NUN, 4], I32)
        for q in range(4):
            I("dve", "tensor_scalar", out=WIf[:, :, q], in0=eu[:], scalar1=512.0, scalar2=pq[:, q:q + 1], op0=ALU.mult, op1=ALU.add,
              reads=[eu.k, pq.k], writes=[WIf.k])
        I("dve", "tensor_copy", out=WIi[:], in_=WIf[:], reads=[WIf.k], writes=[WIi.k])
        if debug:
            dbg_idx = nc.dram_tensor("dbg_idx", [128, 64], F32, kind="ExternalOutput").ap()
            dbg_w = nc.dram_tensor("dbg_w", [128, 64], F32, kind="ExternalOutput").ap()
            dbg_e = nc.dram_tensor("dbg_e", [128, NUN * 5], F32, kind="ExternalOutput").ap()
            dma("sp", dbg_idx[:], IDXf[:].rearrange("p k t -> p (k t)"), [IDXf.k], ["dbg1"])
            dma("sp", dbg_w[:], WGT[:].rearrange("p k t -> p (k t)"), [WGT.k], ["dbg2"])
            dma("sp", dbg_e[:, 0:NUN], eu[:], [eu.k], ["dbg3"])
            dma("sp", dbg_e[:, NUN:NUN * 5], WIf[:].rearrange("p u q -> p (u q)"), [WIf.k], ["dbg3"])
        gfl = sb("gfl", [128, D], F32)
        dma("sp", gfl[:], MODB[2 * l + 0, :, 5 * D:6 * D], ["MODB"], [gfl.k])
        persist = A.mark()
        import concourse.bass as _b
        H2bs = [sb("H2b%d" % i, [128, 8, UNIT], BF16) for i in range(2)]
        NWS = 3
        Wset = [(sb("W1q%d" % i, [128, 8, 896], BF16), sb("W3q%d" % i, [128, 8, 896], BF16), sb("W2q%d" % i, [128, 7, D], BF16)) for i in range(NWS)]
        actq = [sb("actq%d" % i, [128, 7, UNIT], BF16) for i in range(2)]
        yacc = sb("yacc", [128, UNIT // 128, D], F32)
        slt = [sb("slt%d" % i, [128, 512], F32) for i in range(4)]
        hst = [sb("hst%d" % i, [128, D], BF16) for i in range(2)]

        def L13(s_):
            w1q, w3q, _ = Wset[s_ % NWS]
            u, q = s_ // 4, s_ % 4
            for wt, src in ((w1q, MW1q), (w3q, MW3q)):
                P.dma("pool", lambda e, wt=wt, src=src, u=u, q=q: e.indirect_dma_start(
                    out=wt[:].rearrange("p k n -> p (k n)"), out_offset=None, in_=src[:, :],
                    in_offset=_b.IndirectOffsetOnAxis(ap=WIi[:, u, q:q + 1], axis=0)), reads=["Wb", WIi.k], writes=[wt.k])

        def L2(s_):
            w2q = Wset[s_ % NWS][2]
            u, q = s_ // 4, s_ % 4
            P.dma("pool", lambda e, w2q=w2q, u=u, q=q: e.indirect_dma_start(
                out=w2q[:].rearrange("p k n -> p (k n)"), out_offset=None, in_=MW2q[:, :],
                in_offset=_b.IndirectOffsetOnAxis(ap=WIi[:, u, q:q + 1], axis=0)), reads=["Wb", WIi.k], writes=[w2q.k])

        for s_ in range(NWS):
            L13(s_)
            L2(s_)
        dbufs = A.mark()
        htl = [sb("htl%d" % i, [128, D], BF16) for i in range(3)]
        for t in range(32):
            ht_ = htl[t % 3]
            dma("sp", ht_[:], H2T[t * 128:(t + 1) * 128, :], ["H2T"], [ht_.k])
            for k_ in range(2):
                P.dma("pool", lambda e, ht_=ht_, k_=k_, t=t: e.indirect_dma_start(
                    out=HS[:, :], out_offset=_b.IndirectOffsetOnAxis(ap=IDXi[:, k_, t:t + 1], axis=0), in_=ht_[:, :], in_offset=None),
                    reads=[ht_.k, IDXi.k], writes=["HS"])
        A.reset(dbufs)
        P.barrier()
        wkeep = [t.k for ws in Wset for t in ws]
        NU = NUN * 4
        cntd = {"ps": 0, "py": 0, "h": 0}

        def prep_unit(u):
            H2b = H2bs[u % 2]
            for tt in range(UNIT // 128):
                cntd["h"] += 1
                h_ = hst[cntd["h"] % 2]
                r0 = u * UNIT + tt * 128
                dma("sp", h_[:], HS[r0:r0 + 128, :], ["HS"], [h_.k])
                pst = PS[6 + cntd["h"] % 2]
                for kc in range(8):
                    I("pe", "transpose", out=pst[:].bitcast(BF16)[:, kc * 128:(kc + 1) * 128], in_=h_[:, kc * 128:(kc + 1) * 128], identity=identb[:],
                      reads=[h_.k, identb.k], writes=[pst.k])
                I("act", "activation", out=H2b[:, :, tt * 128:(tt + 1) * 128], in_=pst[:].bitcast(BF16)[:, 0:1024].rearrange("p (k t) -> p k t", k=8),
                  func=AF.Copy, reads=[pst.k], writes=[H2b.k])

        def S1(s_):
            w1q, w3q, _ = Wset[s_ % NWS]
            aq = actq[s_ % 2]
            u, q = s_ // 4, s_ % 4
            H2b = H2bs[u % 2]
            if s_ == 0:
                prep_unit(0)
            if q == 3 and u + 1 < NUN:
                prep_unit(u + 1)
            for j in range(7):
                for p0 in range(0, UNIT, 512):
                    cntd["ps"] += 1
                    n_ = cntd["ps"]
                    pa, pb = PS[(n_ % 2) * 2], PS[(n_ % 2) * 2 + 1]
                    for kc in range(8):
                        I("pe", "matmul", pa[:], lhsT=w1q[:, kc, j * 128:(j + 1) * 128], rhs=H2b[:, kc, p0:p0 + 512], start=(kc == 0), stop=(kc == 7),
                          reads=[w1q.k, H2b.k], writes=[pa.k])
                    for kc in range(8):
                        I("pe", "matmul", pb[:], lhsT=w3q[:, kc, j * 128:(j + 1) * 128], rhs=H2b[:, kc, p0:p0 + 512], start=(kc == 0), stop=(kc == 7),
                          reads=[w3q.k, H2b.k], writes=[pb.k])
                    sl_ = slt[n_ % len(slt)]
                    I("act", "activation", out=sl_[:], in_=pa[:], func=AF.Silu, reads=[pa.k], writes=[sl_.k])
                    I("dve", "tensor_tensor", out=aq[:, j, p0:p0 + 512], in0=sl_[:], in1=pb[:], op=ALU.mult, reads=[sl_.k, pb.k], writes=[aq.k + "_%d" % j])

        def S2(s_):
            w2q = Wset[s_ % NWS][2]
            aq = actq[s_ % 2]
            u, q = s_ // 4, s_ % 4
            for tt in range(UNIT // 128):
                ts_ = slice(tt * 128, (tt + 1) * 128)
                yk0 = yacc.k + "_%d" % tt
                for half in range(2):
                    yk = yk0 + "h%d" % half
                    cs = slice(half * 512, (half + 1) * 512)
                    cntd["py"] += 1
                    py = PS[4 + cntd["py"] % 2]
                    for j in range(7):
                        I("pe", "matmul", py[:], lhsT=aq[:, j, ts_], rhs=w2q[:, j, cs], start=(j == 0), stop=(j == 6), reads=[aq.k + "_%d" % j, w2q.k], writes=[py.k])
                    if q == 0:
                        I("act", "activation", out=yacc[:, tt, cs], in_=py[:], func=AF.Copy, reads=[py.k], writes=[yk])
                    else:
                        I("dve", "tensor_tensor", out=yacc[:, tt, cs], in0=py[:], in1=yacc[:, tt, cs], op=ALU.add, reads=[py.k, yk], writes=[yk])
                if q == 3:
                    r0 = u * UNIT + tt * 128
                    I("dve", "tensor_tensor", out=yacc[:, tt, :], in0=yacc[:, tt, :], in1=gfl[:], op=ALU.mult, reads=[yk0 + "h0", yk0 + "h1", gfl.k],
                      writes=[yk0 + "h0", yk0 + "h1"])
                    dma("sp", YS[r0:r0 + 128, :], yacc[:, tt, :], [yk0 + "h0", yk0 + "h1"], ["YS"])

        for s_ in range(NU):
            if s_ >= 1 and s_ + NWS - 1 < NU:
                L13(s_ + NWS - 1)
            S1(s_)
            if s_ >= 1:
                S2(s_ - 1)
                if s_ + NWS - 1 < NU:
                    L2(s_ + NWS - 1)
        S2(NU - 1)
        A.reset(persist)
        P.barrier()
        gfin = sb("gfin", [128, D], F32)
        dma("sp", gfin[:], g_final.rearrange("(o n) -> o n", o=1).broadcast_to([128, D]), [], [gfin.k])
        g1 = [sb("g1_%d" % i, [128, D], F32) for i in range(3)]
        g2 = [sb("g2_%d" % i, [128, D], F32) for i in range(3)]
        x1s = [sb("x1_%d" % i, [128, D], F32) for i in range(3)]
        junk = sb("junk", [128, D], F32)
        ssqs = [sb("ssq%d" % i, [128, 1], F32) for i in range(2)]
        rstds = [sb("rstd%d" % i, [128, 1], F32) for i in range(2)]
        for t in range(32):
            i2 = t % 2
            r0 = NCTX + t * 128
            x1, ssq, rstd, ga, gb_ = x1s[t % 3], ssqs[i2], rstds[i2], g1[t % 3], g2[t % 3]
            for k_, gt in enumerate((ga, gb_)):
                P.dma("pool", lambda e, gt=gt, k_=k_, t=t: e.indirect_dma_start(
                    out=gt[:, :], out_offset=None, in_=YS[:, :], in_offset=_b.IndirectOffsetOnAxis(ap=IDXi[:, k_, t:t + 1], axis=0)),
                    reads=["YS", IDXi.k], writes=[gt.k])
            dma("sp", x1[:], X1[r0:r0 + 128, :], ["X1"], [x1.k])
            I("act", "activation", out=ga[:], in_=ga[:], func=AF.Identity, scale=WGT[:, 0, t:t + 1], reads=[ga.k, WGT.k], writes=[ga.k])
            I("dve", "scalar_tensor_tensor", out=ga[:], in0=gb_[:], scalar=WGT[:, 1, t:t + 1], in1=ga[:], op0=ALU.mult, op1=ALU.add,
              reads=[gb_.k, WGT.k, ga.k], writes=[ga.k])
            I("dve", "tensor_tensor", out=x1[:], in0=x1[:], in1=ga[:], op=ALU.add, reads=[x1.k, ga.k], writes=[x1.k])
            I("act", "activation", out=junk[:], in_=x1[:], func=AF.Square, accum_out=ssq[:], reads=[x1.k], writes=[junk.k, ssq.k])
            sqrt_recip(rstd, ssq, 1.0 / D)
            I("dve", "scalar_tensor_tensor", out=x1[:], in0=x1[:], scalar=rstd[:], in1=gfin[:], op0=ALU.mult, op1=ALU.mult,
              reads=[x1.k, rstd.k, gfin.k], writes=[x1.k])
            dma("sp", out[r0 - NCTX:r0 - NCTX + 128, :], x1[:], [x1.k], ["OUT"])

    try:
      for l in range(n_layers):
        last = l == n_layers - 1 and n_layers == 2
        xsrc = xin if l == 0 else XS
        A.reset(phase_base)
        P.barrier()
        Win = sb("Win", [128, 8, 4096], BF16)
        for kc in range(8):
            for hh in range(2):
                cdma(Win[:, kc, hh * 2048:(hh + 1) * 2048], w_in[l, kc * 128:(kc + 1) * 128, hh * 2048:(hh + 1) * 2048], [], [Win.k])
        if l == 0:
            precast_some(n_ffn_pre)
        win_base = A.mark()
        cc_t = sb("cc", [128, 2, 8], F32)
        dma("sp", cc_t[:, 0, :], c_b.rearrange("(k p) -> p k", p=128), [], [cc_t.k])
        dma("sp", cc_t[:, 1, :], c_ctx.rearrange("(k p) -> p k", p=128), [], [cc_t.k])
        sc_t = sb("sc", [128, 2, 8], F32)
        I("act", "activation", out=sc_t[:], in_=cc_t[:], func=AF.Silu, reads=[cc_t.k], writes=[sc_t.k])
        rep = sb("rep", [128, 2, 8, 128], F32)
        for r in range(2):
            for kc in range(8):
                I("dve", "tensor_scalar", out=rep[:, r, kc, :], in0=ones[:], scalar1=sc_t[:, r, kc:kc + 1],
                                                                scalar2=None, op0=ALU.mult,
                     reads=[ones.k, sc_t.k], writes=[rep.k])
        bm = sb("bm", [128, 6 * D], F32)
        dma("sp", bm[:], b_mod[l:l + 1, :].broadcast_to([128, 6 * D]), [], [bm.k])
        gmx = sb("gmx", [128, D], F32)
        gff = sb("gff", [128, D], F32)
        dma("sp", gmx[:], g_mix[l:l + 1, :].broadcast_to([128, D]), [], [gmx.k])
        dma("sp", gff[:], g_ffn[l:l + 1, :].broadcast_to([128, D]), [], [gff.k])
        modt = [sb("modt%d" % r, [128, 6 * D], F32) for r in range(2)]
        wms = [sb("wm%d" % i, [128, 8, 512], F32) for i in range(2)]
        for cc in range(12):
            wm = wms[cc % 2]
            dma("sp", wm[:], w_mod[l, :, cc * 512:(cc + 1) * 512].rearrange("(k p) n -> p k n", p=128), [], [wm.k])
            for r in range(2):
                ps = PS[(cc * 2 + r) % 4]
                for kc in range(8):
                    I("pe", "matmul", ps[:], lhsT=rep[:, r, kc, :], rhs=wm[:, kc, :],
                                                                           start=(kc == 0), stop=(kc == 7),
                         reads=[rep.k, wm.k], writes=[ps.k])
                I("dve", "tensor_tensor", out=modt[r][:, cc * 512:(cc + 1) * 512], in0=ps[:],
                                                                       in1=bm[:, cc * 512:(cc + 1) * 512], op=ALU.add,
                     reads=[ps.k, bm.k], writes=[modt[r].k])
        for r in range(2):
            I("dve", "scalar_tensor_tensor", out=modt[r][:, D:2 * D], in0=modt[r][:, D:2 * D], scalar=1.0, in1=gmx[:],
                                                             op0=ALU.add, op1=ALU.mult,
                 reads=[modt[r].k, gmx.k], writes=[modt[r].k])
            I("dve", "scalar_tensor_tensor", out=modt[r][:, 4 * D:5 * D], in0=modt[r][:, 4 * D:5 * D], scalar=1.0,
                                                             in1=gff[:], op0=ALU.add, op1=ALU.mult,
                 reads=[modt[r].k, gff.k], writes=[modt[r].k])
            dma("sp", MODB[2 * l + r], modt[r][:], [modt[r].k], ["MODB"])

        def load_mod(slot, name):
            ts = []
            for r in range(2):
                t = sb("%s%d" % (name, r), [128, D], F32)
                dma("sp", t[:], MODB[2 * l + r, :, slot * D:(slot + 1) * D], ["MODB"], [t.k])
                ts.append(t)
            return ts

        def modsel(ts, g0):
            return ts[1] if g0 < NCTX else ts[0]

        chk(l, "M")
        A.reset(win_base)
        P.barrier()
        P.lastw[Win.k] = None
        if l == 1:
            HSv = HS.rearrange("(p r) n -> p (r n)", p=128)
            for zi in range(NROWS // 128 * D // 4096):
                dma("sp", HSv[:, zi * 4096:(zi + 1) * 4096], k_zero[:, :], [], ["HS"])
        G1 = load_mod(1, "G1")
        SH1 = load_mod(0, "SH1")
        for (c0, w) in ((0, 2), (258, 3), (UW - 3, 3)):
            for ch in range(4):
                dma("sp", U[ch * 128:(ch + 1) * 128, c0:c0 + w], zeros_bf[:, 0:w], [zeros_bf.k], ["U"])
        xts = [sb("xt%d" % i, [128, D], F32) for i in range(8)]
        junk = sb("junk", [128, D], F32)
        t1s = [sb("t1_%d" % i, [128, D], F32) for i in range(2)]
        hts = [sb("ht%d" % i, [128, D], BF16) for i in range(2)]
        ssqs = [sb("ssq%d" % i, [128, 1], F32) for i in range(2)]
        rstds = [sb("rstd%d" % i, [128, 1], F32) for i in range(2)]
        hfms = [sb("hfm%d" % i, [128, 8, 512], BF16) for i in range(2)]
        cos_t = [sb("cos%d" % i, [128, 512], F32) for i in range(2)]
        sin_t = [sb("sin%d" % i, [128, 512], F32) for i in range(2)]
        ust = [sb("ust%d" % i, [128, 512], BF16) for i in range(6)]
        qks = [sb("qks%d" % i, [128, 4, 512], BF16) for i in range(2)]
        ra = [sb("ra%d" % i, [128, 512], F32) for i in range(2)]
        rb = [sb("rb%d" % i, [128, 512], F32) for i in range(2)]
        tok_st = [sb("tokst%d" % i, [128, 512], BF16) for i in range(6)]
        cnt = {"x": 0, "u": 0, "r": 0, "ps": 0, "tk": 0}

        def a_loads(mi):
            g0, T_ = MACROS[mi]
            ct, stb = cos_t[mi % 2], sin_t[mi % 2]
            for tt in range(T_ // 128):
                xt = xts[(mi % 2) * 4 + tt]
                r0 = g0 + tt * 128
                precast_some(4)
                dma("sp", xt[:], xsrc[r0:r0 + 128, :], ["XS"], [xt.k])
            dma("sp", ct[:, 0:T_], k_cos[:, g0:g0 + T_], [], [ct.k])
            dma("sp", stb[:, 0:T_], k_sin[:, g0:g0 + T_], [], [stb.k])

        def a_chain(mi, tt):
            g0, T_ = MACROS[mi]
            g1, sh1 = modsel(G1, g0), modsel(SH1, g0)
            i = (mi * 4 + tt) % 2
            xt = xts[(mi % 2) * 4 + tt]
            t1, ht, ssq, rstd = t1s[i], hts[i], ssqs[i], rstds[i]
            I("act", "activation", out=junk[:], in_=xt[:], func=AF.Square, accum_out=ssq[:], reads=[xt.k], writes=[junk.k, ssq.k])
            sqrt_recip(rstd, ssq, 1.0 / D)
            I("dve", "scalar_tensor_tensor", out=t1[:], in0=xt[:], scalar=rstd[:], in1=g1[:], op0=ALU.mult, op1=ALU.mult,
              reads=[xt.k, rstd.k, g1.k], writes=[t1.k])
            I("dve", "tensor_tensor", out=ht[:], in0=t1[:], in1=sh1[:], op=ALU.add, reads=[t1.k, sh1.k], writes=[ht.k])

        def a_xpose(mi, tt):
            i = (mi * 4 + tt) % 2
            ht = hts[i]
            hfm = hfms[mi % 2]
            pst = PS[6 + i]
            for kc in range(8):
                I("pe", "transpose", out=pst[:].bitcast(BF16)[:, kc * 128:(kc + 1) * 128], in_=ht[:, kc * 128:(kc + 1) * 128], identity=identb[:],
                  reads=[ht.k, identb.k], writes=[pst.k])
            I("act", "activation", out=hfm[:, :, tt * 128:(tt + 1) * 128], in_=pst[:].bitcast(BF16)[:, 0:1024].rearrange("p (k t) -> p k t", k=8),
              func=AF.Copy, reads=[pst.k], writes=[hfm.k])

        def nps():
            cnt["ps"] += 1
            return PS[cnt["ps"] % 4]

        def a_section(mi, sec):
            g0, T_ = MACROS[mi]
            hfm = hfms[mi % 2]
            ct, stb = cos_t[mi % 2], sin_t[mi % 2]

            def proj_fm(oc, ps):
                for kc in range(8):
                    I("pe", "matmul", ps[:, 0:T_], lhsT=Win[:, kc, oc * 128:(oc + 1) * 128], rhs=hfm[:, kc, 0:T_], start=(kc == 0), stop=(kc == 7),
                      reads=[Win.k, hfm.k], writes=[ps.k])

            if sec == 0:
                for oc in range(8):
                    ps = nps()
                    proj_fm(oc, ps)
                    u_ = ust[cnt["u"] % 6]
                    cnt["u"] += 1
                    fn = AF.Copy if oc < 4 else AF.Gelu
                    I("act", "activation", out=u_[:, 0:T_], in_=ps[:, 0:T_], func=fn, reads=[ps.k], writes=[u_.k])
                    if oc < 4:
                        dma("sp", U[oc * 128:(oc + 1) * 128, ucol(g0):ucol(g0) + T_], u_[:, 0:T_], [u_.k], ["U"])
                    else:
                        dma("sp", GY[(oc - 4) * 128:(oc - 3) * 128, g0:g0 + T_], u_[:, 0:T_], [u_.k], ["GY"])
            elif sec in (1, 2):
                qk = sec - 1
                st = qks[qk]
                for h in range(4):
                    psq = nps()
                    proj_fm(8 + qk * 4 + h, psq)
                    pss = nps()
                    proj_fm(24 + qk * 4 + h, pss)
                    a_, b_ = ra[cnt["r"] % 2], rb[cnt["r"] % 2]
                    cnt["r"] += 1
                    sc = 1.0 if qk == 0 else 128.0 ** -0.5
                    I("dve", "scalar_tensor_tensor", out=a_[:, 0:T_], in0=psq[:, 0:T_], scalar=sc, in1=ct[:, 0:T_], op0=ALU.mult, op1=ALU.mult,
                      reads=[psq.k, ct.k], writes=[a_.k])
                    I("dve", "scalar_tensor_tensor", out=b_[:, 0:T_], in0=pss[:, 0:T_], scalar=sc, in1=stb[:, 0:T_], op0=ALU.mult, op1=ALU.mult,
                      reads=[pss.k, stb.k], writes=[b_.k])
                    I("dve", "tensor_tensor", out=st[:, h, 0:T_], in0=a_[:, 0:T_], in1=b_[:, 0:T_], op=ALU.add, reads=[a_.k, b_.k], writes=[st.k + "_%d" % h])
                dst = Q if qk == 0 else K
                dma("sp", dst[:, g0:g0 + T_].rearrange("(h d) t -> d h t", d=128), st[:, :, 0:T_], [st.k + "_%d" % h_ for h_ in range(4)], ["Q" if qk == 0 else "K"])
            else:
                kst = qks[1]
                for tt in range(T_ // 128):
                    r0 = g0 + tt * 128
                    pst = PS[4]
                    for h in range(4):
                        I("pe", "transpose", out=pst[:].bitcast(BF16)[:, h * 128:(h + 1) * 128], in_=kst[:, h, tt * 128:(tt + 1) * 128], identity=identb[:],
                          reads=[kst.k + "_%d" % h, identb.k], writes=[pst.k])
                    tk = tok_st[cnt["tk"] % 6]
                    cnt["tk"] += 1
                    I("dve", "tensor_copy", out=tk[:], in_=pst[:].bitcast(BF16)[:, 0:512], reads=[pst.k], writes=[tk.k])
                    dma("sp", KT[r0:r0 + 128, :], tk[:], [tk.k], ["KT"])
                    for which in range(2):
                        ps = PS[5] if which == 0 else nps()
                        c0 = 2048 + which * 512
                        for kc in range(8):
                            I("pe", "matmul", ps[:], lhsT=hfm[:, kc, tt * 128:(tt + 1) * 128], rhs=Win[:, kc, c0:c0 + 512], start=(kc == 0), stop=(kc == 7),
                              reads=[hfm.k, Win.k], writes=[ps.k])
                        tk = tok_st[cnt["tk"] % 6]
                        cnt["tk"] += 1
                        fn = AF.Copy if which == 0 else AF.Silu
                        I("act", "activation", out=tk[:], in_=ps[:], func=fn, reads=[ps.k], writes=[tk.k])
                        dma("sp", (VT if which == 0 else SGT)[r0:r0 + 128, :], tk[:], [tk.k], ["VT" if which == 0 else "SGT"])

        NM = len(MACROS)
        a_loads(0)
        a_loads(1)
        for tt in range(MACROS[0][1] // 128):
            a_chain(0, tt)
            a_xpose(0, tt)
        for mi in range(NM):
            nxt = mi + 1 if mi + 1 < NM else None
            ntn = (MACROS[nxt][1] // 128) if nxt is not None else 0
            for sec in range(4):
                if nxt is not None and sec < ntn:
                    a_chain(nxt, sec)
                a_section(mi, sec)
                if nxt is not None and sec < ntn:
                    a_xpose(nxt, sec)
            if mi + 2 < NM:
                a_loads(mi + 2)

        A.reset(phase_base)
        P.barrier()
        tab = sb("tab", [128, 8, 128], F32)
        dma("sp", tab[:], k_tab[:], [], [tab.k])
        rd = sb("rd", [128, 8], F32)
        dma("sp", rd[:], ret_decay[l:l + 1, :].broadcast_to([128, 8]), [], [rd.k])
        lg = sb("lg", [128, 8], F32)
        I("act", "activation", out=lg[:], in_=rd[:], func=AF.Exp, scale=-1.0, reads=[rd.k], writes=[lg.k])
        I("act", "activation", out=lg[:], in_=lg[:], func=AF.Ln, bias=ones[:, 0:1], reads=[lg.k, ones.k], writes=[lg.k])
        I("dve", "tensor_scalar", out=lg[:], in0=lg[:], scalar1=-1.0, scalar2=None, op0=ALU.mult, reads=[lg.k], writes=[lg.k])
        kd = sb("kd", [128, 8], F32)
        cd = sb("cd", [128, 8], F32)
        for dr in range(2):
            for h in range(4):
                j = dr * 4 + h
                I("act", "activation", out=kd[:, j:j + 1], in_=tab[:, 6 + dr, 0:1], func=AF.Exp, scale=lg[:, j:j + 1],
                     reads=[tab.k, lg.k], writes=[kd.k])
        I("act", "activation", out=cd[:], in_=lg[:], func=AF.Exp, scale=128.0, reads=[lg.k], writes=[cd.k])
        KDT = [sb("KDT%d" % i, [128, 512], F32) for i in range(2)]
        for dr in range(2):
            for h in range(4):
                I("dve", "tensor_scalar", out=KDT[dr][:, h * 128:(h + 1) * 128], in0=ones[:], scalar1=kd[:, dr * 4 + h:dr * 4 + h + 1], scalar2=None,
                  op0=ALU.mult, reads=[ones.k, kd.k], writes=[KDT[dr].k])
        Sd = [sb("S%d" % i, [128, 512], F32) for i in range(2)]
        Sb16 = [sb("Sb16_%d" % i, [128, 512], BF16) for i in range(4)]
        ktl = [sb("ktl%d" % i, [128, 512], BF16) for i in range(8)]
        vtl = [sb("vtl%d" % i, [128, 512], BF16) for i in range(8)]
        kts = [sb("kts%d" % i, [128, 512], BF16) for i in range(4)]
        orders = [list(range(NCH)), [1, 0] + list(range(NCH - 1, 1, -1))]
        for dr in range(2):
            I("dve", "memset", Sd[dr][:], 0.0, writes=[Sd[dr].k + "_%d" % h_ for h_ in range(4)])

        def a2_loads(step):
            for dr in range(2):
                c = orders[dr][step]
                i8 = (step % 4) * 2 + dr
                dma("sp", ktl[i8][:], KT[c * 128:(c + 1) * 128, :], ["KT"], [ktl[i8].k])
                dma("sp", vtl[i8][:], VT[c * 128:(c + 1) * 128, :], ["VT"], [vtl[i8].k])

        for step in range(3):
            a2_loads(step)
        a2n = [0, 0]

        def a2_step(step):
            if step + 3 < NCH:
                a2_loads(step + 3)
            for dr in range(2):
                n = a2n[0] = a2n[0] + 1
                c = orders[dr][step]
                S = Sd[dr]
                dst = SF if dr == 0 else SB
                i = (step % 2) * 2 + dr
                i8 = (step % 4) * 2 + dr
                s16, kt_, vt_, ks_ = Sb16[i], ktl[i8], vtl[i8], kts[i]
                I("act", "activation", out=s16[:], in_=S[:], func=AF.Copy, reads=[S.k + "_%d" % h_ for h_ in range(4)], writes=[s16.k])
                dma("sp", dst[c], s16[:], [s16.k], ["SF" if dr == 0 else "SB"])
                ps = PS[6 + n % 2]
                I("dve", "tensor_tensor", out=ks_[:], in0=kt_[:], in1=KDT[dr][:], op=ALU.mult, reads=[kt_.k, KDT[dr].k], writes=[ks_.k])
                for h in range(4):
                    I("pe", "matmul", ps[:, h * 128:(h + 1) * 128], lhsT=ks_[:, h * 128:(h + 1) * 128], rhs=vt_[:, h * 128:(h + 1) * 128],
                      start=True, stop=True, reads=[ks_.k, vt_.k], writes=[ps.k])
                for h in range(4):
                    j = dr * 4 + h
                    I("dve", "scalar_tensor_tensor", out=S[:, h * 128:(h + 1) * 128], in0=S[:, h * 128:(h + 1) * 128], scalar=cd[:, j:j + 1],
                      in1=ps[:, h * 128:(h + 1) * 128], op0=ALU.mult, op1=ALU.add, reads=[S.k + "_%d" % h, cd.k, ps.k], writes=[S.k + "_%d" % h])

        def a2_advance(k):
            while k > 0 and a2n[1] < NCH:
                a2_step(a2n[1])
                a2n[1] += 1
                k -= 1

        cw = sb("cw", [128, 4, 4], F32)
        dma("sp", cw[:], conv_w[l].rearrange("k (c p) -> p k c", p=128), [], [cw.k])
        cb = sb("cb", [128, 4], F32)
        dma("sp", cb[:], conv_b[l].rearrange("(c p) -> p c", p=128), [], [cb.k])
        gb = sb("gb", [128, 3, 2, 4], F32)
        for wi, src in enumerate((rg_ba, rg_bx, rg_lam)):
            dma("sp", gb[:, wi], src[l].rearrange("d (c p) -> p d c", p=128), [], [gb.k])
        cv = sb("cv", [128, 2, 4], F32)
        I("act", "activation", out=cv[:], in_=gb[:, 2], func=AF.Exp, scale=-1.0, reads=[gb.k], writes=[cv.k])
        I("act", "activation", out=cv[:], in_=cv[:], func=AF.Ln, bias=ones[:, 0:1], reads=[cv.k, ones.k], writes=[cv.k])
        I("dve", "tensor_scalar", out=cv[:], in0=cv[:], scalar1=-8.0, scalar2=None, op0=ALU.mult, reads=[cv.k], writes=[cv.k])
        Up = sb("Up", [128, UW], BF16)
        gyt = sb("gyt", [128, NT], BF16)
        uc32 = sb("uc32", [128, NT], F32)
        ucb = sb("ucb", [128, NT], BF16)
        AB = [[sb("A%d" % d_, [128, NT], F32), sb("B%d" % d_, [128, NT], F32)] for d_ in range(2)]
        Hd = [sb("H%d" % d_, [128, NT], F32) for d_ in range(2)]
        dg = [sb("dg%d" % k_, [128, 128], BF16) for k_ in range(4)]
        bd = [[sb("bd%d%d" % (d_, w_), [128, 128], BF16) for w_ in range(2)] for d_ in range(2)]
        rgs = [sb("rgs%d" % i, [128, 512], BF16) for i in range(2)]
        n = 0
        nb = [0]

        def b_prep(c):
            dma("sp", Up[:], U[c * 128:(c + 1) * 128, :], ["U"], [Up.k])
            for k_ in range(4):
                I("dve", "tensor_scalar", out=dg[k_][:], in0=identf[:], scalar1=cw[:, k_, c:c + 1], scalar2=None, op0=ALU.mult,
                  reads=[identf.k, cw.k], writes=[dg[k_].k])
            for d_ in range(2):
                for w_, src in enumerate((rg_wa, rg_wx)):
                    t_ = bd[d_][w_]
                    I("pool", "memset", t_[:], 0.0, writes=[t_.k])
                    for hb in range(2):
                        cdma(t_[hb * 64:(hb + 1) * 64, hb * 64:(hb + 1) * 64], src[l, d_, 2 * c + hb], [], [t_.k])

        def b_conv(c):
            for (g0, T_) in MACROS:
                nb[0] += 1
                n = nb[0]
                if n % 2 == 0:
                    a2_advance(1)
                ps = PS[n % 2]
                for k_ in range(4):
                    c0 = ucol(g0) + k_ - 2
                    I("pe", "matmul", ps[:, 0:T_], lhsT=dg[k_][:], rhs=Up[:, c0:c0 + T_], start=(k_ == 0), stop=(k_ == 3),
                      reads=[dg[k_].k, Up.k], writes=[ps.k])
                I("act", "activation", out=uc32[:, g0:g0 + T_], in_=ps[:, 0:T_], func=AF.Identity, bias=cb[:, c:c + 1],
                  reads=[ps.k, cb.k], writes=[uc32.k + "_%d" % g0])
                I("dve", "tensor_copy", out=ucb[:, g0:g0 + T_], in_=uc32[:, g0:g0 + T_], reads=[uc32.k + "_%d" % g0], writes=[ucb.k + "_%d" % g0])

        b_prep(0)
        b_conv(0)
        for c in range(4):
            dma("sp", gyt[:], GY[c * 128:(c + 1) * 128, :], ["GY"], [gyt.k])
            AK = [[AB[d_][0].k + "_%d" % g0 for (g0, T_) in MACROS] for d_ in range(2)]
            BK = [[AB[d_][1].k + "_%d" % g0 for (g0, T_) in MACROS] for d_ in range(2)]
            for mi_, (g0, T_) in enumerate(MACROS):
                a2_advance(1)
                for d_ in range(2):
                    n += 1
                    pr, pi = PS[2 + (n % 2) * 2], PS[3 + (n % 2) * 2]
                    Aa, Bb = AB[d_]
                    I("pe", "matmul", pr[:, 0:T_], lhsT=bd[d_][0][:], rhs=ucb[:, g0:g0 + T_], start=True, stop=True,
                      reads=[bd[d_][0].k, ucb.k + "_%d" % g0], writes=[pr.k])
                    I("pe", "matmul", pi[:, 0:T_], lhsT=bd[d_][1][:], rhs=ucb[:, g0:g0 + T_], start=True, stop=True,
                      reads=[bd[d_][1].k, ucb.k + "_%d" % g0], writes=[pi.k])
                    I("act", "activation", out=Aa[:, g0:g0 + T_], in_=pr[:, 0:T_], func=AF.Sigmoid, bias=gb[:, 0, d_, c:c + 1],
                      reads=[pr.k, gb.k], writes=[AK[d_][mi_]])
                    I("act", "activation", out=Bb[:, g0:g0 + T_], in_=pi[:, 0:T_], func=AF.Sigmoid, bias=gb[:, 1, d_, c:c + 1],
                      reads=[pi.k, gb.k], writes=[BK[d_][mi_]])
            uall = [uc32.k + "_%d" % g0 for (g0, T_) in MACROS]
            for d_ in range(2):
                Aa, Bb = AB[d_]
                I("act", "activation", out=Aa[:], in_=Aa[:], func=AF.Exp, scale=cv[:, d_, c:c + 1], reads=AK[d_] + [cv.k], writes=AK[d_])
            for d_ in range(2):
                Aa, Bb = AB[d_]
                I("act", "activation", out=Hd[d_][:], in_=Aa[:], func=AF.Square, reads=AK[d_], writes=[Hd[d_].k])
                I("dve", "tensor_tensor", out=Bb[:], in0=Bb[:], in1=uc32[:], op=ALU.mult, reads=BK[d_] + uall, writes=BK[d_])
            if c + 1 < 4:
                b_prep(c + 1)
            for d_ in range(2):
                I("act", "activation", out=Hd[d_][:], in_=Hd[d_][:], func=AF.Sqrt, scale=-1.0, bias=ones[:, 0:1],
                  reads=[Hd[d_].k, ones.k], writes=[Hd[d_].k])
            for d_ in range(2):
                Aa, Bb = AB[d_]
                I("dve", "tensor_tensor", out=Bb[:], in0=Bb[:], in1=Hd[d_][:], op=ALU.mult, reads=BK[d_] + [Hd[d_].k], writes=BK[d_])
            Aa, Bb = AB[0]
            I("dve", "tensor_tensor_scan", out=Hd[0][:, 0:NCTX], data0=Aa[:, 0:NCTX], data1=Bb[:, 0:NCTX], initial=0.0,
                                                                  op0=ALU.mult, op1=ALU.add, reads=AK[0] + BK[0], writes=[Hd[0].k])
            I("dve", "tensor_tensor_scan", out=Hd[0][:, NCTX:NT], data0=Aa[:, NCTX:NT], data1=Bb[:, NCTX:NT],
                                                                  initial=Hd[0][:, NCTX - 1:NCTX], op0=ALU.mult, op1=ALU.add,
                 reads=AK[0] + BK[0] + [Hd[0].k], writes=[Hd[0].k])
            Aa, Bb = AB[1]
            I("dve", "tensor_tensor_scan", out=Hd[1][:, NCTX - 1::-1], data0=Aa[:, NCTX - 1::-1], data1=Bb[:, NCTX - 1::-1],
                                                                  initial=0.0, op0=ALU.mult, op1=ALU.add, reads=AK[1] + BK[1], writes=[Hd[1].k])
            I("dve", "tensor_tensor_scan", out=Hd[1][:, NT - 1:NCTX - 1:-1], data0=Aa[:, NT - 1:NCTX - 1:-1],
                                                                  data1=Bb[:, NT - 1:NCTX - 1:-1], initial=Hd[1][:, 0:1], op0=ALU.mult, op1=ALU.add,
                 reads=AK[1] + BK[1] + [Hd[1].k], writes=[Hd[1].k])
            if c + 1 < 4:
                b_conv(c + 1)
            I("dve", "tensor_tensor", out=Hd[0][:], in0=Hd[0][:], in1=Hd[1][:], op=ALU.add, reads=[Hd[0].k, Hd[1].k], writes=[Hd[0].k])
            for (g0, T_) in MACROS:
                n += 1
                rg_ = rgs[n % 2]
                I("dve", "tensor_tensor", out=rg_[:, 0:T_], in0=Hd[0][:, g0:g0 + T_], in1=gyt[:, g0:g0 + T_], op=ALU.mult,
                  reads=[Hd[0].k, gyt.k], writes=[rg_.k])
                dma("sp", YRG[c * 128:(c + 1) * 128, g0:g0 + T_], rg_[:, 0:T_], [rg_.k], ["YRG"])

        a2_advance(NCH)
        A.reset(phase_base)
        P.barrier()
        Wout = sb("Wout", [128, 8, D], BF16)
        for kc in range(8):
            cdma(Wout[:, kc, :], w_out[l, kc * 128:(kc + 1) * 128, :], [], [Wout.k])
        tab = sb("tab", [128, 8, 128], F32)
        dma("sp", tab[:], k_tab[:], [], [tab.k])
        rd = sb("rd", [128, 8], F32)
        dma("sp", rd[:], ret_decay[l:l + 1, :].broadcast_to([128, 8]), [], [rd.k])
        lg = sb("lg", [128, 8], F32)
        I("act", "activation", out=lg[:], in_=rd[:], func=AF.Exp, scale=-1.0, reads=[rd.k], writes=[lg.k])
        I("act", "activation", out=lg[:], in_=lg[:], func=AF.Ln, bias=ones[:, 0:1], reads=[lg.k, ones.k], writes=[lg.k])
        I("dve", "tensor_scalar", out=lg[:], in0=lg[:], scalar1=-1.0, scalar2=None, op0=ALU.mult, reads=[lg.k], writes=[lg.k])
        maskT = sb("maskT", [128, 4, 128], F32)
        mtmp = sb("mtmp", [128, 128], F32)
        QFt = sb("QFt", [128, 4, 128], F32)
        QBt = sb("QBt", [128, 4, 128], F32)
        for h in range(4):
            I("act", "activation", out=maskT[:, h, :], in_=tab[:, 0, :], func=AF.Exp, scale=lg[:, h:h + 1],
                 reads=[tab.k, lg.k], writes=[maskT.k])
            I("dve", "tensor_tensor", out=maskT[:, h, :], in0=maskT[:, h, :], in1=tab[:, 1, :], op=ALU.mult,
                 reads=[maskT.k, tab.k], writes=[maskT.k])
            I("act", "activation", out=mtmp[:], in_=tab[:, 2, :], func=AF.Exp, scale=lg[:, 4 + h:5 + h],
                 reads=[tab.k, lg.k], writes=[mtmp.k])
            I("dve", "tensor_tensor", out=mtmp[:], in0=mtmp[:], in1=tab[:, 3, :], op=ALU.mult, reads=[mtmp.k, tab.k], writes=[mtmp.k])
            I("dve", "tensor_tensor", out=maskT[:, h, :], in0=maskT[:, h, :], in1=mtmp[:], op=ALU.add,
                 reads=[maskT.k, mtmp.k], writes=[maskT.k])
            I("act", "activation", out=QFt[:, h, :], in_=tab[:, 4, :], func=AF.Exp, scale=lg[:, h:h + 1],
                 reads=[tab.k, lg.k], writes=[QFt.k])
            I("act", "activation", out=QBt[:, h, :], in_=tab[:, 5, :], func=AF.Exp, scale=lg[:, 4 + h:5 + h],
                 reads=[tab.k, lg.k], writes=[QBt.k])
        GM = load_mod(2, "GM")
        G2 = load_mod(4, "G2")
        SH2 = load_mod(3, "SH2")
        moe_layer = (l == 1)
        if moe_layer:
            wr = sb("wr", [128, 8, 128], F32)
            I("dve", "memset", wr[:], 0.0, writes=[wr.k])
            dma("sp", wr[:, :, 0:NEXP], moe_router.rearrange("(k p) n -> p k n", p=128), [], [wr.k])
            brt = sb("brt", [128, NEXP], F32)
            dma("sp", brt[:], moe_router_b.rearrange("(o n) -> o n", o=1).broadcast_to([128, NEXP]), [], [brt.k])
        Qm = [sb("Qm%d" % i, [128, 4, 512], BF16) for i in range(2)]
        Km = [sb("Km%d" % i, [128, 4, 512], BF16) for i in range(2)]
        Qf = [sb("Qf%d" % i, [128, 4, 512], BF16) for i in range(2)]
        Qb = [sb("Qb%d" % i, [128, 4, 512], BF16) for i in range(2)]
        Yrg = [sb("Yrg%d" % i, [128, 4, 512], BF16) for i in range(2)]
        Yret = [sb("Yret%d" % i, [128, 4, 512], BF16) for i in range(2)]
        H2st = [sb("H2st%d" % i, [128, 8, 512], BF16) for i in range(2)] if not moe_layer else [None, None]
        stm = [sb("stm%d" % i, [128, 512], BF16) for i in range(2)]
        xc = [sb("xc%d" % i, [128, 512], F32) for i in range(2)]
        rett = [sb("rett%d" % i, [128, 512], BF16) for i in range(2)]
        small = [sb("small%d" % i, [128, 16], F32) for i in range(2)]
        junk2 = sb("junk2", [128, 128], F32)
        t2s = [sb("t2_%d" % i, [128, D], F32) for i in range(2)]
        junk = sb("junk", [128, D], F32)
        ssqs = [sb("ssq%d" % i, [128, 1], F32) for i in range(2)]
        rstds = [sb("rstd%d" % i, [128, 1], F32) for i in range(2)]
        h2f = [sb("h2f%d" % i, [128, 8, 128], F32) for i in range(2)] if moe_layer else None
        rt = [sb("rt%d" % i, [128, 6, NEXP], F32) for i in range(2)]
        rsm = [sb("rsm%d" % i, [128, 8], F32) for i in range(2)]
        h2tb = [sb("h2tb%d" % i, [128, D], BF16) for i in range(2)]
        cl3 = [[sb("cl3_%d_%d" % (i, j), [128, 512], BF16) for j in range(4)] for i in range(3)]
        xts4 = [sb("xt4_%d" % i, [128, D], F32) for i in range(8)]
        x1s4 = [sb("x1q_%d" % i, [128, D], F32) for i in range(4)]
        chunks = [(mi, ci) for mi, (g0, T_) in enumerate(MACROS) for ci in range(T_ // 128)]

        def c_macro_loads(mi):
            g0, T_ = MACROS[mi]
            dma("sp", Qm[mi % 2][:, :, 0:T_], Q[:, g0:g0 + T_].rearrange("(h d) t -> d h t", d=128), ["Q"], [Qm[mi % 2].k])
            dma("sp", Km[mi % 2][:, :, 0:T_], K[:, g0:g0 + T_].rearrange("(h d) t -> d h t", d=128), ["K"], [Km[mi % 2].k])
            dma("sp", Yrg[mi % 2][:, :, 0:T_], YRG[:, g0:g0 + T_].rearrange("(c p) t -> p c t", p=128), ["YRG"], [Yrg[mi % 2].k])

        def c_chunk_loads(n_):
            mi, ci = chunks[n_]
            gc = MACROS[mi][0] // 128 + ci
            vt_, sg_, sf_, sb_ = cl3[n_ % 3]
            dma("sp", vt_[:], VT[gc * 128:(gc + 1) * 128, :], ["VT"], [vt_.k])
            dma("sp", sg_[:], SGT[gc * 128:(gc + 1) * 128, :], ["SGT"], [sg_.k])
            dma("sp", sf_[:], SF[gc], ["SF"], [sf_.k])
            dma("sp", sb_[:], SB[gc], ["SB"], [sb_.k])
            if not (MACROS[mi][0] < NCTX and last):
                r0 = gc * 128
                dma("sp", xts4[n_ % 8][:], xsrc[r0:r0 + 128, :], ["XS"], [xts4[n_ % 8].k])

        c_macro_loads(0)
        c_macro_loads(1)
        c_chunk_loads(0)
        c_chunk_loads(1)
        cnt_c = [0]
        ntl = [0]
        for mi, (g0, T_) in enumerate(MACROS):
            is_ctx = g0 < NCTX
            qm, km, qf, qb, yrg, yret, h2st = Qm[mi % 2], Km[mi % 2], Qf[mi % 2], Qb[mi % 2], Yrg[mi % 2], Yret[mi % 2], H2st[mi % 2]
            for ci in range(T_ // 128):
                sl = slice(ci * 128, (ci + 1) * 128)
                I("dve", "tensor_tensor", out=qf[:, :, sl], in0=qm[:, :, sl], in1=QFt[:], op=ALU.mult, reads=[qm.k, QFt.k], writes=[qf.k + "_%d" % ci])
                I("dve", "tensor_tensor", out=qb[:, :, sl], in0=qm[:, :, sl], in1=QBt[:], op=ALU.mult, reads=[qm.k, QBt.k], writes=[qb.k + "_%d" % ci])
            cstate = {}

            def c_front(ci):
                nonlocal_n = cnt_c
                sl = slice(ci * 128, (ci + 1) * 128)
                if nonlocal_n[0] + 2 < len(chunks):
                    c_chunk_loads(nonlocal_n[0] + 2)
                vt_, sg_, sf_, sb_ = cl3[nonlocal_n[0] % 3]
                nonlocal_n[0] += 1
                i2 = nonlocal_n[0] % 2
                pst, pso = PS[i2], PS[2 + i2]
                for h in range(4):
                    I("pe", "matmul", pst[:, h * 128:(h + 1) * 128], lhsT=km[:, h, sl], rhs=qm[:, h, sl], start=True, stop=True,
                      reads=[km.k, qm.k], writes=[pst.k])
                sm_ = stm[i2]
                I("dve", "tensor_tensor", out=sm_[:], in0=pst[:], in1=maskT[:].rearrange("p h i -> p (h i)"), op=ALU.mult,
                  reads=[pst.k, maskT.k], writes=[sm_.k])
                for h in range(4):
                    hs = slice(h * 128, (h + 1) * 128)
                    I("pe", "matmul", pso[:, hs], lhsT=sm_[:, hs], rhs=vt_[:, hs], start=True, stop=False, reads=[sm_.k, vt_.k], writes=[pso.k])
                    I("pe", "matmul", pso[:, hs], lhsT=qf[:, h, sl], rhs=sf_[:, hs], start=False, stop=False, reads=[qf.k + "_%d" % ci, sf_.k], writes=[pso.k])
                    I("pe", "matmul", pso[:, hs], lhsT=qb[:, h, sl], rhs=sb_[:, hs], start=False, stop=True, reads=[qb.k + "_%d" % ci, sb_.k], writes=[pso.k])
                smal = small[i2]
                xc_ = xc[i2]
                for h in range(4):
                    hs = slice(h * 128, (h + 1) * 128)
                    I("act", "activation", out=junk2[:], in_=pso[:, hs], func=AF.Copy, accum_out=smal[:, h:h + 1], reads=[pso.k], writes=[junk2.k, smal.k])
                    I("act", "activation", out=junk2[:], in_=pso[:, hs], func=AF.Square, accum_out=smal[:, 4 + h:5 + h], reads=[pso.k], writes=[junk2.k, smal.k])
                I("dve", "tensor_scalar", out=smal[:, 0:4], in0=smal[:, 0:4], scalar1=1.0 / 128, scalar2=None, op0=ALU.mult, reads=[smal.k], writes=[smal.k])
                I("dve", "tensor_tensor", out=smal[:, 12:16], in0=smal[:, 0:4], in1=smal[:, 0:4], op=ALU.mult, reads=[smal.k], writes=[smal.k])
                I("dve", "scalar_tensor_tensor", out=smal[:, 8:12], in0=smal[:, 4:8], scalar=1.0 / 128, in1=smal[:, 12:16], op0=ALU.mult, op1=ALU.subtract,
                  reads=[smal.k], writes=[smal.k])
                I("act", "activation", out=smal[:, 8:12], in_=smal[:, 8:12], func=AF.Sqrt, bias=epsc[:], reads=[smal.k, epsc.k], writes=[smal.k])
                I("dve", "reciprocal", out=smal[:, 8:12], in_=smal[:, 8:12], reads=[smal.k], writes=[smal.k])
                I("dve", "scalar_tensor_tensor", out=smal[:, 12:16], in0=smal[:, 0:4], scalar=-1.0, in1=smal[:, 8:12], op0=ALU.mult, op1=ALU.mult,
                  reads=[smal.k], writes=[smal.k])
                for h in range(4):
                    hs = slice(h * 128, (h + 1) * 128)
                    I("act", "activation", out=xc_[:, hs], in_=pso[:, hs], func=AF.Identity, scale=smal[:, 8 + h:9 + h], bias=smal[:, 12 + h:13 + h],
                      reads=[pso.k, smal.k], writes=[xc_.k])
                rt_ = rett[i2]
                I("dve", "tensor_tensor", out=rt_[:], in0=xc_[:], in1=sg_[:], op=ALU.mult, reads=[xc_.k, sg_.k], writes=[rt_.k])
                cstate[ci] = (i2, rt_, pst)

            def c_back(ci):
                sl = slice(ci * 128, (ci + 1) * 128)
                i2, rt_, ptr = cstate[ci]
                for h in range(4):
                    hs = slice(h * 128, (h + 1) * 128)
                    I("pe", "transpose", out=ptr[:].bitcast(BF16)[:, hs], in_=rt_[:, hs], identity=identb[:], reads=[rt_.k, identb.k], writes=[ptr.k])
                I("act", "activation", out=yret[:, :, sl], in_=ptr[:].bitcast(BF16)[:, 0:512].rearrange("p (h t) -> p h t", h=4), func=AF.Copy,
                  reads=[ptr.k], writes=[yret.k + "_%d" % ci])

            nci = T_ // 128
            c_front(0)
            for ci in range(nci):
                if ci + 1 < nci:
                    c_front(ci + 1)
                c_back(ci)
            need_ffn = not (is_ctx and last)
            tstate = {}

            def t_front(tt):
                ntl[0] += 1
                ntile = ntl[0]
                i2 = ntile % 2
                r0 = g0 + tt * 128
                ts_ = slice(tt * 128, (tt + 1) * 128)
                x1, t2, ssq, rstd = x1s4[ntile % 4], t2s[i2], ssqs[i2], rstds[i2]
                xt = xts4[(ntile - 1) % 8]
                precast_some(4)
                if is_ctx and last:
                    tstate[tt] = None
                    return
                gm = modsel(GM, g0)
                for half in range(2):
                    cs = slice(half * 512, (half + 1) * 512)
                    py = PS[4 + half]
                    for kc in range(8):
                        src = yrg if kc < 4 else yret
                        I("pe", "matmul", py[:], lhsT=src[:, kc % 4, ts_], rhs=Wout[:, kc, cs], start=(kc == 0), stop=(kc == 7),
                          reads=[src.k if kc < 4 else yret.k + "_%d" % tt, Wout.k], writes=[py.k])
                    I("dve", "tensor_tensor", out=x1[:, cs], in0=py[:], in1=gm[:, cs], op=ALU.mult, reads=[py.k, gm.k], writes=[x1.k])
                I("dve", "tensor_tensor", out=x1[:], in0=x1[:], in1=xt[:], op=ALU.add, reads=[x1.k, xt.k], writes=[x1.k])
                dma("sp", X1[r0:r0 + 128, :], x1[:], [x1.k], ["X1"])
                I("act", "activation", out=junk[:], in_=x1[:], func=AF.Square, accum_out=ssq[:], reads=[x1.k], writes=[junk.k, ssq.k])
                sqrt_recip(rstd, ssq, 1.0 / D)
                g2, sh2 = modsel(G2, g0), modsel(SH2, g0)
                I("dve", "scalar_tensor_tensor", out=t2[:], in0=x1[:], scalar=rstd[:], in1=g2[:], op0=ALU.mult, op1=ALU.mult,
                  reads=[x1.k, rstd.k, g2.k], writes=[t2.k])
                I("dve", "tensor_tensor", out=t2[:], in0=t2[:], in1=sh2[:], op=ALU.add, reads=[t2.k, sh2.k], writes=[t2.k])
                tstate[tt] = (i2, r0, ts_, t2)

            def t_back(tt):
                if tstate[tt] is None:
                    return
                i2, r0, ts_, t2 = tstate[tt]
                if not moe_layer:
                    hb_ = h2tb[i2]
                    I("act", "activation", out=hb_[:], in_=t2[:], func=AF.Copy, reads=[t2.k], writes=[hb_.k])
                    pa = PS[6 + i2]
                    for kc in range(8):
                        I("pe", "transpose", out=pa[:].bitcast(BF16)[:, kc * 128:(kc + 1) * 128], in_=hb_[:, kc * 128:(kc + 1) * 128], identity=identb[:],
                          reads=[hb_.k, identb.k], writes=[pa.k])
                    I("act", "activation", out=h2st[:, :, ts_], in_=pa[:].bitcast(BF16)[:, 0:1024].rearrange("p (k t) -> p k t", k=8), func=AF.Copy,
                      reads=[pa.k], writes=[h2st.k])
                elif not is_ctx:
                    pa, pb = PS[6], PS[7]
                    for kc in range(8):
                        pp = pa if kc < 4 else pb
                        I("pe", "transpose", out=pp[:, (kc % 4) * 128:(kc % 4 + 1) * 128], in_=t2[:, kc * 128:(kc + 1) * 128], identity=identf[:],
                          reads=[t2.k, identf.k], writes=[pp.k])
                    hf = h2f[i2]
                    for hh, pp in enumerate((pa, pb)):
                        I("dve", "tensor_copy", out=hf[:, hh * 4:(hh + 1) * 4, :], in_=pp[:].rearrange("p (k t) -> p k t", k=4), reads=[pp.k], writes=[hf.k])
                    pl = PS[4]
                    for kc in range(8):
                        I("pe", "matmul", pl[:, 0:128], lhsT=hf[:, kc, :], rhs=wr[:, kc, :], start=(kc == 0), stop=(kc == 7),
                          reads=[hf.k, wr.k], writes=[pl.k])
                    r_ = rt[i2]
                    s_ = rsm[i2]
                    ti = (r0 - NCTX) // 128
                    I("dve", "tensor_tensor", out=r_[:, 0, :], in0=pl[:, 0:NEXP], in1=brt[:], op=ALU.add, reads=[pl.k, brt.k], writes=[r_.k])
                    I("dve", "reduce_max", out=s_[:, 0:1], in_=r_[:, 0, :], axis=AX.X, reads=[r_.k], writes=[s_.k])
                    I("dve", "tensor_scalar", out=r_[:, 1, :], in0=r_[:, 0, :], scalar1=s_[:, 0:1], scalar2=None, op0=ALU.is_ge, reads=[r_.k, s_.k], writes=[r_.k])
                    I("dve", "scalar_tensor_tensor", out=r_[:, 2, :], in0=r_[:, 1, :], scalar=-1e30, in1=r_[:, 0, :], op0=ALU.mult, op1=ALU.add, reads=[r_.k], writes=[r_.k])
                    I("dve", "reduce_max", out=s_[:, 1:2], in_=r_[:, 2, :], axis=AX.X, reads=[r_.k], writes=[s_.k])
                    I("dve", "tensor_scalar", out=r_[:, 3, :], in0=r_[:, 0, :], scalar1=s_[:, 1:2], scalar2=None, op0=ALU.is_ge, reads=[r_.k, s_.k], writes=[r_.k])
                    I("dve", "tensor_scalar", out=s_[:, 2:3], in0=s_[:, 0:1], scalar1=-1.0, scalar2=None, op0=ALU.mult, reads=[s_.k], writes=[s_.k])
                    I("act", "activation", out=r_[:, 4, :], in_=r_[:, 0, :], func=AF.Exp, bias=s_[:, 2:3], reads=[r_.k, s_.k], writes=[r_.k])
                    I("dve", "tensor_tensor", out=r_[:, 5, :], in0=r_[:, 4, :], in1=r_[:, 3, :], op=ALU.mult, reads=[r_.k], writes=[r_.k])
                    I("dve", "reduce_sum", out=s_[:, 3:4], in_=r_[:, 5, :], axis=AX.X, reads=[r_.k], writes=[s_.k])
                    I("dve", "reciprocal", out=s_[:, 4:5], in_=s_[:, 3:4], reads=[s_.k], writes=[s_.k])
                    I("dve", "tensor_scalar", out=RW[:, ti, :], in0=r_[:, 5, :], scalar1=s_[:, 4:5], scalar2=None, op0=ALU.mult, reads=[r_.k, s_.k], writes=[RW.k])
                    I("dve", "tensor_copy", out=SEL[:, ti, :], in_=r_[:, 3, :], reads=[r_.k], writes=[SEL.k])
                    I("dve", "tensor_copy", out=OH1[:, ti, :], in_=r_[:, 1, :], reads=[r_.k], writes=[OH1.k])
                    hb_ = h2tb[i2]
                    I("act", "activation", out=hb_[:], in_=t2[:], func=AF.Copy, reads=[t2.k], writes=[hb_.k])
                    dma("sp", H2T[r0 - NCTX:r0 - NCTX + 128, :], hb_[:], [hb_.k], ["H2T"])
            ntt = T_ // 128
            t_front(0)
            for tt in range(ntt):
                if tt + 1 < ntt:
                    t_front(tt + 1)
                t_back(tt)
            if need_ffn and not moe_layer:
                dma("sp", H2[:, g0:g0 + T_].rearrange("(k p) t -> p k t", p=128), h2st[:, :, 0:T_], [h2st.k], ["H2"])
            if mi + 2 < len(MACROS):
                c_macro_loads(mi + 2)

        if moe_layer:
            chk(l, "C")
            moe_sorted(l)
            continue
        A.reset(phase_base)
        P.barrier()
        GF = load_mod(5, "GF")
        if last:
            gfin = sb("gfin", [128, D], F32)
            dma("sp", gfin[:], g_final.rearrange("(o n) -> o n", o=1).broadcast_to([128, D]), [], [gfin.k])
        if moe_layer:
            blocks = [(NCTX + 1024 * i, 1024) for i in range(4)]
            experts = [(MW1b[e_], MW3b[e_], MW2b[e_], e_) for e_ in range(n_exp)]
            FC = DEXP // 128
            FS = 7
        else:
            blocks = [(0, 256)] + [(NCTX + 1024 * i, 1024) for i in range(4)]
            experts = [(FW1b, FW3b, FW2b, None)]
            FC = DFF // 128
            FS = 6
        H2bs = [sb("H2b%d" % i, [128, 8, 1024], BF16) for i in range(2)]
        Wset = [(sb("W1q%d" % i, [128, 8, 7 * 128], BF16), sb("W3q%d" % i, [128, 8, 7 * 128], BF16), sb("W2q%d" % i, [128, 7, D], BF16))
                for i in range(2)]
        actq = [sb("actq%d" % i, [128, 7, 1024], BF16) for i in range(2)]
        yacc = sb("yacc", [128, 8, D], F32)
        slt = [sb("slt%d" % i, [128, 512], F32) for i in range(3)]
        x1s = [sb("x1_%d" % i, [128, D], F32) for i in range(2)]
        junk = sb("junk", [128, D], F32)
        ssqs = [sb("ssq%d" % i, [128, 1], F32) for i in range(2)]
        rstds = [sb("rstd%d" % i, [128, 1], F32) for i in range(2)]
        units = []
        for bi, (g0, TD) in enumerate(blocks):
            per = [(w, f0, min(FS, FC - f0)) for w in experts for f0 in range(0, FC, FS)]
            for ui, (w, f0, fs) in enumerate(per):
                units.append((bi, g0, TD, w, f0, fs, ui == 0, ui == len(per) - 1))
        cntd = {"ps": 0, "fin": 0, "py": 0}

        def L13(s_):
            bi, g0, TD, w, f0, fs, fst, lst = units[s_]
            w1q, w3q, _ = Wset[s_ % 2]
            dma("sp", w1q[:, :, 0:fs * 128], w[0][:, :, f0 * 128:(f0 + fs) * 128], ["Wb"], [w1q.k])
            dma("sp", w3q[:, :, 0:fs * 128], w[1][:, :, f0 * 128:(f0 + fs) * 128], ["Wb"], [w3q.k])

        def L2(s_):
            bi, g0, TD, w, f0, fs, fst, lst = units[s_]
            w2q = Wset[s_ % 2][2]
            dma("sp", w2q[:, 0:fs, :], w[2][:, f0:f0 + fs, :], ["Wb"], [w2q.k])

        def S1(s_):
            bi, g0, TD, w, f0, fs, fst, lst = units[s_]
            w1q, w3q, _ = Wset[s_ % 2]
            aq = actq[s_ % 2]
            H2b = H2bs[bi % 2]
            if fst and bi + 1 < len(blocks):
                g0n, TDn = blocks[bi + 1]
                dma("sp", H2bs[(bi + 1) % 2][:, :, 0:TDn], H2[:, g0n:g0n + TDn].rearrange("(k p) t -> p k t", p=128), ["H2"], [H2bs[(bi + 1) % 2].k])
            pieces = [(p0, min(512, TD - p0)) for p0 in range(0, TD, 512)]
            for j in range(fs):
                for (p0, pw) in pieces:
                    cntd["ps"] += 1
                    n_ = cntd["ps"]
                    pa, pb = PS[(n_ % 2) * 2], PS[(n_ % 2) * 2 + 1]
                    for kc in range(8):
                        I("pe", "matmul", pa[:, 0:pw], lhsT=w1q[:, kc, j * 128:(j + 1) * 128], rhs=H2b[:, kc, p0:p0 + pw], start=(kc == 0), stop=(kc == 7),
                          reads=[w1q.k, H2b.k], writes=[pa.k])
                    for kc in range(8):
                        I("pe", "matmul", pb[:, 0:pw], lhsT=w3q[:, kc, j * 128:(j + 1) * 128], rhs=H2b[:, kc, p0:p0 + pw], start=(kc == 0), stop=(kc == 7),
                          reads=[w3q.k, H2b.k], writes=[pb.k])
                    sl_ = slt[n_ % len(slt)]
                    I("act", "activation", out=sl_[:, 0:pw], in_=pa[:, 0:pw], func=AF.Silu, reads=[pa.k], writes=[sl_.k])
                    I("dve", "tensor_tensor", out=aq[:, j, p0:p0 + pw], in0=sl_[:, 0:pw], in1=pb[:, 0:pw], op=ALU.mult,
                      reads=[sl_.k, pb.k], writes=[aq.k + "_%d_%d" % (j, p0)])

        def S2(s_):
            bi, g0, TD, w, f0, fs, fst, lst = units[s_]
            w2q = Wset[s_ % 2][2]
            aq = actq[s_ % 2]
            eidx = w[3]
            for tt in range(TD // 128):
                ts_ = slice(tt * 128, (tt + 1) * 128)
                for half in range(2):
                    cs = slice(half * 512, (half + 1) * 512)
                    cntd["py"] += 1
                    py = PS[4 + cntd["py"] % 4]
                    for j in range(fs):
                        I("pe", "matmul", py[:], lhsT=aq[:, j, ts_], rhs=w2q[:, j, cs], start=(j == 0), stop=(j == fs - 1),
                          reads=[aq.k + "_%d_0" % j, aq.k + "_%d_512" % j, w2q.k], writes=[py.k])
                    yk = yacc.k + "_%dh%d" % (tt, half)
                    if eidx is None:
                        if fst:
                            I("act", "activation", out=yacc[:, tt, cs], in_=py[:], func=AF.Copy, reads=[py.k], writes=[yk])
                        else:
                            I("dve", "tensor_tensor", out=yacc[:, tt, cs], in0=py[:], in1=yacc[:, tt, cs], op=ALU.add, reads=[py.k, yk], writes=[yk])
                    else:
                        ti = (g0 - NCTX) // 128 + tt
                        if fst:
                            I("dve", "tensor_scalar", out=yacc[:, tt, cs], in0=py[:], scalar1=RW[:, ti, eidx:eidx + 1], scalar2=None, op0=ALU.mult,
                              reads=[py.k, RW.k], writes=[yk])
                        else:
                            I("dve", "scalar_tensor_tensor", out=yacc[:, tt, cs], in0=py[:], scalar=RW[:, ti, eidx:eidx + 1], in1=yacc[:, tt, cs],
                              op0=ALU.mult, op1=ALU.add, reads=[py.k, RW.k, yk], writes=[yk])
            if lst:
                gf = modsel(GF, g0)
                for tt in range(TD // 128):
                    cntd["fin"] += 1
                    i2 = cntd["fin"] % 2
                    r0 = g0 + tt * 128
                    x1, ssq, rstd = x1s[i2], ssqs[i2], rstds[i2]
                    yk = yacc.k + "_%dh0" % tt
                    yk1 = yacc.k + "_%dh1" % tt
                    dma("sp", x1[:], X1[r0:r0 + 128, :], ["X1"], [x1.k])
                    I("dve", "tensor_tensor", out=yacc[:, tt, :], in0=yacc[:, tt, :], in1=gf[:], op=ALU.mult, reads=[yk, yk1, gf.k], writes=[yk, yk1])
                    I("dve", "tensor_tensor", out=x1[:], in0=x1[:], in1=yacc[:, tt, :], op=ALU.add, reads=[x1.k, yk, yk1], writes=[x1.k])
                    if not last:
                        dma("sp", XS[r0:r0 + 128, :], x1[:], [x1.k], ["XS"])
                    else:
                        I("act", "activation", out=junk[:], in_=x1[:], func=AF.Square, accum_out=ssq[:],
                          reads=[x1.k], writes=[junk.k, ssq.k])
                        sqrt_recip(rstd, ssq, 1.0 / D)
                        I("dve", "scalar_tensor_tensor", out=x1[:], in0=x1[:], scalar=rstd[:], in1=gfin[:], op0=ALU.mult, op1=ALU.mult,
                          reads=[x1.k, rstd.k, gfin.k], writes=[x1.k])
                        dma("sp", out[r0 - NCTX:r0 - NCTX + 128, :], x1[:], [x1.k], ["OUT"])

        NU = len(units)
        dma("sp", H2bs[0][:, :, 0:blocks[0][1]], H2[:, blocks[0][0]:blocks[0][0] + blocks[0][1]].rearrange("(k p) t -> p k t", p=128), ["H2"], [H2bs[0].k])
        for s_ in range(min(2, NU)):
            L13(s_)
            L2(s_)
        for s_ in range(NU):
            if s_ >= 1 and s_ + 1 < NU:
                L13(s_ + 1)
            S1(s_)
            if s_ >= 1:
                S2(s_ - 1)
                if s_ + 1 < NU:
                    L2(s_ + 1)
            if not moe_layer:
                precast_some(10)
        S2(NU - 1)

    except _Stop:
        pass
    P.barrier()
    with nc.allow_non_contiguous_dma(reason="small parameter / head-split loads"):
        P.emit()
    psum_ctx.close()
    return nc


def host_constants():
    n_freq = 32
    pos = np.arange(NLAT)
    row = (pos // 64).astype(np.float32)
    col = (pos % 64).astype(np.float32)
    inv = (10000.0 ** (-np.arange(n_freq, dtype=np.float32) / n_freq)).astype(np.float32)
    ang = np.concatenate([row[:, None] * inv, col[:, None] * inv], axis=-1).astype(np.float32)
    cos = np.cos(ang).astype(np.float32).T
    sin = np.sin(ang).astype(np.float32).T
    k_cos = np.ones((128, NT), np.float32)
    k_sin = np.zeros((128, NT), np.float32)
    k_cos[0:64, NCTX:] = cos
    k_cos[64:128, NCTX:] = cos
    k_sin[0:64, NCTX:] = -sin
    k_sin[64:128, NCTX:] = sin
    j = np.arange(128, dtype=np.float32)[:, None]
    i = np.arange(128, dtype=np.float32)[None, :]
    tab = np.zeros((128, 8, 128), np.float32)
    tab[:, 0] = np.maximum(i - j, 0)
    tab[:, 1] = (i >= j)
    tab[:, 2] = np.maximum(j - i, 0)
    tab[:, 3] = (j > i)
    tab[:, 4] = np.broadcast_to(i + 1, (128, 128))
    tab[:, 5] = np.broadcast_to(128 - i, (128, 128))
    tab[:, 6] = np.broadcast_to(127 - j, (128, 128))
    tab[:, 7] = np.broadcast_to(j, (128, 128))
    tab2 = np.zeros((128, 2, 128), np.float32)
    tab2[:, 0] = (j <= i)
    tab2[:, 1] = np.broadcast_to(j, (128, 128))
    return {
        "k_zero": np.zeros((128, 4096), np.float32).astype(ml_dtypes.bfloat16),
        "k_tab2": tab2,
        "k_identb": np.eye(128, dtype=np.float32).astype(ml_dtypes.bfloat16),
        "k_identf": np.eye(128, dtype=np.float32),
        "k_cos": k_cos, "k_sin": k_sin, "k_tab": tab,
    }


def make_in_maps(inputs, cores):
    f = lambda a: np.ascontiguousarray(np.asarray(a, dtype=np.float32))
    w_in = f(inputs["w_in"])
    idx = []
    for blk in range(8):
        b0 = 1024 + blk * 128
        idx.extend(list(range(b0 + 64, b0 + 128)) + list(range(b0, b0 + 64)))
    w_in_ext = np.ascontiguousarray(np.concatenate([w_in, w_in[:, :, idx]], axis=-1))
    shared = {
        "c_ctx": f(inputs["c_ctx"]), "w_mod": f(inputs["w_mod"]), "b_mod": f(inputs["b_mod"]),
        "g_mix": f(inputs["g_mix"]), "g_ffn": f(inputs["g_ffn"]), "g_final": f(inputs["g_final"]),
        "w_in": w_in_ext, "w_out": f(inputs["w_out"]), "conv_w": f(inputs["conv_w"]), "conv_b": f(inputs["conv_b"]),
        "rg_wa": f(inputs["rg_wa"]), "rg_ba": f(inputs["rg_ba"]), "rg_wx": f(inputs["rg_wx"]), "rg_bx": f(inputs["rg_bx"]),
        "rg_lam": f(inputs["rg_lam"]), "ret_decay": f(inputs["ret_decay"]).reshape(2, 8),
        "ffn_w1": f(inputs["ffn_w1"]), "ffn_w3": f(inputs["ffn_w3"]), "ffn_w2": f(inputs["ffn_w2"]),
        "moe_router": f(inputs["moe_router"])[0], "moe_router_b": f(inputs["moe_router_b"])[0],
        "moe_w1": f(inputs["moe_w1"])[0], "moe_w3": f(inputs["moe_w3"])[0], "moe_w2": f(inputs["moe_w2"])[0],
    }
    shared.update(host_constants())
    x = f(inputs["x"])
    ctx = f(inputs["ctx"])
    c = f(inputs["c"])
    maps = []
    for b in cores:
        m = dict(shared)
        m["xin"] = np.ascontiguousarray(np.concatenate([ctx[b], x[b]], axis=0))
        m["c_b"] = np.ascontiguousarray(c[b])
        maps.append(m)
    return maps


_NC_CACHE = {}


def kernel(**inputs):
    if "nc" not in _NC_CACHE:
        _NC_CACHE["nc"] = build_program()
    nc = _NC_CACHE["nc"]
    in_maps = make_in_maps(inputs, list(range(8)))
    res = run_bass_kernel_spmd(nc, in_maps, core_ids=list(range(8)))
    return np.stack([np.asarray(r["out"], dtype=np.float32) for r in res.results], axis=0)
```

```python
import contextlib
import numpy as np
import ml_dtypes
import concourse.bass as bass
import concourse.mybir as mybir
from concourse.bass_utils import run_bass_kernel_spmd

F32 = mybir.dt.float32
BF16 = mybir.dt.bfloat16
I32 = mybir.dt.int32
AF = mybir.ActivationFunctionType
ALU = mybir.AluOpType
AX = mybir.AxisListType

D = 1024
NLAT = 4096
NCTX = 256
NT = NLAT + NCTX
NCH = NT // 128
UW = NT + 8
DFF = 2816
DEXP = 3584
NEXP = 8
EPS = 1e-6
MACROS = [(0, 256)] + [(256 + 512 * i, 512) for i in range(8)]

ENGS = ("pe", "act", "dve", "pool", "sp")
DMA_RING = 24
SAME_ENGINE_ALL = True


def ucol(g):
    return g + 2 if g < NCTX else g + 5


class Op:
    __slots__ = ("eng", "fn", "deps", "dma", "signals", "sigval", "dsem", "dval", "dprev", "ring")

    def __init__(self, eng, fn, dma):
        self.eng = eng
        self.fn = fn
        self.dma = dma
        self.deps = []
        self.signals = False
        self.sigval = 0
        self.dsem = None
        self.dval = 0
        self.dprev = 0
        self.ring = "n"


class Prog:
    def __init__(self, nc):
        self.nc = nc
        self.ops = {e: [] for e in ENGS}
        self.lastw = {}
        self.readers = {}
        self.ndma = {e: 0 for e in ENGS}
        self.ndma_bg = {e: 0 for e in ENGS}
        self.dmas_bg = {e: [] for e in ENGS}
        self.lastc = {e: None for e in ENGS}
        self.dmas = {e: [] for e in ENGS}

    def _add(self, eng, fn, reads, writes, dma, extra=(), bg=False):
        op = Op(eng, fn, dma)
        deps = list(extra)
        for k in reads:
            w = self.lastw.get(k)
            if w is not None:
                deps.append(w)
            if k.startswith("ps"):
                deps.extend(r for r in self.readers.get(k, ()) if r.eng != eng)
        nraw = len(deps)
        for k in writes:
            w = self.lastw.get(k)
            if w is not None:
                deps.append(w)
            deps.extend(self.readers.get(k, ()))
        raw_ids = set(id(d) for d in deps[:nraw])
        for k in reads:
            self.readers.setdefault(k, []).append(op)
        for k in writes:
            self.lastw[k] = op
            self.readers[k] = []
        seen = set()
        for d in deps:
            if d is op or id(d) in seen:
                continue
            seen.add(id(d))
            if (not d.dma) and (not dma) and d.eng == eng and fn is not None:
                if eng == "pe" or (id(d) not in raw_ids and not SAME_ENGINE_ALL):
                    continue
            op.deps.append(d)
            if not d.dma:
                d.signals = True
        if dma and bg:
            i = self.ndma_bg[eng]
            self.ndma_bg[eng] = i + 1
            op.ring = "bg"
            op.dsem = i % DMA_RING
            op.dval = 16 * (i // DMA_RING + 1)
            op.dprev = 16 * (i // DMA_RING)
            self.dmas_bg[eng].append(op)
        elif dma:
            i = self.ndma[eng]
            self.ndma[eng] = i + 1
            op.dsem = i % DMA_RING
            op.dval = 16 * (i // DMA_RING + 1)
            op.dprev = 16 * (i // DMA_RING)
            self.dmas[eng].append(op)
        elif fn is not None:
            self.lastc[eng] = op
        self.ops[eng].append(op)
        return op

    def op(self, eng, fn, reads=(), writes=()):
        return self._add(eng, fn, reads, writes, False)

    def dma(self, eng, fn, reads=(), writes=(), bg=False):
        return self._add(eng, fn, reads, writes, True, bg=bg)

    def barrier(self, include_bg=False):
        pend = [o for o in self.lastc.values() if o is not None]
        for e in ENGS:
            pend.extend(self.dmas[e][-DMA_RING:])
            if include_bg:
                pend.extend(self.dmas_bg[e][-DMA_RING:])
        for e in ENGS:
            self._add(e, None, (), (), False, extra=pend)
        self.lastw = {}
        self.readers = {}

    def emit(self):
        nc = self.nc
        for e in ENGS:
            cnt = 0
            for op in self.ops[e]:
                if (not op.dma) and op.fn is not None and op.signals:
                    cnt += 1
                    op.sigval = cnt
        with contextlib.ExitStack() as st:
            csem = {e: st.enter_context(nc.semaphore("c_" + e)) for e in ENGS}
            dsem = {(e, "n"): [st.enter_context(nc.semaphore("d_%s%d" % (e, i))) for i in range(DMA_RING)]
                    for e in ENGS if self.ndma[e] > 0}
            dsem.update({(e, "bg"): [st.enter_context(nc.semaphore("b_%s%d" % (e, i))) for i in range(DMA_RING)]
                         for e in ENGS if self.ndma_bg[e] > 0})
            block = st.enter_context(nc.Block())
            handles = {"pe": nc.tensor, "act": nc.scalar, "dve": nc.vector, "pool": nc.gpsimd, "sp": nc.sync}

            def build(e):
                eng = handles[e]
                seen = {}

                def wait(key, sem, val):
                    if seen.get(key, 0) >= val:
                        return
                    seen[key] = val
                    eng.wait_ge(sem, val)

                for op in self.ops[e]:
                    for d in op.deps:
                        if d.dma:
                            wait(("d", d.eng, d.ring, d.dsem), dsem[(d.eng, d.ring)][d.dsem], d.dval)
                        else:
                            wait(("c", d.eng), csem[d.eng], d.sigval)
                    if op.fn is None:
                        continue
                    if op.dma:
                        if op.dprev > 0:
                            wait(("d", e, op.ring, op.dsem), dsem[(e, op.ring)][op.dsem], op.dprev)
                        op.fn(eng).then_inc(dsem[(e, op.ring)][op.dsem], 16)
                    else:
                        ins = op.fn(eng)
                        if op.signals:
                            ins.then_inc(csem[e], 1)

            @block.tensor
            def _(t):
                build("pe")

            @block.scalar
            def _(t):
                build("act")

            @block.vector
            def _(t):
                build("dve")

            @block.gpsimd
            def _(t):
                build("pool")

            @block.sync
            def _(t):
                build("sp")


class Arena:
    def __init__(self, nc):
        self.nc = nc
        self.base = (nc.sbuf_base + 63) // 64 * 64
        self.top = nc.sbuf_top
        self.off = self.base
        self.n = 0

    def alloc(self, name, shape, dt):
        nbytes = int(np.prod(shape[1:])) * (2 if dt == BF16 else 4)
        nbytes = (nbytes + 63) // 64 * 64
        assert self.off + nbytes <= self.top, ("SBUF overflow", name, self.off, nbytes, self.top)
        self.n += 1
        t = self.nc.alloc_sbuf_tensor_at("%s_%d" % (name, self.n), list(shape), dt, offset=self.off)
        self.off += nbytes
        return t

    def mark(self):
        return self.off

    def reset(self, m):
        self.off = m


class _Stop(Exception):
    pass


def build_program(n_layers=2, debug=False, stop=None, n_exp=NEXP):
    nc = bass.Bass("TRN2", target_bir_lowering=False)
    P = Prog(nc)
    A = Arena(nc)
    ikind = "ExternalOutput" if debug else "Internal"

    def din(name, shape, dt=F32):
        return nc.dram_tensor(name, list(shape), dt, kind="ExternalInput").ap()

    def dscr(name, shape, dt):
        return nc.dram_tensor(name, list(shape), dt, kind=ikind).ap()

    xin = din("xin", [NT, D])
    c_b = din("c_b", [D])
    c_ctx = din("c_ctx", [D])
    w_mod = din("w_mod", [2, D, 6 * D])
    b_mod = din("b_mod", [2, 6 * D])
    g_mix = din("g_mix", [2, D])
    g_ffn = din("g_ffn", [2, D])
    g_final = din("g_final", [D])
    w_in = din("w_in", [2, D, 4096])
    w_out = din("w_out", [2, D, D])
    conv_w = din("conv_w", [2, 4, 512])
    conv_b = din("conv_b", [2, 512])
    rg_wa = din("rg_wa", [2, 2, 8, 64, 64])
    rg_ba = din("rg_ba", [2, 2, 512])
    rg_wx = din("rg_wx", [2, 2, 8, 64, 64])
    rg_bx = din("rg_bx", [2, 2, 512])
    rg_lam = din("rg_lam", [2, 2, 512])
    ret_decay = din("ret_decay", [2, 8])
    ffn_w1 = din("ffn_w1", [1, D, DFF])
    ffn_w3 = din("ffn_w3", [1, D, DFF])
    ffn_w2 = din("ffn_w2", [1, DFF, D])
    moe_router = din("moe_router", [D, NEXP])
    moe_router_b = din("moe_router_b", [NEXP])
    moe_w1 = din("moe_w1", [NEXP, D, DEXP])
    moe_w3 = din("moe_w3", [NEXP, D, DEXP])
    moe_w2 = din("moe_w2", [NEXP, DEXP, D])
    k_identb = din("k_identb", [128, 128], BF16)
    k_identf = din("k_identf", [128, 128])
    k_cos = din("k_cos", [128, NT])
    k_sin = din("k_sin", [128, NT])
    k_tab = din("k_tab", [128, 8, 128])
    out = nc.dram_tensor("out", [NLAT, D], F32, kind="ExternalOutput").ap()

    XS = dscr("s_x", [NT, D], F32)
    X1 = dscr("s_x1", [NT, D], F32)
    MODB = dscr("s_modb", [4, 128, 6 * D], F32)
    U = dscr("s_u", [512, UW], BF16)
    GY = dscr("s_gy", [512, NT], BF16)
    Q = dscr("s_q", [512, NT], BF16)
    K = dscr("s_k", [512, NT], BF16)
    KT = dscr("s_kt", [NT, 512], BF16)
    VT = dscr("s_vt", [NT, 512], BF16)
    SGT = dscr("s_sgt", [NT, 512], BF16)
    SF = dscr("s_sf", [NCH, 128, 512], BF16)
    SB = dscr("s_sb", [NCH, 128, 512], BF16)
    YRG = dscr("s_yrg", [512, NT], BF16)
    H2 = dscr("s_h2", [D, NT], BF16)

    def dscr_i(name, shape, dt):
        return nc.dram_tensor(name, list(shape), dt, kind="Internal").ap()

    FW1b = dscr_i("s_fw1", [128, 8, DFF], BF16)
    FW3b = dscr_i("s_fw3", [128, 8, DFF], BF16)
    FW2b = dscr_i("s_fw2", [128, DFF // 128, D], BF16)
    MW1q = dscr_i("s_mw1", [NEXP * 4 * 128, 8 * 896], BF16)
    MW3q = dscr_i("s_mw3", [NEXP * 4 * 128, 8 * 896], BF16)
    MW2q = dscr_i("s_mw2", [NEXP * 4 * 128, 7 * D], BF16)
    UNIT = 512
    NUN = 8192 // UNIT + NEXP
    NROWS = NUN * UNIT
    H2T = dscr("s_h2t", [NLAT, D], BF16)
    HS = dscr("s_hs", [NROWS, D], BF16)
    YS = dscr("s_ys", [NROWS, D], F32)
    k_zero = din("k_zero", [128, 4096], BF16)
    k_tab2 = din("k_tab2", [128, 2, 128])

    uid = [0]

    def key(s):
        uid[0] += 1
        return "%s#%d" % (s, uid[0])

    class T:
        def __init__(self, t, k):
            self.t = t
            self.k = k

        def __getitem__(self, idx):
            return self.t[idx]

    def sb(name, shape, dt):
        return T(A.alloc(name, shape, dt), key(name))

    psum_ctx = contextlib.ExitStack()
    PS = [T(psum_ctx.enter_context(nc.psum_tensor("ps%d" % i, [128, 512], F32)), "ps%d" % i) for i in range(8)]

    def ps_bf(i):
        return PS[i].t.bitcast(BF16) if hasattr(PS[i].t, "bitcast") else None

    def I(eng, name, *args, reads=(), writes=(), **kw):
        return P.op(eng, lambda e: getattr(e, name)(*args, **kw), reads=reads, writes=writes)

    def dma(q, out_ap, in_ap, reads, writes):
        return P.dma(q, lambda e: e.dma_start(out=out_ap, in_=in_ap), reads=reads, writes=writes)

    def cdma(out_ap, in_ap, reads, writes):
        return P.dma("pool", lambda e: e.dma_start(out=out_ap, in_=in_ap), reads=reads, writes=writes)

    pre_list = []

    def add_pre(w1, w3, w2, d1, d3, d2, F):
        hw = F // 2
        for kc in range(8):
            for hh in range(2):
                pre_list.append((d1[:, kc, hh * hw:(hh + 1) * hw], w1[kc * 128:(kc + 1) * 128, hh * hw:(hh + 1) * hw]))
                pre_list.append((d3[:, kc, hh * hw:(hh + 1) * hw], w3[kc * 128:(kc + 1) * 128, hh * hw:(hh + 1) * hw]))
        for fc in range(F // 128):
            pre_list.append((d2[:, fc, :], w2[fc * 128:(fc + 1) * 128, :]))

    add_pre(ffn_w1[0], ffn_w3[0], ffn_w2[0], FW1b, FW3b, FW2b, DFF)
    n_ffn_pre = len(pre_list)
    for e_ in range(n_exp):
        for q in range(4):
            r0_ = (e_ * 4 + q) * 128
            for kc in range(8):
                pre_list.append((MW1q[r0_:r0_ + 128, kc * 896:(kc + 1) * 896], moe_w1[e_][kc * 128:(kc + 1) * 128, q * 896:(q + 1) * 896]))
                pre_list.append((MW3q[r0_:r0_ + 128, kc * 896:(kc + 1) * 896], moe_w3[e_][kc * 128:(kc + 1) * 128, q * 896:(q + 1) * 896]))
            for j in range(7):
                pre_list.append((MW2q[r0_:r0_ + 128, j * D:(j + 1) * D], moe_w2[e_][(q * 7 + j) * 128:(q * 7 + j + 1) * 128, :]))
    pre_pos = [0]

    def precast_some(n):
        while n > 0 and pre_pos[0] < len(pre_list):
            d_, s_ = pre_list[pre_pos[0]]
            pre_pos[0] += 1
            n -= 1
            P.dma("pool", lambda e, d_=d_, s_=s_: e.dma_start(out=d_, in_=s_), reads=[], writes=["Wb"], bg=(pre_pos[0] > n_ffn_pre))

    g_base = A.mark()
    identb = sb("identb", [128, 128], BF16)
    identf = sb("identf", [128, 128], F32)
    ones = sb("ones", [128, 128], F32)
    zeros_bf = sb("zeros", [128, 8], BF16)
    RW = sb("RW", [128, 32, NEXP], F32)
    SEL = sb("SEL", [128, 32, NEXP], F32)
    OH1 = sb("OH1", [128, 32, NEXP], F32)
    dma("sp", identb[:], k_identb[:], [], [identb.k])
    dma("sp", identf[:], k_identf[:], [], [identf.k])
    I("dve", "memset", ones[:], 1.0, writes=[ones.k])
    I("dve", "memset", zeros_bf[:], 0.0, writes=[zeros_bf.k])
    phase_base = A.mark()

    def sqrt_recip(dst, src, scale, nparts=128):
        I("act", "activation", out=dst[:], in_=src[:], func=AF.Sqrt, bias=epsc[:], scale=scale,
             reads=[src.k, epsc.k], writes=[dst.k])
        I("dve", "reciprocal", out=dst[:], in_=dst[:], reads=[dst.k], writes=[dst.k])

    epsc = sb("epsc", [128, 1], F32)
    I("dve", "memset", epsc[:], EPS, writes=[epsc.k])
    phase_base = A.mark()

    def chk(l_, ph):
        if stop is not None and stop == (l_, ph):
            raise _Stop()

    def moe_sorted(l):
        A.reset(phase_base)
        precast_some(10 ** 6)
        P.barrier(include_bg=True)
        tab2 = sb("tab2", [128, 2, 128], F32)
        dma("sp", tab2[:], k_tab2[:], [], [tab2.k])
        ACCa = sb("ACCa", [128, 33, NEXP], F32)
        I("dve", "memset", ACCa[:, 0, :], 0.0, writes=[ACCa.k])
        for t in range(32):
            I("dve", "tensor_tensor", out=ACCa[:, t + 1, :], in0=ACCa[:, t, :], in1=SEL[:, t, :], op=ALU.add, reads=[ACCa.k, SEL.k], writes=[ACCa.k])
        pc_, pr_ = PS[0], PS[1]
        I("pe", "matmul", pc_[:, 0:NEXP], lhsT=ones[:], rhs=ACCa[:, 32, :], start=True, stop=True, reads=[ones.k, ACCa.k], writes=[pc_.k])
        sm = sb("sm", [128, 8, NEXP], F32)
        I("dve", "tensor_copy", out=sm[:, 0, :], in_=pc_[:, 0:NEXP], reads=[pc_.k], writes=[sm.k])
        I("dve", "tensor_scalar", out=sm[:, 1, :], in0=sm[:, 0, :], scalar1=0.5, scalar2=None, op0=ALU.is_gt, reads=[sm.k], writes=[sm.k])
        for j in range(1, 8):
            I("dve", "scalar_tensor_tensor", out=sm[:, 1, :], in0=sm[:, 0, :], scalar=UNIT * j + 0.5, in1=sm[:, 1, :], op0=ALU.is_gt, op1=ALU.add,
              reads=[sm.k], writes=[sm.k])
        I("dve", "tensor_copy", out=sm[:, 2, 0:1], in_=sm[:, 1, 0:1], reads=[sm.k], writes=[sm.k])
        for e_ in range(1, NEXP):
            I("dve", "tensor_tensor", out=sm[:, 2, e_:e_ + 1], in0=sm[:, 2, e_ - 1:e_], in1=sm[:, 1, e_:e_ + 1], op=ALU.add, reads=[sm.k], writes=[sm.k])
        I("dve", "tensor_tensor", out=sm[:, 3, :], in0=sm[:, 2, :], in1=sm[:, 1, :], op=ALU.subtract, reads=[sm.k], writes=[sm.k])
        I("dve", "tensor_scalar", out=sm[:, 4, :], in0=sm[:, 3, :], scalar1=UNIT / 128.0, scalar2=None, op0=ALU.mult, reads=[sm.k], writes=[sm.k])
        I("dve", "tensor_scalar", out=sm[:, 2, :], in0=sm[:, 2, :], scalar1=float(UNIT), scalar2=None, op0=ALU.mult, reads=[sm.k], writes=[sm.k])
        for t in range(32):
            cs = slice(t * NEXP, (t + 1) * NEXP)
            I("pe", "matmul", pr_[:, cs], lhsT=ones[:], rhs=ACCa[:, t, :], start=True, stop=False, reads=[ones.k, ACCa.k], writes=[pr_.k])
            I("pe", "matmul", pr_[:, cs], lhsT=ones[:], rhs=sm[:, 4, :], start=False, stop=False, reads=[ones.k, sm.k], writes=[pr_.k])
            I("pe", "matmul", pr_[:, cs], lhsT=tab2[:, 0, :], rhs=SEL[:, t, :], start=False, stop=True, reads=[tab2.k, SEL.k], writes=[pr_.k])
        DST = sb("DST", [128, 32, NEXP], F32)
        I("dve", "tensor_tensor", out=DST[:].rearrange("p t e -> p (t e)"), in0=pr_[:, 0:256], in1=SEL[:].rearrange("p t e -> p (t e)"), op=ALU.subtract,
          reads=[pr_.k, SEL.k], writes=[DST.k])
        OH2 = sb("OH2", [128, 32, NEXP], F32)
        I("dve", "tensor_tensor", out=OH2[:], in0=SEL[:], in1=OH1[:], op=ALU.subtract, reads=[SEL.k, OH1.k], writes=[OH2.k])
        tmpr = sb("tmpr", [128, 32, NEXP], F32)
        IDXf = sb("IDXf", [128, 2, 32], F32)
        WGT = sb("WGT", [128, 2, 32], F32)
        IDXi = sb("IDXi", [128, 2, 32], I32)
        for k_, oh in enumerate((OH1, OH2)):
            I("dve", "tensor_tensor", out=tmpr[:], in0=oh[:], in1=DST[:], op=ALU.mult, reads=[oh.k, DST.k], writes=[tmpr.k])
            I("dve", "tensor_reduce", out=IDXf[:, k_, :], in_=tmpr[:], axis=AX.X, op=ALU.add, reads=[tmpr.k], writes=[IDXf.k])
            I("dve", "tensor_tensor", out=tmpr[:], in0=oh[:], in1=RW[:], op=ALU.mult, reads=[oh.k, RW.k], writes=[tmpr.k])
            I("dve", "tensor_reduce", out=WGT[:, k_, :], in_=tmpr[:], axis=AX.X, op=ALU.add, reads=[tmpr.k], writes=[WGT.k])
        I("dve", "tensor_copy", out=IDXi[:], in_=IDXf[:], reads=[IDXf.k], writes=[IDXi.k])
        eu = sb("eu", [128, NUN], F32)
        pq = sb("pq", [128, 4], F32)
        for q in range(4):
            I("dve", "tensor_scalar", out=pq[:, q:q + 1], in0=tab2[:, 1, 0:1], scalar1=128.0 * q, scalar2=None, op0=ALU.add, reads=[tab2.k], writes=[pq.k])
        for u in range(NUN):
            I("dve", "tensor_scalar", out=sm[:, 5, :], in0=sm[:, 2, :], scalar1=float(UNIT) * u + 0.5, scalar2=None, op0=ALU.is_lt, reads=[sm.k], writes=[sm.k])
            I("dve", "tensor_reduce", out=eu[:, u:u + 1], in_=sm[:, 5, :], axis=AX.X, op=ALU.add, reads=[sm.k], writes=[eu.k])
        I("dve", "tensor_scalar", out=eu[:], in0=eu[:], scalar1=7.0, scalar2=None, op0=ALU.min, reads=[eu.k], writes=[eu.k])
        WIf = sb("WIf", [128, NUN, 4], F32)
        WIi = sb("WIi", [128, NUN, 4], I32)
        for q in range(4):
            I("dve", "tensor_scalar", out=WIf[:, :, q], in0=eu[:], scalar1=512.0, scalar2=pq[:, q:q + 1], op0=ALU.mult, op1=ALU.add,
              reads=[eu.k, pq.k], writes=[WIf.k])
        I("dve", "tensor_copy", out=WIi[:], in_=WIf[:], reads=[WIf.k], writes=[WIi.k])
        if debug:
            dbg_idx = nc.dram_tensor("dbg_idx", [128, 64], F32, kind="ExternalOutput").ap()
            dbg_w = nc.dram_tensor("dbg_w", [128, 64], F32, kind="ExternalOutput").ap()
            dbg_e = nc.dram_tensor("dbg_e", [128, NUN * 5], F32, kind="ExternalOutput").ap()
            dma("sp", dbg_idx[:], IDXf[:].rearrange("p k t -> p (k t)"), [IDXf.k], ["dbg1"])
            dma("sp", dbg_w[:], WGT[:].rearrange("p k t -> p (k t)"), [WGT.k], ["dbg2"])
            dma("sp", dbg_e[:, 0:NUN], eu[:], [eu.k], ["dbg3"])
            dma("sp", dbg_e[:, NUN:NUN * 5], WIf[:].rearrange("p u q -> p (u q)"), [WIf.k], ["dbg3"])
        gfl = sb("gfl", [128, D], F32)
        dma("sp", gfl[:], MODB[2 * l + 0, :, 5 * D:6 * D], ["MODB"], [gfl.k])
        persist = A.mark()
        import concourse.bass as _b
        H2bs = [sb("H2b%d" % i, [128, 8, UNIT], BF16) for i in range(2)]
        NWS = 3
        Wset = [(sb("W1q%d" % i, [128, 8, 896], BF16), sb("W3q%d" % i, [128, 8, 896], BF16), sb("W2q%d" % i, [128, 7, D], BF16)) for i in range(NWS)]
        actq = [sb("actq%d" % i, [128, 7, UNIT], BF16) for i in range(2)]
        yacc = sb("yacc", [128, UNIT // 128, D], F32)
        slt = [sb("slt%d" % i, [128, 512], F32) for i in range(4)]
        hst = [sb("hst%d" % i, [128, D], BF16) for i in range(2)]

        def L13(s_):
            w1q, w3q, _ = Wset[s_ % NWS]
            u, q = s_ // 4, s_ % 4
            for wt, src in ((w1q, MW1q), (w3q, MW3q)):
                P.dma("pool", lambda e, wt=wt, src=src, u=u, q=q: e.indirect_dma_start(
                    out=wt[:].rearrange("p k n -> p (k n)"), out_offset=None, in_=src[:, :],
                    in_offset=_b.IndirectOffsetOnAxis(ap=WIi[:, u, q:q + 1], axis=0)), reads=["Wb", WIi.k], writes=[wt.k])

        def L2(s_):
            w2q = Wset[s_ % NWS][2]
            u, q = s_ // 4, s_ % 4
            P.dma("pool", lambda e, w2q=w2q, u=u, q=q: e.indirect_dma_start(
                out=w2q[:].rearrange("p k n -> p (k n)"), out_offset=None, in_=MW2q[:, :],
                in_offset=_b.IndirectOffsetOnAxis(ap=WIi[:, u, q:q + 1], axis=0)), reads=["Wb", WIi.k], writes=[w2q.k])

        for s_ in range(NWS):
            L13(s_)
            L2(s_)
        dbufs = A.mark()
        htl = [sb("htl%d" % i, [128, D], BF16) for i in range(3)]
        for t in range(32):
            ht_ = htl[t % 3]
            dma("sp", ht_[:], H2T[t * 128:(t + 1) * 128, :], ["H2T"], [ht_.k])
            for k_ in range(2):
                P.dma("pool", lambda e, ht_=ht_, k_=k_, t=t: e.indirect_dma_start(
                    out=HS[:, :], out_offset=_b.IndirectOffsetOnAxis(ap=IDXi[:, k_, t:t + 1], axis=0), in_=ht_[:, :], in_offset=None),
                    reads=[ht_.k, IDXi.k], writes=["HS"])
        A.reset(dbufs)
        P.barrier()
        wkeep = [t.k for ws in Wset for t in ws]
        NU = NUN * 4
        cntd = {"ps": 0, "py": 0, "h": 0}

        def prep_unit(u):
            H2b = H2bs[u % 2]
            for tt in range(UNIT // 128):
                cntd["h"] += 1
                h_ = hst[cntd["h"] % 2]
                r0 = u * UNIT + tt * 128
                dma("sp", h_[:], HS[r0:r0 + 128, :], ["HS"], [h_.k])
                pst = PS[6 + cntd["h"] % 2]
                for kc in range(8):
                    I("pe", "transpose", out=pst[:].bitcast(BF16)[:, kc * 128:(kc + 1) * 128], in_=h_[:, kc * 128:(kc + 1) * 128], identity=identb[:],
                      reads=[h_.k, identb.k], writes=[pst.k])
                I("act", "activation", out=H2b[:, :, tt * 128:(tt + 1) * 128], in_=pst[:].bitcast(BF16)[:, 0:1024].rearrange("p (k t) -> p k t", k=8),
                  func=AF.Copy, reads=[pst.k], writes=[H2b.k])

        def S1(s_):
            w1q, w3q, _ = Wset[s_ % NWS]
            aq = actq[s_ % 2]
            u, q = s_ // 4, s_ % 4
            H2b = H2bs[u % 2]
            if s_ == 0:
                prep_unit(0)
            if q == 3 and u + 1 < NUN:
                prep_unit(u + 1)
            for j in range(7):
                for p0 in range(0, UNIT, 512):
                    cntd["ps"] += 1
                    n_ = cntd["ps"]
                    pa, pb = PS[(n_ % 2) * 2], PS[(n_ % 2) * 2 + 1]
                    for kc in range(8):
                        I("pe", "matmul", pa[:], lhsT=w1q[:, kc, j * 128:(j + 1) * 128], rhs=H2b[:, kc, p0:p0 + 512], start=(kc == 0), stop=(kc == 7),
                          reads=[w1q.k, H2b.k], writes=[pa.k])
                    for kc in range(8):
                        I("pe", "matmul", pb[:], lhsT=w3q[:, kc, j * 128:(j + 1) * 128], rhs=H2b[:, kc, p0:p0 + 512], start=(kc == 0), stop=(kc == 7),
                          reads=[w3q.k, H2b.k], writes=[pb.k])
                    sl_ = slt[n_ % len(slt)]
                    I("act", "activation", out=sl_[:], in_=pa[:], func=AF.Silu, reads=[pa.k], writes=[sl_.k])
                    I("dve", "tensor_tensor", out=aq[:, j, p0:p0 + 512], in0=sl_[:], in1=pb[:], op=ALU.mult, reads=[sl_.k, pb.k], writes=[aq.k + "_%d" % j])

        def S2(s_):
            w2q = Wset[s_ % NWS][2]
            aq = actq[s_ % 2]
            u, q = s_ // 4, s_ % 4
            for tt in range(UNIT // 128):
                ts_ = slice(tt * 128, (tt + 1) * 128)
                yk0 = yacc.k + "_%d" % tt
                for half in range(2):
                    yk = yk0 + "h%d" % half
                    cs = slice(half * 512, (half + 1) * 512)
                    cntd["py"] += 1
                    py = PS[4 + cntd["py"] % 2]
                    for j in range(7):
                        I("pe", "matmul", py[:], lhsT=aq[:, j, ts_], rhs=w2q[:, j, cs], start=(j == 0), stop=(j == 6), reads=[aq.k + "_%d" % j, w2q.k], writes=[py.k])
                    if q == 0:
                        I("act", "activation", out=yacc[:, tt, cs], in_=py[:], func=AF.Copy, reads=[py.k], writes=[yk])
                    else:
                        I("dve", "tensor_tensor", out=yacc[:, tt, cs], in0=py[:], in1=yacc[:, tt, cs], op=ALU.add, reads=[py.k, yk], writes=[yk])
                if q == 3:
                    r0 = u * UNIT + tt * 128
                    I("dve", "tensor_tensor", out=yacc[:, tt, :], in0=yacc[:, tt, :], in1=gfl[:], op=ALU.mult, reads=[yk0 + "h0", yk0 + "h1", gfl.k],
                      writes=[yk0 + "h0", yk0 + "h1"])
                    dma("sp", YS[r0:r0 + 128, :], yacc[:, tt, :], [yk0 + "h0", yk0 + "h1"], ["YS"])

        for s_ in range(NU):
            if s_ >= 1 and s_ + NWS - 1 < NU:
                L13(s_ + NWS - 1)
            S1(s_)
            if s_ >= 1:
                S2(s_ - 1)
                if s_ + NWS - 1 < NU:
                    L2(s_ + NWS - 1)
        S2(NU - 1)
        A.reset(persist)
        P.barrier()
        gfin = sb("gfin", [128, D], F32)
        dma("sp", gfin[:], g_final.rearrange("(o n) -> o n", o=1).broadcast_to([128, D]), [], [gfin.k])
        g1 = [sb("g1_%d" % i, [128, D], F32) for i in range(3)]
        g2 = [sb("g2_%d" % i, [128, D], F32) for i in range(3)]
        x1s = [sb("x1_%d" % i, [128, D], F32) for i in range(3)]
        junk = sb("junk", [128, D], F32)
        ssqs = [sb("ssq%d" % i, [128, 1], F32) for i in range(2)]
        rstds = [sb("rstd%d" % i, [128, 1], F32) for i in range(2)]
        for t in range(32):
            i2 = t % 2
            r0 = NCTX + t * 128
            x1, ssq, rstd, ga, gb_ = x1s[t % 3], ssqs[i2], rstds[i2], g1[t % 3], g2[t % 3]
            for k_, gt in enumerate((ga, gb_)):
                P.dma("pool", lambda e, gt=gt, k_=k_, t=t: e.indirect_dma_start(
                    out=gt[:, :], out_offset=None, in_=YS[:, :], in_offset=_b.IndirectOffsetOnAxis(ap=IDXi[:, k_, t:t + 1], axis=0)),
                    reads=["YS", IDXi.k], writes=[gt.k])
            dma("sp", x1[:], X1[r0:r0 + 128, :], ["X1"], [x1.k])
            I("act", "activation", out=ga[:], in_=ga[:], func=AF.Identity, scale=WGT[:, 0, t:t + 1], reads=[ga.k, WGT.k], writes=[ga.k])
            I("dve", "scalar_tensor_tensor", out=ga[:], in0=gb_[:], scalar=WGT[:, 1, t:t + 1], in1=ga[:], op0=ALU.mult, op1=ALU.add,
              reads=[gb_.k, WGT.k, ga.k], writes=[ga.k])
            I("dve", "tensor_tensor", out=x1[:], in0=x1[:], in1=ga[:], op=ALU.add, reads=[x1.k, ga.k], writes=[x1.k])
            I("act", "activation", out=junk[:], in_=x1[:], func=AF.Square, accum_out=ssq[:], reads=[x1.k], writes=[junk.k, ssq.k])
            sqrt_recip(rstd, ssq, 1.0 / D)
            I("dve", "scalar_tensor_tensor", out=x1[:], in0=x1[:], scalar=rstd[:], in1=gfin[:], op0=ALU.mult, op1=ALU.mult,
              reads=[x1.k, rstd.k, gfin.k], writes=[x1.k])
            dma("sp", out[r0 - NCTX:r0 - NCTX + 128, :], x1[:], [x1.k], ["OUT"])

    try:
      for l in range(n_layers):
        last = l == n_layers - 1 and n_layers == 2
        xsrc = xin if l == 0 else XS
        A.reset(phase_base)
        P.barrier()
        Win = sb("Win", [128, 8, 4096], BF16)
        for kc in range(8):
            for hh in range(2):
                cdma(Win[:, kc, hh * 2048:(hh + 1) * 2048], w_in[l, kc * 128:(kc + 1) * 128, hh * 2048:(hh + 1) * 2048], [], [Win.k])
        if l == 0:
            precast_some(n_ffn_pre)
        win_base = A.mark()
        cc_t = sb("cc", [128, 2, 8], F32)
        dma("sp", cc_t[:, 0, :], c_b.rearrange("(k p) -> p k", p=128), [], [cc_t.k])
        dma("sp", cc_t[:, 1, :], c_ctx.rearrange("(k p) -> p k", p=128), [], [cc_t.k])
        sc_t = sb("sc", [128, 2, 8], F32)
        I("act", "activation", out=sc_t[:], in_=cc_t[:], func=AF.Silu, reads=[cc_t.k], writes=[sc_t.k])
        rep = sb("rep", [128, 2, 8, 128], F32)
        for r in range(2):
            for kc in range(8):
                I("dve", "tensor_scalar", out=rep[:, r, kc, :], in0=ones[:], scalar1=sc_t[:, r, kc:kc + 1],
                                                                scalar2=None, op0=ALU.mult,
                     reads=[ones.k, sc_t.k], writes=[rep.k])
        bm = sb("bm", [128, 6 * D], F32)
        dma("sp", bm[:], b_mod[l:l + 1, :].broadcast_to([128, 6 * D]), [], [bm.k])
        gmx = sb("gmx", [128, D], F32)
        gff = sb("gff", [128, D], F32)
        dma("sp", gmx[:], g_mix[l:l + 1, :].broadcast_to([128, D]), [], [gmx.k])
        dma("sp", gff[:], g_ffn[l:l + 1, :].broadcast_to([128, D]), [], [gff.k])
        modt = [sb("modt%d" % r, [128, 6 * D], F32) for r in range(2)]
        wms = [sb("wm%d" % i, [128, 8, 512], F32) for i in range(2)]
        for cc in range(12):
            wm = wms[cc % 2]
            dma("sp", wm[:], w_mod[l, :, cc * 512:(cc + 1) * 512].rearrange("(k p) n -> p k n", p=128), [], [wm.k])
            for r in range(2):
                ps = PS[(cc * 2 + r) % 4]
                for kc in range(8):
                    I("pe", "matmul", ps[:], lhsT=rep[:, r, kc, :], rhs=wm[:, kc, :],
                                                                           start=(kc == 0), stop=(kc == 7),
                         reads=[rep.k, wm.k], writes=[ps.k])
                I("dve", "tensor_tensor", out=modt[r][:, cc * 512:(cc + 1) * 512], in0=ps[:],
                                                                       in1=bm[:, cc * 512:(cc + 1) * 512], op=ALU.add,
                     reads=[ps.k, bm.k], writes=[modt[r].k])
        for r in range(2):
            I("dve", "scalar_tensor_tensor", out=modt[r][:, D:2 * D], in0=modt[r][:, D:2 * D], scalar=1.0, in1=gmx[:],
                                                             op0=ALU.add, op1=ALU.mult,
                 reads=[modt[r].k, gmx.k], writes=[modt[r].k])
            I("dve", "scalar_tensor_tensor", out=modt[r][:, 4 * D:5 * D], in0=modt[r][:, 4 * D:5 * D], scalar=1.0,
                                                             in1=gff[:], op0=ALU.add, op1=ALU.mult,
                 reads=[modt[r].k, gff.k], writes=[modt[r].k])
            dma("sp", MODB[2 * l + r], modt[r][:], [modt[r].k], ["MODB"])

        def load_mod(slot, name):
            ts = []
            for r in range(2):
                t = sb("%s%d" % (name, r), [128, D], F32)
                dma("sp", t[:], MODB[2 * l + r, :, slot * D:(slot + 1) * D], ["MODB"], [t.k])
                ts.append(t)
            return ts

        def modsel(ts, g0):
            return ts[1] if g0 < NCTX else ts[0]

        chk(l, "M")
        A.reset(win_base)
        P.barrier()
        P.lastw[Win.k] = None
        if l == 1:
            HSv = HS.rearrange("(p r) n -> p (r n)", p=128)
            for zi in range(NROWS // 128 * D // 4096):
                dma("sp", HSv[:, zi * 4096:(zi + 1) * 4096], k_zero[:, :], [], ["HS"])
        G1 = load_mod(1, "G1")
        SH1 = load_mod(0, "SH1")
        for (c0, w) in ((0, 2), (258, 3), (UW - 3, 3)):
            for ch in range(4):
                dma("sp", U[ch * 128:(ch + 1) * 128, c0:c0 + w], zeros_bf[:, 0:w], [zeros_bf.k], ["U"])
        xts = [sb("xt%d" % i, [128, D], F32) for i in range(8)]
        junk = sb("junk", [128, D], F32)
        t1s = [sb("t1_%d" % i, [128, D], F32) for i in range(2)]
        hts = [sb("ht%d" % i, [128, D], BF16) for i in range(2)]
        ssqs = [sb("ssq%d" % i, [128, 1], F32) for i in range(2)]
        rstds = [sb("rstd%d" % i, [128, 1], F32) for i in range(2)]
        hfms = [sb("hfm%d" % i, [128, 8, 512], BF16) for i in range(2)]
        cos_t = [sb("cos%d" % i, [128, 512], F32) for i in range(2)]
        sin_t = [sb("sin%d" % i, [128, 512], F32) for i in range(2)]
        ust = [sb("ust%d" % i, [128, 512], BF16) for i in range(6)]
        qks = [sb("qks%d" % i, [128, 4, 512], BF16) for i in range(2)]
        ra = [sb("ra%d" % i, [128, 512], F32) for i in range(2)]
        rb = [sb("rb%d" % i, [128, 512], F32) for i in range(2)]
        tok_st = [sb("tokst%d" % i, [128, 512], BF16) for i in range(6)]
        cnt = {"x": 0, "u": 0, "r": 0, "ps": 0, "tk": 0}

        def a_loads(mi):
            g0, T_ = MACROS[mi]
            ct, stb = cos_t[mi % 2], sin_t[mi % 2]
            for tt in range(T_ // 128):
                xt = xts[(mi % 2) * 4 + tt]
                r0 = g0 + tt * 128
                precast_some(4)
                dma("sp", xt[:], xsrc[r0:r0 + 128, :], ["XS"], [xt.k])
            dma("sp", ct[:, 0:T_], k_cos[:, g0:g0 + T_], [], [ct.k])
            dma("sp", stb[:, 0:T_], k_sin[:, g0:g0 + T_], [], [stb.k])

        def a_chain(mi, tt):
            g0, T_ = MACROS[mi]
            g1, sh1 = modsel(G1, g0), modsel(SH1, g0)
            i = (mi * 4 + tt) % 2
            xt = xts[(mi % 2) * 4 + tt]
            t1, ht, ssq, rstd = t1s[i], hts[i], ssqs[i], rstds[i]
            I("act", "activation", out=junk[:], in_=xt[:], func=AF.Square, accum_out=ssq[:], reads=[xt.k], writes=[junk.k, ssq.k])
            sqrt_recip(rstd, ssq, 1.0 / D)
            I("dve", "scalar_tensor_tensor", out=t1[:], in0=xt[:], scalar=rstd[:], in1=g1[:], op0=ALU.mult, op1=ALU.mult,
              reads=[xt.k, rstd.k, g1.k], writes=[t1.k])
            I("dve", "tensor_tensor", out=ht[:], in0=t1[:], in1=sh1[:], op=ALU.add, reads=[t1.k, sh1.k], writes=[ht.k])

        def a_xpose(mi, tt):
            i = (mi * 4 + tt) % 2
            ht = hts[i]
            hfm = hfms[mi % 2]
            pst = PS[6 + i]
            for kc in range(8):
                I("pe", "transpose", out=pst[:].bitcast(BF16)[:, kc * 128:(kc + 1) * 128], in_=ht[:, kc * 128:(kc + 1) * 128], identity=identb[:],
                  reads=[ht.k, identb.k], writes=[pst.k])
            I("act", "activation", out=hfm[:, :, tt * 128:(tt + 1) * 128], in_=pst[:].bitcast(BF16)[:, 0:1024].rearrange("p (k t) -> p k t", k=8),
              func=AF.Copy, reads=[pst.k], writes=[hfm.k])

        def nps():
            cnt["ps"] += 1
            return PS[cnt["ps"] % 4]

        def a_section(mi, sec):
            g0, T_ = MACROS[mi]
            hfm = hfms[mi % 2]
            ct, stb = cos_t[mi % 2], sin_t[mi % 2]

            def proj_fm(oc, ps):
                for kc in range(8):
                    I("pe", "matmul", ps[:, 0:T_], lhsT=Win[:, kc, oc * 128:(oc + 1) * 128], rhs=hfm[:, kc, 0:T_], start=(kc == 0), stop=(kc == 7),
                      reads=[Win.k, hfm.k], writes=[ps.k])

            if sec == 0:
                for oc in range(8):
                    ps = nps()
                    proj_fm(oc, ps)
                    u_ = ust[cnt["u"] % 6]
                    cnt["u"] += 1
                    fn = AF.Copy if oc < 4 else AF.Gelu
                    I("act", "activation", out=u_[:, 0:T_], in_=ps[:, 0:T_], func=fn, reads=[ps.k], writes=[u_.k])
                    if oc < 4:
                        dma("sp", U[oc * 128:(oc + 1) * 128, ucol(g0):ucol(g0) + T_], u_[:, 0:T_], [u_.k], ["U"])
                    else:
                        dma("sp", GY[(oc - 4) * 128:(oc - 3) * 128, g0:g0 + T_], u_[:, 0:T_], [u_.k], ["GY"])
            elif sec in (1, 2):
                qk = sec - 1
                st = qks[qk]
                for h in range(4):
                    psq = nps()
                    proj_fm(8 + qk * 4 + h, psq)
                    pss = nps()
                    proj_fm(24 + qk * 4 + h, pss)
                    a_, b_ = ra[cnt["r"] % 2], rb[cnt["r"] % 2]
                    cnt["r"] += 1
                    sc = 1.0 if qk == 0 else 128.0 ** -0.5
                    I("dve", "scalar_tensor_tensor", out=a_[:, 0:T_], in0=psq[:, 0:T_], scalar=sc, in1=ct[:, 0:T_], op0=ALU.mult, op1=ALU.mult,
                      reads=[psq.k, ct.k], writes=[a_.k])
                    I("dve", "scalar_tensor_tensor", out=b_[:, 0:T_], in0=pss[:, 0:T_], scalar=sc, in1=stb[:, 0:T_], op0=ALU.mult, op1=ALU.mult,
                      reads=[pss.k, stb.k], writes=[b_.k])
                    I("dve", "tensor_tensor", out=st[:, h, 0:T_], in0=a_[:, 0:T_], in1=b_[:, 0:T_], op=ALU.add, reads=[a_.k, b_.k], writes=[st.k + "_%d" % h])
                dst = Q if qk == 0 else K
                dma("sp", dst[:, g0:g0 + T_].rearrange("(h d) t -> d h t", d=128), st[:, :, 0:T_], [st.k + "_%d" % h_ for h_ in range(4)], ["Q" if qk == 0 else "K"])
            else:
                kst = qks[1]
                for tt in range(T_ // 128):
                    r0 = g0 + tt * 128
                    pst = PS[4]
                    for h in range(4):
                        I("pe", "transpose", out=pst[:].bitcast(BF16)[:, h * 128:(h + 1) * 128], in_=kst[:, h, tt * 128:(tt + 1) * 128], identity=identb[:],
                          reads=[kst.k + "_%d" % h, identb.k], writes=[pst.k])
                    tk = tok_st[cnt["tk"] % 6]
                    cnt["tk"] += 1
                    I("dve", "tensor_copy", out=tk[:], in_=pst[:].bitcast(BF16)[:, 0:512], reads=[pst.k], writes=[tk.k])
                    dma("sp", KT[r0:r0 + 128, :], tk[:], [tk.k], ["KT"])
                    for which in range(2):
                        ps = PS[5] if which == 0 else nps()
                        c0 = 2048 + which * 512
                        for kc in range(8):
                            I("pe", "matmul", ps[:], lhsT=hfm[:, kc, tt * 128:(tt + 1) * 128], rhs=Win[:, kc, c0:c0 + 512], start=(kc == 0), stop=(kc == 7),
                              reads=[hfm.k, Win.k], writes=[ps.k])
                        tk = tok_st[cnt["tk"] % 6]
                        cnt["tk"] += 1
                        fn = AF.Copy if which == 0 else AF.Silu
                        I("act", "activation", out=tk[:], in_=ps[:], func=fn, reads=[ps.k], writes=[tk.k])
                        dma("sp", (VT if which == 0 else SGT)[r0:r0 + 128, :], tk[:], [tk.k], ["VT" if which == 0 else "SGT"])

        NM = len(MACROS)
        a_loads(0)
        a_loads(1)
        for tt in range(MACROS[0][1] // 128):
            a_chain(0, tt)
            a_xpose(0, tt)
        for mi in range(NM):
            nxt = mi + 1 if mi + 1 < NM else None
            ntn = (MACROS[nxt][1] // 128) if nxt is not None else 0
            for sec in range(4):
                if nxt is not None and sec < ntn:
                    a_chain(nxt, sec)
                a_section(mi, sec)
                if nxt is not None and sec < ntn:
                    a_xpose(nxt, sec)
            if mi + 2 < NM:
                a_loads(mi + 2)

        A.reset(phase_base)
        P.barrier()
        tab = sb("tab", [128, 8, 128], F32)
        dma("sp", tab[:], k_tab[:], [], [tab.k])
        rd = sb("rd", [128, 8], F32)
        dma("sp", rd[:], ret_decay[l:l + 1, :].broadcast_to([128, 8]), [], [rd.k])
        lg = sb("lg", [128, 8], F32)
        I("act", "activation", out=lg[:], in_=rd[:], func=AF.Exp, scale=-1.0, reads=[rd.k], writes=[lg.k])
        I("act", "activation", out=lg[:], in_=lg[:], func=AF.Ln, bias=ones[:, 0:1], reads=[lg.k, ones.k], writes=[lg.k])
        I("dve", "tensor_scalar", out=lg[:], in0=lg[:], scalar1=-1.0, scalar2=None, op0=ALU.mult, reads=[lg.k], writes=[lg.k])
        kd = sb("kd", [128, 8], F32)
        cd = sb("cd", [128, 8], F32)
        for dr in range(2):
            for h in range(4):
                j = dr * 4 + h
                I("act", "activation", out=kd[:, j:j + 1], in_=tab[:, 6 + dr, 0:1], func=AF.Exp, scale=lg[:, j:j + 1],
                     reads=[tab.k, lg.k], writes=[kd.k])
        I("act", "activation", out=cd[:], in_=lg[:], func=AF.Exp, scale=128.0, reads=[lg.k], writes=[cd.k])
        KDT = [sb("KDT%d" % i, [128, 512], F32) for i in range(2)]
        for dr in range(2):
            for h in range(4):
                I("dve", "tensor_scalar", out=KDT[dr][:, h * 128:(h + 1) * 128], in0=ones[:], scalar1=kd[:, dr * 4 + h:dr * 4 + h + 1], scalar2=None,
                  op0=ALU.mult, reads=[ones.k, kd.k], writes=[KDT[dr].k])
        Sd = [sb("S%d" % i, [128, 512], F32) for i in range(2)]
        Sb16 = [sb("Sb16_%d" % i, [128, 512], BF16) for i in range(4)]
        ktl = [sb("ktl%d" % i, [128, 512], BF16) for i in range(8)]
        vtl = [sb("vtl%d" % i, [128, 512], BF16) for i in range(8)]
        kts = [sb("kts%d" % i, [128, 512], BF16) for i in range(4)]
        orders = [list(range(NCH)), [1, 0] + list(range(NCH - 1, 1, -1))]
        for dr in range(2):
            I("dve", "memset", Sd[dr][:], 0.0, writes=[Sd[dr].k + "_%d" % h_ for h_ in range(4)])

        def a2_loads(step):
            for dr in range(2):
                c = orders[dr][step]
                i8 = (step % 4) * 2 + dr
                dma("sp", ktl[i8][:], KT[c * 128:(c + 1) * 128, :], ["KT"], [ktl[i8].k])
                dma("sp", vtl[i8][:], VT[c * 128:(c + 1) * 128, :], ["VT"], [vtl[i8].k])

        for step in range(3):
            a2_loads(step)
        a2n = [0, 0]

        def a2_step(step):
            if step + 3 < NCH:
                a2_loads(step + 3)
            for dr in range(2):
                n = a2n[0] = a2n[0] + 1
                c = orders[dr][step]
                S = Sd[dr]
                dst = SF if dr == 0 else SB
                i = (step % 2) * 2 + dr
                i8 = (step % 4) * 2 + dr
                s16, kt_, vt_, ks_ = Sb16[i], ktl[i8], vtl[i8], kts[i]
                I("act", "activation", out=s16[:], in_=S[:], func=AF.Copy, reads=[S.k + "_%d" % h_ for h_ in range(4)], writes=[s16.k])
                dma("sp", dst[c], s16[:], [s16.k], ["SF" if dr == 0 else "SB"])
                ps = PS[6 + n % 2]
                I("dve", "tensor_tensor", out=ks_[:], in0=kt_[:], in1=KDT[dr][:], op=ALU.mult, reads=[kt_.k, KDT[dr].k], writes=[ks_.k])
                for h in range(4):
                    I("pe", "matmul", ps[:, h * 128:(h + 1) * 128], lhsT=ks_[:, h * 128:(h + 1) * 128], rhs=vt_[:, h * 128:(h + 1) * 128],
                      start=True, stop=True, reads=[ks_.k, vt_.k], writes=[ps.k])
                for h in range(4):
                    j = dr * 4 + h
                    I("dve", "scalar_tensor_tensor", out=S[:, h * 128:(h + 1) * 128], in0=S[:, h * 128:(h + 1) * 128], scalar=cd[:, j:j + 1],
                      in1=ps[:, h * 128:(h + 1) * 128], op0=ALU.mult, op1=ALU.add, reads=[S.k + "_%d" % h, cd.k, ps.k], writes=[S.k + "_%d" % h])

        def a2_advance(k):
            while k > 0 and a2n[1] < NCH:
                a2_step(a2n[1])
                a2n[1] += 1
                k -= 1

        cw = sb("cw", [128, 4, 4], F32)
        dma("sp", cw[:], conv_w[l].rearrange("k (c p) -> p k c", p=128), [], [cw.k])
        cb = sb("cb", [128, 4], F32)
        dma("sp", cb[:], conv_b[l].rearrange("(c p) -> p c", p=128), [], [cb.k])
        gb = sb("gb", [128, 3, 2, 4], F32)
        for wi, src in enumerate((rg_ba, rg_bx, rg_lam)):
            dma("sp", gb[:, wi], src[l].rearrange("d (c p) -> p d c", p=128), [], [gb.k])
        cv = sb("cv", [128, 2, 4], F32)
        I("act", "activation", out=cv[:], in_=gb[:, 2], func=AF.Exp, scale=-1.0, reads=[gb.k], writes=[cv.k])
        I("act", "activation", out=cv[:], in_=cv[:], func=AF.Ln, bias=ones[:, 0:1], reads=[cv.k, ones.k], writes=[cv.k])
        I("dve", "tensor_scalar", out=cv[:], in0=cv[:], scalar1=-8.0, scalar2=None, op0=ALU.mult, reads=[cv.k], writes=[cv.k])
        Up = sb("Up", [128, UW], BF16)
        gyt = sb("gyt", [128, NT], BF16)
        uc32 = sb("uc32", [128, NT], F32)
        ucb = sb("ucb", [128, NT], BF16)
        AB = [[sb("A%d" % d_, [128, NT], F32), sb("B%d" % d_, [128, NT], F32)] for d_ in range(2)]
        Hd = [sb("H%d" % d_, [128, NT], F32) for d_ in range(2)]
        dg = [sb("dg%d" % k_, [128, 128], BF16) for k_ in range(4)]
        bd = [[sb("bd%d%d" % (d_, w_), [128, 128], BF16) for w_ in range(2)] for d_ in range(2)]
        rgs = [sb("rgs%d" % i, [128, 512], BF16) for i in range(2)]
        n = 0
        nb = [0]

        def b_prep(c):
            dma("sp", Up[:], U[c * 128:(c + 1) * 128, :], ["U"], [Up.k])
            for k_ in range(4):
                I("dve", "tensor_scalar", out=dg[k_][:], in0=identf[:], scalar1=cw[:, k_, c:c + 1], scalar2=None, op0=ALU.mult,
                  reads=[identf.k, cw.k], writes=[dg[k_].k])
            for d_ in range(2):
                for w_, src in enumerate((rg_wa, rg_wx)):
                    t_ = bd[d_][w_]
                    I("pool", "memset", t_[:], 0.0, writes=[t_.k])
                    for hb in range(2):
                        cdma(t_[hb * 64:(hb + 1) * 64, hb * 64:(hb + 1) * 64], src[l, d_, 2 * c + hb], [], [t_.k])

        def b_conv(c):
            for (g0, T_) in MACROS:
                nb[0] += 1
                n = nb[0]
                if n % 2 == 0:
                    a2_advance(1)
                ps = PS[n % 2]
                for k_ in range(4):
                    c0 = ucol(g0) + k_ - 2
                    I("pe", "matmul", ps[:, 0:T_], lhsT=dg[k_][:], rhs=Up[:, c0:c0 + T_], start=(k_ == 0), stop=(k_ == 3),
                      reads=[dg[k_].k, Up.k], writes=[ps.k])
                I("act", "activation", out=uc32[:, g0:g0 + T_], in_=ps[:, 0:T_], func=AF.Identity, bias=cb[:, c:c + 1],
                  reads=[ps.k, cb.k], writes=[uc32.k + "_%d" % g0])
                I("act", "activation", out=ucb[:, g0:g0 + T_], in_=ps[:, 0:T_], func=AF.Identity, bias=cb[:, c:c + 1],
                  reads=[ps.k, cb.k], writes=[ucb.k + "_%d" % g0])

        b_prep(0)
        b_conv(0)
        for c in range(4):
            dma("sp", gyt[:], GY[c * 128:(c + 1) * 128, :], ["GY"], [gyt.k])
            AK = [[AB[d_][0].k + "_%d" % g0 for (g0, T_) in MACROS] for d_ in range(2)]
            BK = [[AB[d_][1].k + "_%d" % g0 for (g0, T_) in MACROS] for d_ in range(2)]
            for mi_, (g0, T_) in enumerate(MACROS):
                a2_advance(1)
                for d_ in range(2):
                    n += 1
                    pr, pi = PS[2 + (n % 2) * 2], PS[3 + (n % 2) * 2]
                    Aa, Bb = AB[d_]
                    I("pe", "matmul", pr[:, 0:T_], lhsT=bd[d_][0][:], rhs=ucb[:, g0:g0 + T_], start=True, stop=True,
                      reads=[bd[d_][0].k, ucb.k + "_%d" % g0], writes=[pr.k])
                    I("pe", "matmul", pi[:, 0:T_], lhsT=bd[d_][1][:], rhs=ucb[:, g0:g0 + T_], start=True, stop=True,
                      reads=[bd[d_][1].k, ucb.k + "_%d" % g0], writes=[pi.k])
                    I("act", "activation", out=Aa[:, g0:g0 + T_], in_=pr[:, 0:T_], func=AF.Sigmoid, bias=gb[:, 0, d_, c:c + 1],
                      reads=[pr.k, gb.k], writes=[AK[d_][mi_]])
                    I("act", "activation", out=Bb[:, g0:g0 + T_], in_=pi[:, 0:T_], func=AF.Sigmoid, bias=gb[:, 1, d_, c:c + 1],
                      reads=[pi.k, gb.k], writes=[BK[d_][mi_]])
            uall = [uc32.k + "_%d" % g0 for (g0, T_) in MACROS]
            for d_ in range(2):
                Aa, Bb = AB[d_]
                I("act", "activation", out=Aa[:], in_=Aa[:], func=AF.Exp, scale=cv[:, d_, c:c + 1], reads=AK[d_] + [cv.k], writes=AK[d_])
            for d_ in range(2):
                Aa, Bb = AB[d_]
                I("act", "activation", out=Hd[d_][:], in_=Aa[:], func=AF.Square, reads=AK[d_], writes=[Hd[d_].k])
                I("dve", "tensor_tensor", out=Bb[:], in0=Bb[:], in1=uc32[:], op=ALU.mult, reads=BK[d_] + uall, writes=BK[d_])
            if c + 1 < 4:
                b_prep(c + 1)
            for d_ in range(2):
                I("act", "activation", out=Hd[d_][:], in_=Hd[d_][:], func=AF.Sqrt, scale=-1.0, bias=ones[:, 0:1],
                  reads=[Hd[d_].k, ones.k], writes=[Hd[d_].k])
            for d_ in range(2):
                Aa, Bb = AB[d_]
                I("dve", "tensor_tensor", out=Bb[:], in0=Bb[:], in1=Hd[d_][:], op=ALU.mult, reads=BK[d_] + [Hd[d_].k], writes=BK[d_])
            Aa, Bb = AB[0]
            I("dve", "tensor_tensor_scan", out=Hd[0][:, 0:NCTX], data0=Aa[:, 0:NCTX], data1=Bb[:, 0:NCTX], initial=0.0,
                                                                  op0=ALU.mult, op1=ALU.add, reads=AK[0] + BK[0], writes=[Hd[0].k])
            I("dve", "tensor_tensor_scan", out=Hd[0][:, NCTX:NT], data0=Aa[:, NCTX:NT], data1=Bb[:, NCTX:NT],
                                                                  initial=Hd[0][:, NCTX - 1:NCTX], op0=ALU.mult, op1=ALU.add,
                 reads=AK[0] + BK[0] + [Hd[0].k], writes=[Hd[0].k])
            Aa, Bb = AB[1]
            I("dve", "tensor_tensor_scan", out=Hd[1][:, NCTX - 1::-1], data0=Aa[:, NCTX - 1::-1], data1=Bb[:, NCTX - 1::-1],
                                                                  initial=0.0, op0=ALU.mult, op1=ALU.add, reads=AK[1] + BK[1], writes=[Hd[1].k])
            I("dve", "tensor_tensor_scan", out=Hd[1][:, NT - 1:NCTX - 1:-1], data0=Aa[:, NT - 1:NCTX - 1:-1],
                                                                  data1=Bb[:, NT - 1:NCTX - 1:-1], initial=Hd[1][:, 0:1], op0=ALU.mult, op1=ALU.add,
                 reads=AK[1] + BK[1] + [Hd[1].k], writes=[Hd[1].k])
            if c + 1 < 4:
                b_conv(c + 1)
            I("dve", "tensor_tensor", out=Hd[0][:], in0=Hd[0][:], in1=Hd[1][:], op=ALU.add, reads=[Hd[0].k, Hd[1].k], writes=[Hd[0].k])
            for (g0, T_) in MACROS:
                n += 1
                rg_ = rgs[n % 2]
                I("dve", "tensor_tensor", out=rg_[:, 0:T_], in0=Hd[0][:, g0:g0 + T_], in1=gyt[:, g0:g0 + T_], op=ALU.mult,
                  reads=[Hd[0].k, gyt.k], writes=[rg_.k])
                dma("sp", YRG[c * 128:(c + 1) * 128, g0:g0 + T_], rg_[:, 0:T_], [rg_.k], ["YRG"])

        a2_advance(NCH)
        A.reset(phase_base)
        P.barrier()
        Wout = sb("Wout", [128, 8, D], BF16)
        for kc in range(8):
            cdma(Wout[:, kc, :], w_out[l, kc * 128:(kc + 1) * 128, :], [], [Wout.k])
        tab = sb("tab", [128, 8, 128], F32)
        dma("sp", tab[:], k_tab[:], [], [tab.k])
        rd = sb("rd", [128, 8], F32)
        dma("sp", rd[:], ret_decay[l:l + 1, :].broadcast_to([128, 8]), [], [rd.k])
        lg = sb("lg", [128, 8], F32)
        I("act", "activation", out=lg[:], in_=rd[:], func=AF.Exp, scale=-1.0, reads=[rd.k], writes=[lg.k])
        I("act", "activation", out=lg[:], in_=lg[:], func=AF.Ln, bias=ones[:, 0:1], reads=[lg.k, ones.k], writes=[lg.k])
        I("dve", "tensor_scalar", out=lg[:], in0=lg[:], scalar1=-1.0, scalar2=None, op0=ALU.mult, reads=[lg.k], writes=[lg.k])
        maskT = sb("maskT", [128, 4, 128], F32)
        mtmp = sb("mtmp", [128, 128], F32)
        QFt = sb("QFt", [128, 4, 128], F32)
        QBt = sb("QBt", [128, 4, 128], F32)
        for h in range(4):
            I("act", "activation", out=maskT[:, h, :], in_=tab[:, 0, :], func=AF.Exp, scale=lg[:, h:h + 1],
                 reads=[tab.k, lg.k], writes=[maskT.k])
            I("dve", "tensor_tensor", out=maskT[:, h, :], in0=maskT[:, h, :], in1=tab[:, 1, :], op=ALU.mult,
                 reads=[maskT.k, tab.k], writes=[maskT.k])
            I("act", "activation", out=mtmp[:], in_=tab[:, 2, :], func=AF.Exp, scale=lg[:, 4 + h:5 + h],
                 reads=[tab.k, lg.k], writes=[mtmp.k])
            I("dve", "tensor_tensor", out=mtmp[:], in0=mtmp[:], in1=tab[:, 3, :], op=ALU.mult, reads=[mtmp.k, tab.k], writes=[mtmp.k])
            I("dve", "tensor_tensor", out=maskT[:, h, :], in0=maskT[:, h, :], in1=mtmp[:], op=ALU.add,
                 reads=[maskT.k, mtmp.k], writes=[maskT.k])
            I("act", "activation", out=QFt[:, h, :], in_=tab[:, 4, :], func=AF.Exp, scale=lg[:, h:h + 1],
                 reads=[tab.k, lg.k], writes=[QFt.k])
            I("act", "activation", out=QBt[:, h, :], in_=tab[:, 5, :], func=AF.Exp, scale=lg[:, 4 + h:5 + h],
                 reads=[tab.k, lg.k], writes=[QBt.k])
        GM = load_mod(2, "GM")
        G2 = load_mod(4, "G2")
        SH2 = load_mod(3, "SH2")
        moe_layer = (l == 1)
        if moe_layer:
            wr = sb("wr", [128, 8, 128], F32)
            I("dve", "memset", wr[:], 0.0, writes=[wr.k])
            dma("sp", wr[:, :, 0:NEXP], moe_router.rearrange("(k p) n -> p k n", p=128), [], [wr.k])
            brt = sb("brt", [128, NEXP], F32)
            dma("sp", brt[:], moe_router_b.rearrange("(o n) -> o n", o=1).broadcast_to([128, NEXP]), [], [brt.k])
        Qm = [sb("Qm%d" % i, [128, 4, 512], BF16) for i in range(2)]
        Km = [sb("Km%d" % i, [128, 4, 512], BF16) for i in range(2)]
        Qf = [sb("Qf%d" % i, [128, 4, 512], BF16) for i in range(2)]
        Qb = [sb("Qb%d" % i, [128, 4, 512], BF16) for i in range(2)]
        Yrg = [sb("Yrg%d" % i, [128, 4, 512], BF16) for i in range(2)]
        Yret = [sb("Yret%d" % i, [128, 4, 512], BF16) for i in range(2)]
        H2st = [sb("H2st%d" % i, [128, 8, 512], BF16) for i in range(2)] if not moe_layer else [None, None]
        stm = [sb("stm%d" % i, [128, 512], BF16) for i in range(2)]
        xc = [sb("xc%d" % i, [128, 512], F32) for i in range(2)]
        rett = [sb("rett%d" % i, [128, 512], BF16) for i in range(2)]
        small = [sb("small%d" % i, [128, 16], F32) for i in range(2)]
        junk2 = sb("junk2", [128, 128], F32)
        t2s = [sb("t2_%d" % i, [128, D], F32) for i in range(2)]
        junk = sb("junk", [128, D], F32)
        ssqs = [sb("ssq%d" % i, [128, 1], F32) for i in range(2)]
        rstds = [sb("rstd%d" % i, [128, 1], F32) for i in range(2)]
        h2f = [sb("h2f%d" % i, [128, 8, 128], F32) for i in range(2)] if moe_layer else None
        rt = [sb("rt%d" % i, [128, 6, NEXP], F32) for i in range(2)]
        rsm = [sb("rsm%d" % i, [128, 8], F32) for i in range(2)]
        h2tb = [sb("h2tb%d" % i, [128, D], BF16) for i in range(2)]
        cl3 = [[sb("cl3_%d_%d" % (i, j), [128, 512], BF16) for j in range(4)] for i in range(3)]
        xts4 = [sb("xt4_%d" % i, [128, D], F32) for i in range(8)]
        x1s4 = [sb("x1q_%d" % i, [128, D], F32) for i in range(4)]
        chunks = [(mi, ci) for mi, (g0, T_) in enumerate(MACROS) for ci in range(T_ // 128)]

        def c_macro_loads(mi):
            g0, T_ = MACROS[mi]
            dma("sp", Qm[mi % 2][:, :, 0:T_], Q[:, g0:g0 + T_].rearrange("(h d) t -> d h t", d=128), ["Q"], [Qm[mi % 2].k])
            dma("sp", Km[mi % 2][:, :, 0:T_], K[:, g0:g0 + T_].rearrange("(h d) t -> d h t", d=128), ["K"], [Km[mi % 2].k])
            dma("sp", Yrg[mi % 2][:, :, 0:T_], YRG[:, g0:g0 + T_].rearrange("(c p) t -> p c t", p=128), ["YRG"], [Yrg[mi % 2].k])

        def c_chunk_loads(n_):
            mi, ci = chunks[n_]
            gc = MACROS[mi][0] // 128 + ci
            vt_, sg_, sf_, sb_ = cl3[n_ % 3]
            dma("sp", vt_[:], VT[gc * 128:(gc + 1) * 128, :], ["VT"], [vt_.k])
            dma("sp", sg_[:], SGT[gc * 128:(gc + 1) * 128, :], ["SGT"], [sg_.k])
            dma("sp", sf_[:], SF[gc], ["SF"], [sf_.k])
            dma("sp", sb_[:], SB[gc], ["SB"], [sb_.k])
            if not (MACROS[mi][0] < NCTX and last):
                r0 = gc * 128
                dma("sp", xts4[n_ % 8][:], xsrc[r0:r0 + 128, :], ["XS"], [xts4[n_ % 8].k])

        c_macro_loads(0)
        c_macro_loads(1)
        c_chunk_loads(0)
        c_chunk_loads(1)
        cnt_c = [0]
        ntl = [0]
        for mi, (g0, T_) in enumerate(MACROS):
            is_ctx = g0 < NCTX
            qm, km, qf, qb, yrg, yret, h2st = Qm[mi % 2], Km[mi % 2], Qf[mi % 2], Qb[mi % 2], Yrg[mi % 2], Yret[mi % 2], H2st[mi % 2]
            for ci in range(T_ // 128):
                sl = slice(ci * 128, (ci + 1) * 128)
                I("dve", "tensor_tensor", out=qf[:, :, sl], in0=qm[:, :, sl], in1=QFt[:], op=ALU.mult, reads=[qm.k, QFt.k], writes=[qf.k + "_%d" % ci])
                I("dve", "tensor_tensor", out=qb[:, :, sl], in0=qm[:, :, sl], in1=QBt[:], op=ALU.mult, reads=[qm.k, QBt.k], writes=[qb.k + "_%d" % ci])
            cstate = {}

            def c_front(ci):
                nonlocal_n = cnt_c
                sl = slice(ci * 128, (ci + 1) * 128)
                if nonlocal_n[0] + 2 < len(chunks):
                    c_chunk_loads(nonlocal_n[0] + 2)
                vt_, sg_, sf_, sb_ = cl3[nonlocal_n[0] % 3]
                nonlocal_n[0] += 1
                i2 = nonlocal_n[0] % 2
                pst, pso = PS[i2], PS[2 + i2]
                for h in range(4):
                    I("pe", "matmul", pst[:, h * 128:(h + 1) * 128], lhsT=km[:, h, sl], rhs=qm[:, h, sl], start=True, stop=True,
                      reads=[km.k, qm.k], writes=[pst.k])
                sm_ = stm[i2]
                I("dve", "tensor_tensor", out=sm_[:], in0=pst[:], in1=maskT[:].rearrange("p h i -> p (h i)"), op=ALU.mult,
                  reads=[pst.k, maskT.k], writes=[sm_.k])
                for h in range(4):
                    hs = slice(h * 128, (h + 1) * 128)
                    I("pe", "matmul", pso[:, hs], lhsT=sm_[:, hs], rhs=vt_[:, hs], start=True, stop=False, reads=[sm_.k, vt_.k], writes=[pso.k])
                    I("pe", "matmul", pso[:, hs], lhsT=qf[:, h, sl], rhs=sf_[:, hs], start=False, stop=False, reads=[qf.k + "_%d" % ci, sf_.k], writes=[pso.k])
                    I("pe", "matmul", pso[:, hs], lhsT=qb[:, h, sl], rhs=sb_[:, hs], start=False, stop=True, reads=[qb.k + "_%d" % ci, sb_.k], writes=[pso.k])
                smal = small[i2]
                xc_ = xc[i2]
                for h in range(4):
                    hs = slice(h * 128, (h + 1) * 128)
                    I("act", "activation", out=junk2[:], in_=pso[:, hs], func=AF.Copy, accum_out=smal[:, h:h + 1], reads=[pso.k], writes=[junk2.k, smal.k])
                    I("act", "activation", out=junk2[:], in_=pso[:, hs], func=AF.Square, accum_out=smal[:, 4 + h:5 + h], reads=[pso.k], writes=[junk2.k, smal.k])
                I("dve", "tensor_scalar", out=smal[:, 0:4], in0=smal[:, 0:4], scalar1=1.0 / 128, scalar2=None, op0=ALU.mult, reads=[smal.k], writes=[smal.k])
                I("dve", "tensor_tensor", out=smal[:, 12:16], in0=smal[:, 0:4], in1=smal[:, 0:4], op=ALU.mult, reads=[smal.k], writes=[smal.k])
                I("dve", "scalar_tensor_tensor", out=smal[:, 8:12], in0=smal[:, 4:8], scalar=1.0 / 128, in1=smal[:, 12:16], op0=ALU.mult, op1=ALU.subtract,
                  reads=[smal.k], writes=[smal.k])
                I("act", "activation", out=smal[:, 8:12], in_=smal[:, 8:12], func=AF.Sqrt, bias=epsc[:], reads=[smal.k, epsc.k], writes=[smal.k])
                I("dve", "reciprocal", out=smal[:, 8:12], in_=smal[:, 8:12], reads=[smal.k], writes=[smal.k])
                I("dve", "scalar_tensor_tensor", out=smal[:, 12:16], in0=smal[:, 0:4], scalar=-1.0, in1=smal[:, 8:12], op0=ALU.mult, op1=ALU.mult,
                  reads=[smal.k], writes=[smal.k])
                for h in range(4):
                    hs = slice(h * 128, (h + 1) * 128)
                    I("act", "activation", out=xc_[:, hs], in_=pso[:, hs], func=AF.Identity, scale=smal[:, 8 + h:9 + h], bias=smal[:, 12 + h:13 + h],
                      reads=[pso.k, smal.k], writes=[xc_.k])
                rt_ = rett[i2]
                I("dve", "tensor_tensor", out=rt_[:], in0=xc_[:], in1=sg_[:], op=ALU.mult, reads=[xc_.k, sg_.k], writes=[rt_.k])
                cstate[ci] = (i2, rt_, pst)

            def c_back(ci):
                sl = slice(ci * 128, (ci + 1) * 128)
                i2, rt_, ptr = cstate[ci]
                for h in range(4):
                    hs = slice(h * 128, (h + 1) * 128)
                    I("pe", "transpose", out=ptr[:].bitcast(BF16)[:, hs], in_=rt_[:, hs], identity=identb[:], reads=[rt_.k, identb.k], writes=[ptr.k])
                I("act", "activation", out=yret[:, :, sl], in_=ptr[:].bitcast(BF16)[:, 0:512].rearrange("p (h t) -> p h t", h=4), func=AF.Copy,
                  reads=[ptr.k], writes=[yret.k + "_%d" % ci])

            nci = T_ // 128
            c_front(0)
            for ci in range(nci):
                if ci + 1 < nci:
                    c_front(ci + 1)
                c_back(ci)
            need_ffn = not (is_ctx and last)
            tstate = {}

            def t_front(tt):
                ntl[0] += 1
                ntile = ntl[0]
                i2 = ntile % 2
                r0 = g0 + tt * 128
                ts_ = slice(tt * 128, (tt + 1) * 128)
                x1, t2, ssq, rstd = x1s4[ntile % 4], t2s[i2], ssqs[i2], rstds[i2]
                xt = xts4[(ntile - 1) % 8]
                precast_some(4)
                if is_ctx and last:
                    tstate[tt] = None
                    return
                gm = modsel(GM, g0)
                for half in range(2):
                    cs = slice(half * 512, (half + 1) * 512)
                    py = PS[4 + half]
                    for kc in range(8):
                        src = yrg if kc < 4 else yret
                        I("pe", "matmul", py[:], lhsT=src[:, kc % 4, ts_], rhs=Wout[:, kc, cs], start=(kc == 0), stop=(kc == 7),
                          reads=[src.k if kc < 4 else yret.k + "_%d" % tt, Wout.k], writes=[py.k])
                    I("dve", "tensor_tensor", out=x1[:, cs], in0=py[:], in1=gm[:, cs], op=ALU.mult, reads=[py.k, gm.k], writes=[x1.k])
                I("dve", "tensor_tensor", out=x1[:], in0=x1[:], in1=xt[:], op=ALU.add, reads=[x1.k, xt.k], writes=[x1.k])
                dma("sp", X1[r0:r0 + 128, :], x1[:], [x1.k], ["X1"])
                I("act", "activation", out=junk[:], in_=x1[:], func=AF.Square, accum_out=ssq[:], reads=[x1.k], writes=[junk.k, ssq.k])
                sqrt_recip(rstd, ssq, 1.0 / D)
                g2, sh2 = modsel(G2, g0), modsel(SH2, g0)
                I("dve", "scalar_tensor_tensor", out=t2[:], in0=x1[:], scalar=rstd[:], in1=g2[:], op0=ALU.mult, op1=ALU.mult,
                  reads=[x1.k, rstd.k, g2.k], writes=[t2.k])
                I("dve", "tensor_tensor", out=t2[:], in0=t2[:], in1=sh2[:], op=ALU.add, reads=[t2.k, sh2.k], writes=[t2.k])
                tstate[tt] = (i2, r0, ts_, t2)

            def t_back(tt):
                if tstate[tt] is None:
                    return
                i2, r0, ts_, t2 = tstate[tt]
                if not moe_layer:
                    hb_ = h2tb[i2]
                    I("act", "activation", out=hb_[:], in_=t2[:], func=AF.Copy, reads=[t2.k], writes=[hb_.k])
                    pa = PS[6 + i2]
                    for kc in range(8):
                        I("pe", "transpose", out=pa[:].bitcast(BF16)[:, kc * 128:(kc + 1) * 128], in_=hb_[:, kc * 128:(kc + 1) * 128], identity=identb[:],
                          reads=[hb_.k, identb.k], writes=[pa.k])
                    I("act", "activation", out=h2st[:, :, ts_], in_=pa[:].bitcast(BF16)[:, 0:1024].rearrange("p (k t) -> p k t", k=8), func=AF.Copy,
                      reads=[pa.k], writes=[h2st.k])
                elif not is_ctx:
                    pa, pb = PS[6], PS[7]
                    for kc in range(8):
                        pp = pa if kc < 4 else pb
                        I("pe", "transpose", out=pp[:, (kc % 4) * 128:(kc % 4 + 1) * 128], in_=t2[:, kc * 128:(kc + 1) * 128], identity=identf[:],
                          reads=[t2.k, identf.k], writes=[pp.k])
                    hf = h2f[i2]
                    for hh, pp in enumerate((pa, pb)):
                        I("dve", "tensor_copy", out=hf[:, hh * 4:(hh + 1) * 4, :], in_=pp[:].rearrange("p (k t) -> p k t", k=4), reads=[pp.k], writes=[hf.k])
                    pl = PS[4]
                    for kc in range(8):
                        I("pe", "matmul", pl[:, 0:128], lhsT=hf[:, kc, :], rhs=wr[:, kc, :], start=(kc == 0), stop=(kc == 7),
                          reads=[hf.k, wr.k], writes=[pl.k])
                    r_ = rt[i2]
                    s_ = rsm[i2]
                    ti = (r0 - NCTX) // 128
                    I("dve", "tensor_tensor", out=r_[:, 0, :], in0=pl[:, 0:NEXP], in1=brt[:], op=ALU.add, reads=[pl.k, brt.k], writes=[r_.k])
                    I("dve", "reduce_max", out=s_[:, 0:1], in_=r_[:, 0, :], axis=AX.X, reads=[r_.k], writes=[s_.k])
                    I("dve", "tensor_scalar", out=r_[:, 1, :], in0=r_[:, 0, :], scalar1=s_[:, 0:1], scalar2=None, op0=ALU.is_ge, reads=[r_.k, s_.k], writes=[r_.k])
                    I("dve", "scalar_tensor_tensor", out=r_[:, 2, :], in0=r_[:, 1, :], scalar=-1e30, in1=r_[:, 0, :], op0=ALU.mult, op1=ALU.add, reads=[r_.k], writes=[r_.k])
                    I("dve", "reduce_max", out=s_[:, 1:2], in_=r_[:, 2, :], axis=AX.X, reads=[r_.k], writes=[s_.k])
                    I("dve", "tensor_scalar", out=r_[:, 3, :], in0=r_[:, 0, :], scalar1=s_[:, 1:2], scalar2=None, op0=ALU.is_ge, reads=[r_.k, s_.k], writes=[r_.k])
                    I("dve", "tensor_scalar", out=s_[:, 2:3], in0=s_[:, 0:1], scalar1=-1.0, scalar2=None, op0=ALU.mult, reads=[s_.k], writes=[s_.k])
                    I("act", "activation", out=r_[:, 4, :], in_=r_[:, 0, :], func=AF.Exp, bias=s_[:, 2:3], reads=[r_.k, s_.k], writes=[r_.k])
                    I("dve", "tensor_tensor", out=r_[:, 5, :], in0=r_[:, 4, :], in1=r_[:, 3, :], op=ALU.mult, reads=[r_.k], writes=[r_.k])
                    I("dve", "reduce_sum", out=s_[:, 3:4], in_=r_[:, 5, :], axis=AX.X, reads=[r_.k], writes=[s_.k])
                    I("dve", "reciprocal", out=s_[:, 4:5], in_=s_[:, 3:4], reads=[s_.k], writes=[s_.k])
                    I("dve", "tensor_scalar", out=RW[:, ti, :], in0=r_[:, 5, :], scalar1=s_[:, 4:5], scalar2=None, op0=ALU.mult, reads=[r_.k, s_.k], writes=[RW.k])
                    I("dve", "tensor_copy", out=SEL[:, ti, :], in_=r_[:, 3, :], reads=[r_.k], writes=[SEL.k])
                    I("dve", "tensor_copy", out=OH1[:, ti, :], in_=r_[:, 1, :], reads=[r_.k], writes=[OH1.k])
                    hb_ = h2tb[i2]
                    I("act", "activation", out=hb_[:], in_=t2[:], func=AF.Copy, reads=[t2.k], writes=[hb_.k])
                    dma("sp", H2T[r0 - NCTX:r0 - NCTX + 128, :], hb_[:], [hb_.k], ["H2T"])
            ntt = T_ // 128
            t_front(0)
            for tt in range(ntt):
                if tt + 1 < ntt:
                    t_front(tt + 1)
                t_back(tt)
            if need_ffn and not moe_layer:
                dma("sp", H2[:, g0:g0 + T_].rearrange("(k p) t -> p k t", p=128), h2st[:, :, 0:T_], [h2st.k], ["H2"])
            if mi + 2 < len(MACROS):
                c_macro_loads(mi + 2)

        if moe_layer:
            chk(l, "C")
            moe_sorted(l)
            continue
        A.reset(phase_base)
        P.barrier()
        GF = load_mod(5, "GF")
        if last:
            gfin = sb("gfin", [128, D], F32)
            dma("sp", gfin[:], g_final.rearrange("(o n) -> o n", o=1).broadcast_to([128, D]), [], [gfin.k])
        if moe_layer:
            blocks = [(NCTX + 1024 * i, 1024) for i in range(4)]
            experts = [(MW1b[e_], MW3b[e_], MW2b[e_], e_) for e_ in range(n_exp)]
            FC = DEXP // 128
            FS = 7
        else:
            blocks = [(0, 256)] + [(NCTX + 1024 * i, 1024) for i in range(4)]
            experts = [(FW1b, FW3b, FW2b, None)]
            FC = DFF // 128
            FS = 6
        H2bs = [sb("H2b%d" % i, [128, 8, 1024], BF16) for i in range(2)]
        Wset = [(sb("W1q%d" % i, [128, 8, 7 * 128], BF16), sb("W3q%d" % i, [128, 8, 7 * 128], BF16), sb("W2q%d" % i, [128, 7, D], BF16))
                for i in range(2)]
        actq = [sb("actq%d" % i, [128, 7, 1024], BF16) for i in range(2)]
        yacc = sb("yacc", [128, 8, D], F32)
        slt = [sb("slt%d" % i, [128, 512], F32) for i in range(3)]
        x1s = [sb("x1_%d" % i, [128, D], F32) for i in range(2)]
        junk = sb("junk", [128, D], F32)
        ssqs = [sb("ssq%d" % i, [128, 1], F32) for i in range(2)]
        rstds = [sb("rstd%d" % i, [128, 1], F32) for i in range(2)]
        units = []
        for bi, (g0, TD) in enumerate(blocks):
            per = [(w, f0, min(FS, FC - f0)) for w in experts for f0 in range(0, FC, FS)]
            for ui, (w, f0, fs) in enumerate(per):
                units.append((bi, g0, TD, w, f0, fs, ui == 0, ui == len(per) - 1))
        cntd = {"ps": 0, "fin": 0, "py": 0}

        def L13(s_):
            bi, g0, TD, w, f0, fs, fst, lst = units[s_]
            w1q, w3q, _ = Wset[s_ % 2]
            dma("sp", w1q[:, :, 0:fs * 128], w[0][:, :, f0 * 128:(f0 + fs) * 128], ["Wb"], [w1q.k])
            dma("sp", w3q[:, :, 0:fs * 128], w[1][:, :, f0 * 128:(f0 + fs) * 128], ["Wb"], [w3q.k])

        def L2(s_):
            bi, g0, TD, w, f0, fs, fst, lst = units[s_]
            w2q = Wset[s_ % 2][2]
            dma("sp", w2q[:, 0:fs, :], w[2][:, f0:f0 + fs, :], ["Wb"], [w2q.k])

        def S1(s_):
            bi, g0, TD, w, f0, fs, fst, lst = units[s_]
            w1q, w3q, _ = Wset[s_ % 2]
            aq = actq[s_ % 2]
            H2b = H2bs[bi % 2]
            if fst and bi + 1 < len(blocks):
                g0n, TDn = blocks[bi + 1]
                dma("sp", H2bs[(bi + 1) % 2][:, :, 0:TDn], H2[:, g0n:g0n + TDn].rearrange("(k p) t -> p k t", p=128), ["H2"], [H2bs[(bi + 1) % 2].k])
            pieces = [(p0, min(512, TD - p0)) for p0 in range(0, TD, 512)]
            for j in range(fs):
                for (p0, pw) in pieces:
                    cntd["ps"] += 1
                    n_ = cntd["ps"]
                    pa, pb = PS[(n_ % 2) * 2], PS[(n_ % 2) * 2 + 1]
                    for kc in range(8):
                        I("pe", "matmul", pa[:, 0:pw], lhsT=w1q[:, kc, j * 128:(j + 1) * 128], rhs=H2b[:, kc, p0:p0 + pw], start=(kc == 0), stop=(kc == 7),
                          reads=[w1q.k, H2b.k], writes=[pa.k])
                    for kc in range(8):
                        I("pe", "matmul", pb[:, 0:pw], lhsT=w3q[:, kc, j * 128:(j + 1) * 128], rhs=H2b[:, kc, p0:p0 + pw], start=(kc == 0), stop=(kc == 7),
                          reads=[w3q.k, H2b.k], writes=[pb.k])
                    sl_ = slt[n_ % len(slt)]
                    I("act", "activation", out=sl_[:, 0:pw], in_=pa[:, 0:pw], func=AF.Silu, reads=[pa.k], writes=[sl_.k])
                    I("dve", "tensor_tensor", out=aq[:, j, p0:p0 + pw], in0=sl_[:, 0:pw], in1=pb[:, 0:pw], op=ALU.mult,
                      reads=[sl_.k, pb.k], writes=[aq.k + "_%d_%d" % (j, p0)])

        def S2(s_):
            bi, g0, TD, w, f0, fs, fst, lst = units[s_]
            w2q = Wset[s_ % 2][2]
            aq = actq[s_ % 2]
            eidx = w[3]
            for tt in range(TD // 128):
                ts_ = slice(tt * 128, (tt + 1) * 128)
                for half in range(2):
                    cs = slice(half * 512, (half + 1) * 512)
                    cntd["py"] += 1
                    py = PS[4 + cntd["py"] % 4]
                    for j in range(fs):
                        I("pe", "matmul", py[:], lhsT=aq[:, j, ts_], rhs=w2q[:, j, cs], start=(j == 0), stop=(j == fs - 1),
                          reads=[aq.k + "_%d_0" % j, aq.k + "_%d_512" % j, w2q.k], writes=[py.k])
                    yk = yacc.k + "_%dh%d" % (tt, half)
                    if eidx is None:
                        if fst:
                            I("act", "activation", out=yacc[:, tt, cs], in_=py[:], func=AF.Copy, reads=[py.k], writes=[yk])
                        else:
                            I("dve", "tensor_tensor", out=yacc[:, tt, cs], in0=py[:], in1=yacc[:, tt, cs], op=ALU.add, reads=[py.k, yk], writes=[yk])
                    else:
                        ti = (g0 - NCTX) // 128 + tt
                        if fst:
                            I("dve", "tensor_scalar", out=yacc[:, tt, cs], in0=py[:], scalar1=RW[:, ti, eidx:eidx + 1], scalar2=None, op0=ALU.mult,
                              reads=[py.k, RW.k], writes=[yk])
                        else:
                            I("dve", "scalar_tensor_tensor", out=yacc[:, tt, cs], in0=py[:], scalar=RW[:, ti, eidx:eidx + 1], in1=yacc[:, tt, cs],
                              op0=ALU.mult, op1=ALU.add, reads=[py.k, RW.k, yk], writes=[yk])
            if lst:
                gf = modsel(GF, g0)
                for tt in range(TD // 128):
                    cntd["fin"] += 1
                    i2 = cntd["fin"] % 2
                    r0 = g0 + tt * 128
                    x1, ssq, rstd = x1s[i2], ssqs[i2], rstds[i2]
                    yk = yacc.k + "_%dh0" % tt
                    yk1 = yacc.k + "_%dh1" % tt
                    dma("sp", x1[:], X1[r0:r0 + 128, :], ["X1"], [x1.k])
                    I("dve", "tensor_tensor", out=yacc[:, tt, :], in0=yacc[:, tt, :], in1=gf[:], op=ALU.mult, reads=[yk, yk1, gf.k], writes=[yk, yk1])
                    I("dve", "tensor_tensor", out=x1[:], in0=x1[:], in1=yacc[:, tt, :], op=ALU.add, reads=[x1.k, yk, yk1], writes=[x1.k])
                    if not last:
                        dma("sp", XS[r0:r0 + 128, :], x1[:], [x1.k], ["XS"])
                    else:
                        I("act", "activation", out=junk[:], in_=x1[:], func=AF.Square, accum_out=ssq[:],
                          reads=[x1.k], writes=[junk.k, ssq.k])
                        sqrt_recip(rstd, ssq, 1.0 / D)
                        I("dve", "scalar_tensor_tensor", out=x1[:], in0=x1[:], scalar=rstd[:], in1=gfin[:], op0=ALU.mult, op1=ALU.mult,
                          reads=[x1.k, rstd.k, gfin.k], writes=[x1.k])
                        dma("sp", out[r0 - NCTX:r0 - NCTX + 128, :], x1[:], [x1.k], ["OUT"])

        NU = len(units)
        dma("sp", H2bs[0][:, :, 0:blocks[0][1]], H2[:, blocks[0][0]:blocks[0][0] + blocks[0][1]].rearrange("(k p) t -> p k t", p=128), ["H2"], [H2bs[0].k])
        for s_ in range(min(2, NU)):
            L13(s_)
            L2(s_)
        for s_ in range(NU):
            if s_ >= 1 and s_ + 1 < NU:
                L13(s_ + 1)
            S1(s_)
            if s_ >= 1:
                S2(s_ - 1)
                if s_ + 1 < NU:
                    L2(s_ + 1)
            if not moe_layer:
                precast_some(10)
        S2(NU - 1)

    except _Stop:
        pass
    P.barrier()
    with nc.allow_non_contiguous_dma(reason="small parameter / head-split loads"):
        P.emit()
    psum_ctx.close()
    return nc


def host_constants():
    n_freq = 32
    pos = np.arange(NLAT)
    row = (pos // 64).astype(np.float32)
    col = (pos % 64).astype(np.float32)
    inv = (10000.0 ** (-np.arange(n_freq, dtype=np.float32) / n_freq)).astype(np.float32)
    ang = np.concatenate([row[:, None] * inv, col[:, None] * inv], axis=-1).astype(np.float32)
    cos = np.cos(ang).astype(np.float32).T
    sin = np.sin(ang).astype(np.float32).T
    k_cos = np.ones((128, NT), np.float32)
    k_sin = np.zeros((128, NT), np.float32)
    k_cos[0:64, NCTX:] = cos
    k_cos[64:128, NCTX:] = cos
    k_sin[0:64, NCTX:] = -sin
    k_sin[64:128, NCTX:] = sin
    j = np.arange(128, dtype=np.float32)[:, None]
    i = np.arange(128, dtype=np.float32)[None, :]
    tab = np.zeros((128, 8, 128), np.float32)
    tab[:, 0] = np.maximum(i - j, 0)
    tab[:, 1] = (i >= j)
    tab[:, 2] = np.maximum(j - i, 0)
    tab[:, 3] = (j > i)
    tab[:, 4] = np.broadcast_to(i + 1, (128, 128))
    tab[:, 5] = np.broadcast_to(128 - i, (128, 128))
    tab[:, 6] = np.broadcast_to(127 - j, (128, 128))
    tab[:, 7] = np.broadcast_to(j, (128, 128))
    tab2 = np.zeros((128, 2, 128), np.float32)
    tab2[:, 0] = (j <= i)
    tab2[:, 1] = np.broadcast_to(j, (128, 128))
    return {
        "k_zero": np.zeros((128, 4096), np.float32).astype(ml_dtypes.bfloat16),
        "k_tab2": tab2,
        "k_identb": np.eye(128, dtype=np.float32).astype(ml_dtypes.bfloat16),
        "k_identf": np.eye(128, dtype=np.float32),
        "k_cos": k_cos, "k_sin": k_sin, "k_tab": tab,
    }


def make_in_maps(inputs, cores):
    f = lambda a: np.ascontiguousarray(np.asarray(a, dtype=np.float32))
    w_in = f(inputs["w_in"])
    idx = []
    for blk in range(8):
        b0 = 1024 + blk * 128
        idx.extend(list(range(b0 + 64, b0 + 128)) + list(range(b0, b0 + 64)))
    w_in_ext = np.ascontiguousarray(np.concatenate([w_in, w_in[:, :, idx]], axis=-1))
    shared = {
        "c_ctx": f(inputs["c_ctx"]), "w_mod": f(inputs["w_mod"]), "b_mod": f(inputs["b_mod"]),
        "g_mix": f(inputs["g_mix"]), "g_ffn": f(inputs["g_ffn"]), "g_final": f(inputs["g_final"]),
        "w_in": w_in_ext, "w_out": f(inputs["w_out"]), "conv_w": f(inputs["conv_w"]), "conv_b": f(inputs["conv_b"]),
        "rg_wa": f(inputs["rg_wa"]), "rg_ba": f(inputs["rg_ba"]), "rg_wx": f(inputs["rg_wx"]), "rg_bx": f(inputs["rg_bx"]),
        "rg_lam": f(inputs["rg_lam"]), "ret_decay": f(inputs["ret_decay"]).reshape(2, 8),
        "ffn_w1": f(inputs["ffn_w1"]), "ffn_w3": f(inputs["ffn_w3"]), "ffn_w2": f(inputs["ffn_w2"]),
        "moe_router": f(inputs["moe_router"])[0], "moe_router_b": f(inputs["moe_router_b"])[0],
        "moe_w1": f(inputs["moe_w1"])[0], "moe_w3": f(inputs["moe_w3"])[0], "moe_w2": f(inputs["moe_w2"])[0],
    }
    shared.update(host_constants())
    x = f(inputs["x"])
    ctx = f(inputs["ctx"])
    c = f(inputs["c"])
    maps = []
    for b in cores:
        m = dict(shared)
        m["xin"] = np.ascontiguousarray(np.concatenate([ctx[b], x[b]], axis=0))
        m["c_b"] = np.ascontiguousarray(c[b])
        maps.append(m)
    return maps


_NC_CACHE = {}


def kernel(**inputs):
    if "nc" not in _NC_CACHE:
        _NC_CACHE["nc"] = build_program()
    nc = _NC_CACHE["nc"]
    in_maps = make_in_maps(inputs, list(range(8)))
    res = run_bass_kernel_spmd(nc, in_maps, core_ids=list(range(8)))
    return np.stack([np.asarray(r["out"], dtype=np.float32) for r in res.results], axis=0)
```
